# Optimizing a Trainium2 kernel written in Bass

```python
import jax, jax.numpy as jnp
from jax import lax
import numpy as np

D_MODEL = 1024
BATCH = 8
SEQ = 4096
DEPTH = 2

GRID_W = 64
CTX_LEN = 256
EPS = 1e-6
N_MOD = 6

GLA_HEADS = 4
GLA_DK = D_MODEL // (2 * GLA_HEADS)
GLA_DV = D_MODEL // GLA_HEADS
GLA_RANK = 16
GLA_GATE_NORM = 16.0
CHUNK = 64

LRU_WIDTH = D_MODEL
LRU_BLOCKS = 4
LRU_BLOCK = LRU_WIDTH // LRU_BLOCKS
LRU_C = 8.0
CONV_W = 4
CONV_LEFT = 2
CONV_RIGHT = CONV_W - 1 - CONV_LEFT

RET_HEADS = 4
RET_DK = D_MODEL // (2 * RET_HEADS)
RET_DV = D_MODEL // RET_HEADS
ROPE_BASE = 10000.0

N_BRANCH = 3
BRANCH_WIDTH = D_MODEL

N_EXPERTS = 32
TOP_K = 4
D_EXPERT = D_MODEL
SWIGLU_LIMIT = 7.0
SWIGLU_ALPHA = 1.702
MOE_BLOCK = 256

IN_SIZES = (
    GLA_HEADS * GLA_DK, GLA_HEADS * GLA_DK, GLA_HEADS * GLA_DV, GLA_HEADS * GLA_DV,
    GLA_RANK, GLA_RANK,
    LRU_WIDTH, LRU_WIDTH,
    RET_HEADS * RET_DK, RET_HEADS * RET_DK, RET_HEADS * RET_DV, RET_HEADS * RET_DV,
    N_BRANCH * D_MODEL,
)
IN_TOTAL = int(sum(IN_SIZES))
IN_OFFSETS = tuple(int(o) for o in np.cumsum(IN_SIZES)[:-1])

kernel_name = 'hybrid_gla_rglru_retention_moe_dit'


def rms_norm(x, gain):
    xf = x.astype(jnp.float32)
    y = xf * lax.rsqrt(jnp.mean(xf * xf, axis=-1, keepdims=True) + EPS)
    return (y * gain.astype(jnp.float32)).astype(x.dtype)


def modulate(h, shift, scale):
    return h * (1.0 + scale) + shift


def to_heads(a, n):
    b, t, _ = a.shape
    return a.reshape(b, t, n, -1).transpose(0, 2, 1, 3)


def from_heads(a):
    b, n, t, d = a.shape
    return a.transpose(0, 2, 1, 3).reshape(b, t, n * d)


def head_norm(o, gain, center):
    o = o.astype(jnp.float32)
    if center:
        o = o - jnp.mean(o, axis=-1, keepdims=True)
    o = o * lax.rsqrt(jnp.mean(o * o, axis=-1, keepdims=True) + EPS)
    return from_heads(o) * gain.astype(jnp.float32)


def chunked_linear_attn(q, k, v, g, s0, strict):
    b, h, t, _ = q.shape
    dv = v.shape[-1]
    n = t // CHUNK

    def blocks(a):
        return a.reshape(a.shape[0], a.shape[1], n, CHUNK, a.shape[-1])

    qc, kc, vc = blocks(q), blocks(k), blocks(v)
    G = jnp.cumsum(blocks(g).astype(jnp.float32), axis=3)
    G_last = G[:, :, :, -1:, :]
    q_dec = qc * jnp.exp(G)
    k_inv = kc * jnp.exp(-G)
    k_end = kc * jnp.exp(G_last - G)
    mask = jnp.tril(jnp.ones((CHUNK, CHUNK), bool), k=-1 if strict else 0)
    scores = jnp.where(mask, jnp.einsum('bhncd,bhnsd->bhncs', q_dec, k_inv), 0.0)
    o_intra = jnp.einsum('bhncs,bhnse->bhnce', scores, vc)
    kv = jnp.einsum('bhncd,bhnce->bhnde', k_end, vc)
    decay = jnp.exp(G_last[:, :, :, 0, :])

    def step(state, inp):
        kv_n, dec_n = inp
        return dec_n[..., None] * state + kv_n, state

    final, states_in = lax.scan(step, s0, (jnp.moveaxis(kv, 2, 0), jnp.moveaxis(decay, 2, 0)))
    states_in = jnp.moveaxis(states_in, 0, 2)
    o_inter = jnp.einsum('bhncd,bhnde->bhnce', q_dec, states_in)
    return (o_intra + o_inter).reshape(b, h, t, dv), final


def bidir_linear_attn(q, k, v, g_f, g_b, s0_f, s0_b):
    flip = lambda a: jnp.flip(a, axis=2)
    o_f, s_f = chunked_linear_attn(q, k, v, g_f, s0_f, strict=False)
    o_b, s_b = chunked_linear_attn(flip(q), flip(k), flip(v), flip(g_b), s0_b, strict=True)
    return o_f + flip(o_b), s_f, s_b


def gla_inputs(q, k, v, lr_f, lr_b, wa2, ba):
    qh = to_heads(q, GLA_HEADS).astype(jnp.float32) * GLA_DK ** -0.5
    kh = to_heads(k, GLA_HEADS)
    vh = to_heads(v, GLA_HEADS)
    g_f = to_heads(jax.nn.log_sigmoid((lr_f @ wa2[0] + ba[0]).astype(jnp.float32)) / GLA_GATE_NORM, GLA_HEADS)
    g_b = to_heads(jax.nn.log_sigmoid((lr_b @ wa2[1] + ba[1]).astype(jnp.float32)) / GLA_GATE_NORM, GLA_HEADS)
    return qh, kh, vh, g_f, g_b


def axial_rope(rows, head_dim):
    row = jnp.broadcast_to(jnp.arange(rows)[:, None], (rows, GRID_W)).reshape(-1).astype(jnp.float32)
    col = jnp.broadcast_to(jnp.arange(GRID_W)[None, :], (rows, GRID_W)).reshape(-1).astype(jnp.float32)
    n_freq = head_dim // 4
    inv = ROPE_BASE ** (-jnp.arange(n_freq, dtype=jnp.float32) / n_freq)
    ang = jnp.concatenate([row[:, None] * inv, col[:, None] * inv], axis=-1)
    return jnp.cos(ang), jnp.sin(ang)


def apply_rope(x, cos, sin):
    half = x.shape[-1] // 2
    x1, x2 = x[..., :half], x[..., half:]
    return jnp.concatenate([x1 * cos - x2 * sin, x1 * sin + x2 * cos], axis=-1)


def ret_inputs(q, k, v, cos, sin):
    qh = to_heads(q, RET_HEADS).astype(jnp.float32)
    kh = to_heads(k, RET_HEADS).astype(jnp.float32) * RET_DK ** -0.5
    if cos is not None:
        qh = apply_rope(qh, cos, sin)
        kh = apply_rope(kh, cos, sin)
    return qh, kh, to_heads(v, RET_HEADS)


def ret_log_decay(n_tokens):
    gamma = 1.0 - 2.0 ** (-5.0 - jnp.arange(RET_HEADS, dtype=jnp.float32))
    return jnp.broadcast_to(jnp.log(gamma)[None, :, None, None], (1, RET_HEADS, n_tokens, 1))


def depthwise_conv(x, w, b):
    y = lax.conv_general_dilated(x, w[:, None, :].astype(x.dtype), window_strides=(1,),
                                 padding=[(CONV_LEFT, CONV_RIGHT)],
                                 dimension_numbers=('NWC', 'WIO', 'NWC'),
                                 feature_group_count=x.shape[-1])
    return y + b


def block_diag(x, w):
    b, t, c = x.shape
    return jnp.einsum('btgi,gij->btgj', x.reshape(b, t, LRU_BLOCKS, LRU_BLOCK), w).reshape(b, t, c)


def linear_scan(a, b, h0):
    b = b.at[:, 0].add(a[:, 0] * h0)

    def combine(e1, e2):
        a1, b1 = e1
        a2, b2 = e2
        return a1 * a2, a2 * b1 + b2

    _, h = lax.associative_scan(combine, (a, b), axis=1)
    return h


def rglru_scan(xc, wa, ba, wi, bi, lam, h0):
    r = jax.nn.sigmoid((block_diag(xc, wa) + ba).astype(jnp.float32))
    i = jax.nn.sigmoid((block_diag(xc, wi) + bi).astype(jnp.float32))
    log_a = LRU_C * r * jax.nn.log_sigmoid(lam.astype(jnp.float32))
    a = jnp.exp(log_a)
    b = jnp.sqrt(-jnp.expm1(2.0 * log_a)) * (i * xc.astype(jnp.float32))
    h = linear_scan(a, b, h0)
    return h, h[:, -1]


def rglru_bidir(xc, wa, ba, wi, bi, lam, h0_f, h0_b):
    h_f, last_f = rglru_scan(xc, wa[0], ba[0], wi[0], bi[0], lam[0], h0_f)
    h_b, last_b = rglru_scan(jnp.flip(xc, 1), wa[1], ba[1], wi[1], bi[1], lam[1], h0_b)
    return h_f + jnp.flip(h_b, 1), last_f, last_b


def hybrid_mixer(xn, cn, rope_cos, rope_sin, w_in, gla_wa2, gla_ba, gla_norm, lru_conv_w, lru_conv_b,
                 lru_wa, lru_ba, lru_wi, lru_bi, lru_lam, ret_norm, w_branch, w_out, with_ctx_out):
    f32 = jnp.float32
    nb = xn.shape[0]
    px = jnp.split(xn @ w_in, IN_OFFSETS, axis=-1)
    pc = jnp.split(cn @ w_in, IN_OFFSETS, axis=-1)

    zero_gla = jnp.zeros((nb, GLA_HEADS, GLA_DK, GLA_DV), f32)
    o_gla_c, gs_f, gs_b = bidir_linear_attn(*gla_inputs(*pc[0:3], *pc[4:6], gla_wa2, gla_ba), zero_gla, zero_gla)
    o_gla_x, _, _ = bidir_linear_attn(*gla_inputs(*px[0:3], *px[4:6], gla_wa2, gla_ba), gs_f, gs_b)

    lru_p = (lru_wa, lru_ba, lru_wi, lru_bi, lru_lam)
    zero_lru = jnp.zeros((nb, LRU_WIDTH), f32)
    h_c, hs_f, hs_b = rglru_bidir(depthwise_conv(pc[6], lru_conv_w, lru_conv_b), *lru_p, zero_lru, zero_lru)
    h_x, _, _ = rglru_bidir(depthwise_conv(px[6], lru_conv_w, lru_conv_b), *lru_p, hs_f, hs_b)

    zero_ret = jnp.zeros((nb, RET_HEADS, RET_DK, RET_DV), f32)
    g_c = ret_log_decay(cn.shape[1])
    o_ret_c, rs_f, rs_b = bidir_linear_attn(*ret_inputs(*pc[8:11], None, None), g_c, g_c, zero_ret, zero_ret)
    g_x = ret_log_decay(xn.shape[1])
    o_ret_x, _, _ = bidir_linear_attn(*ret_inputs(*px[8:11], rope_cos, rope_sin), g_x, g_x, rs_f, rs_b)

    def finish(p, o_gla, h_lru, o_ret, dtype):
        gla = head_norm(o_gla, gla_norm, False) * jax.nn.silu(p[3].astype(f32))
        lru = h_lru * jax.nn.gelu(p[7].astype(f32))
        ret = head_norm(o_ret, ret_norm, True) * jax.nn.silu(p[11].astype(f32))
        g_gla, g_lru, g_ret = jnp.split(jax.nn.sigmoid(p[12].astype(f32)), N_BRANCH, axis=-1)
        merged = (g_gla * (gla.astype(dtype) @ w_branch[0])
                  + g_lru * (lru.astype(dtype) @ w_branch[1])
                  + g_ret * (ret.astype(dtype) @ w_branch[2]))
        return merged.astype(dtype) @ w_out

    out_x = finish(px, o_gla_x, h_x, o_ret_x, xn.dtype)
    out_c = finish(pc, o_gla_c, h_c, o_ret_c, cn.dtype) if with_ctx_out else None
    return out_x, out_c


def moe_ffn(h, w_router, b_router, w_gu, b_gu, w_down, b_down):
    n_tok, d = h.shape
    n_assign = n_tok * TOP_K
    logits = (h @ w_router).astype(jnp.float32) + b_router.astype(jnp.float32)
    top_logits, top_idx = lax.top_k(logits, TOP_K)
    top_w = jax.nn.softmax(top_logits, axis=-1)
    flat_e = top_idx.reshape(-1)
    order = jnp.argsort(flat_e)
    sorted_e = flat_e[order]
    sorted_tok = (order // TOP_K).astype(jnp.int32)
    sorted_w = top_w.reshape(-1)[order]
    counts = jnp.bincount(flat_e, length=N_EXPERTS)
    padded = (counts + MOE_BLOCK - 1) // MOE_BLOCK * MOE_BLOCK
    pad_end = jnp.cumsum(padded)
    pad_start = pad_end - padded
    grp_start = jnp.cumsum(counts) - counts
    slot = pad_start[sorted_e] + jnp.arange(n_assign) - grp_start[sorted_e]
    n_blocks = -(-n_assign // MOE_BLOCK) + N_EXPERTS
    n_slots = n_blocks * MOE_BLOCK
    slot_tok = jnp.full((n_slots,), n_tok, jnp.int32).at[slot].set(sorted_tok)
    slot_w = jnp.zeros((n_slots,), jnp.float32).at[slot].set(sorted_w)
    block_e = jnp.minimum(jnp.searchsorted(pad_end, jnp.arange(n_blocks) * MOE_BLOCK, side='right'),
                          N_EXPERTS - 1)
    h_pad = jnp.concatenate([h, jnp.zeros((1, d), h.dtype)], axis=0)

    def expert_block(acc, blk):
        tok, wgt, e = blk
        gu = h_pad[tok] @ w_gu[e] + b_gu[e]
        gate = jnp.minimum(gu[:, 0::2], SWIGLU_LIMIT)
        up = jnp.clip(gu[:, 1::2], -SWIGLU_LIMIT, SWIGLU_LIMIT)
        act = gate * jax.nn.sigmoid(SWIGLU_ALPHA * gate) * (up + 1.0)
        y = act @ w_down[e] + b_down[e]
        return acc.at[tok].add(y.astype(jnp.float32) * wgt[:, None]), None

    acc, _ = lax.scan(expert_block, jnp.zeros((n_tok + 1, d), jnp.float32),
                      (slot_tok.reshape(n_blocks, MOE_BLOCK), slot_w.reshape(n_blocks, MOE_BLOCK), block_e))
    return acc[:n_tok].astype(h.dtype)


def setup_inputs(seed: int = 0) -> dict:
    key = jax.random.key(seed)
    ks = jax.random.split(key, 32)
    f32 = jnp.float32
    D = D_MODEL
    L = DEPTH

    def nrm(k, shape, scale):
        return jax.random.normal(k, shape, f32) * scale

    a0 = jax.random.uniform(ks[17], (L, 2, LRU_WIDTH), f32, 0.9, 0.999)
    root = a0 ** (1.0 / LRU_C)
    lru_lam = jnp.log(root) - jnp.log1p(-root)
    return {
        'x': nrm(ks[0], (BATCH, SEQ, D), 1.0),
        'c': nrm(ks[1], (BATCH, D), 1.0),
        'ctx': nrm(ks[2], (BATCH, CTX_LEN, D), 1.0),
        'c_ctx': nrm(ks[3], (D,), 1.0),
        'w_ada': nrm(ks[4], (L, D, N_MOD * D), 0.5 * D ** -0.5),
        'b_ada': nrm(ks[5], (L, N_MOD * D), 0.02),
        'norm1': 1.0 + nrm(ks[6], (L, D), 0.02),
        'norm2': 1.0 + nrm(ks[7], (L, D), 0.02),
        'w_in': nrm(ks[8], (L, D, IN_TOTAL), D ** -0.5),
        'gla_wa2': nrm(ks[9], (L, 2, GLA_RANK, GLA_HEADS * GLA_DK), GLA_RANK ** -0.5),
        'gla_ba': nrm(ks[10], (L, 2, GLA_HEADS * GLA_DK), 0.1),
        'gla_norm': 1.0 + nrm(ks[11], (L, GLA_HEADS * GLA_DV), 0.02),
        'lru_conv_w': nrm(ks[12], (L, CONV_W, LRU_WIDTH), CONV_W ** -0.5),
        'lru_conv_b': nrm(ks[13], (L, LRU_WIDTH), 0.02),
        'lru_wa': nrm(ks[14], (L, 2, LRU_BLOCKS, LRU_BLOCK, LRU_BLOCK), LRU_BLOCK ** -0.5),
        'lru_ba': nrm(ks[15], (L, 2, LRU_WIDTH), 0.02),
        'lru_wi': nrm(ks[16], (L, 2, LRU_BLOCKS, LRU_BLOCK, LRU_BLOCK), LRU_BLOCK ** -0.5),
        'lru_bi': nrm(ks[18], (L, 2, LRU_WIDTH), 0.02),
        'lru_lam': lru_lam,
        'ret_norm': 1.0 + nrm(ks[19], (L, RET_HEADS * RET_DV), 0.02),
        'w_branch': nrm(ks[20], (L, N_BRANCH, BRANCH_WIDTH, D), BRANCH_WIDTH ** -0.5),
        'w_out': nrm(ks[21], (L, D, D), D ** -0.5),
        'w_router': nrm(ks[22], (L, D, N_EXPERTS), D ** -0.5),
        'b_router': nrm(ks[23], (L, N_EXPERTS), 0.01),
        'w_gu': nrm(ks[24], (L, N_EXPERTS, D, 2 * D_EXPERT), D ** -0.5),
        'b_gu': nrm(ks[25], (L, N_EXPERTS, 2 * D_EXPERT), 0.02),
        'w_down': nrm(ks[26], (L, N_EXPERTS, D_EXPERT, D), D_EXPERT ** -0.5),
        'b_down': nrm(ks[27], (L, N_EXPERTS, D), 0.02),
        'final_norm': 1.0 + nrm(ks[28], (D,), 0.02),
    }


def reference(x, c, ctx, c_ctx, w_ada, b_ada, norm1, norm2, w_in, gla_wa2, gla_ba, gla_norm,
              lru_conv_w, lru_conv_b, lru_wa, lru_ba, lru_wi, lru_bi, lru_lam, ret_norm,
              w_branch, w_out, w_router, b_router, w_gu, b_gu, w_down, b_down, final_norm):
    nb, n_lat, d = x.shape
    rows = n_lat // GRID_W
    rope_cos, rope_sin = axial_rope(rows, RET_DK)
    silu_c = jax.nn.silu(c)
    silu_cc = jax.nn.silu(c_ctx)
    for li in range(DEPTH):
        last = li == DEPTH - 1
        mx = (silu_c @ w_ada[li] + b_ada[li]).reshape(nb, N_MOD, 1, d)
        mc = (silu_cc @ w_ada[li] + b_ada[li]).reshape(N_MOD, d)
        xn = modulate(rms_norm(x, norm1[li]), mx[:, 0], mx[:, 1])
        cn = modulate(rms_norm(ctx, norm1[li]), mc[0], mc[1])
        mix_x, mix_c = hybrid_mixer(xn, cn, rope_cos, rope_sin, w_in[li], gla_wa2[li], gla_ba[li], gla_norm[li],
                                    lru_conv_w[li], lru_conv_b[li], lru_wa[li], lru_ba[li], lru_wi[li],
                                    lru_bi[li], lru_lam[li], ret_norm[li], w_branch[li], w_out[li],
                                    not last)
        x = x + mx[:, 2] * mix_x
        xn2 = modulate(rms_norm(x, norm2[li]), mx[:, 3], mx[:, 4])
        moe_p = (w_router[li], b_router[li], w_gu[li], b_gu[li], w_down[li], b_down[li])
        if last:
            x = x + mx[:, 5] * moe_ffn(xn2.reshape(-1, d), *moe_p).reshape(x.shape)
        else:
            ctx = ctx + mc[2] * mix_c
            cn2 = modulate(rms_norm(ctx, norm2[li]), mc[3], mc[4])
            y = moe_ffn(jnp.concatenate([xn2.reshape(-1, d), cn2.reshape(-1, d)], axis=0), *moe_p)
            x = x + mx[:, 5] * y[:nb * n_lat].reshape(x.shape)
            ctx = ctx + mc[5] * y[nb * n_lat:].reshape(ctx.shape)
    return rms_norm(x, final_norm)
```

```python
import numpy as np
import ml_dtypes
from contextlib import ExitStack
import concourse.bass as bass
import concourse.mybir as mybir
from concourse.ap import AP
from concourse.bass_utils import run_bass_kernel_spmd

F32 = mybir.dt.float32
BF16 = mybir.dt.bfloat16
AF = mybir.ActivationFunctionType
ALU = mybir.AluOpType
AX = mybir.AxisListType

SEM_EPOCH = 30000
D = 1024
NCTX = 256
NLAT = 4096
TALL = NCTX + NLAT
NT = TALL // 128
EPS = 1e-6
NCOLP = 11296 + 1024
OFF = dict(q=0, k=512, v=1024, p3=2048, lrf=3072, lrb=3088, p6=3104, p7=4128, rq=5152, rk=5664, rv=6176,
           p11=7200, p12=8224, rqs=11296, rks=11808)


class Buf:
    __slots__ = ("name", "last_w", "readers")

    def __init__(self, name):
        self.name = name
        self.last_w = None
        self.readers = []


class DmaGroup:
    def __init__(self, sem, name):
        self.sem = sem
        self.count = 0
        self.name = name


class Op:
    __slots__ = ("eng", "fn", "waits", "signal", "idx", "semval", "dma_group")

    def __init__(self, eng, fn, idx):
        self.eng = eng
        self.fn = fn
        self.idx = idx
        self.waits = []
        self.signal = False
        self.semval = None
        self.dma_group = None


class TT_:
    def __init__(self, h, name):
        self.h = h
        self.b = Buf(name)
        self.dsem = None

    def __getitem__(self, k):
        return self.h[k]


class Prog:
    ENGS = ("pe", "act", "dve", "pool", "sp")

    def __init__(self, nc, gstack):
        self.nc = nc
        self.gstack = gstack
        self.base = {e: 0 for e in self.ENGS}
        self.ops = {e: [] for e in self.ENGS}
        self.waited_ops = {e: {x: -1 for x in self.ENGS} for e in self.ENGS}
        self.waited_dma = {e: {} for e in self.ENGS}
        self.groups = []
        self.nsem = 0
        self.cur_sem = {e: None for e in self.ENGS}
        self.cur_cnt = {e: 0 for e in self.ENGS}
        self.bufs = []
        self.ninst = 0
        self.free_dsems = {}
        self.used_dsems = []
        self.gen = 0
        self.eng_sems = {}

    def tile_sem(self, t, queue="sp"):
        if t.dsem is None or getattr(t, "dsem_gen", -1) != self.gen:
            t.dsem_gen = self.gen
            fl = self.free_dsems.setdefault(queue, [])
            if fl:
                t.dsem = fl.pop()
            else:
                t.dsem = DmaGroup(self.new_sem(f"d{self.nsem}"), f"d{self.nsem}")
                t.dsem.queue = queue
            self.used_dsems.append(t.dsem)
        assert t.dsem.queue == queue, "tile DMA'd from two queue types"
        return t.dsem

    def new_sem(self, name):
        self.nsem += 1
        return self.gstack.enter_context(self.nc.semaphore(name))

    def group(self, name):
        g = DmaGroup(self.new_sem("g_" + name), name)
        self.groups.append(g)
        return g

    def buf(self, name):
        b = Buf(name)
        self.bufs.append(b)
        return b

    def _add_dep(self, op, tok):
        if tok is None:
            return
        E = op.eng
        if tok[0] == "op":
            x = tok[1]
            if x.eng == E and E == "pe":
                return
            if self.waited_ops[E][x.eng] >= x.idx:
                return
            self.waited_ops[E][x.eng] = x.idx
            x.signal = True
            op.waits.append(tok)
        else:
            _, g, cnt, gen = tok
            if gen != self.gen:
                return
            if self.waited_dma[E].get(g, 0) >= cnt:
                return
            self.waited_dma[E][g] = cnt
            op.waits.append(tok)

    def _record(self, eng, fn, reads, writes, dma_group=None):
        op = Op(eng, fn, self.base[eng] + len(self.ops[eng]))
        self.ops[eng].append(op)
        for b in reads:
            self._add_dep(op, b.last_w)
        for b in writes:
            self._add_dep(op, b.last_w)
            for r in b.readers:
                self._add_dep(op, r)
        if dma_group is not None:
            dma_group.count += 1
            op.dma_group = dma_group
            tok = ("dma", dma_group, dma_group.count, self.gen)
        else:
            tok = ("op", op)
        for b in writes:
            b.last_w = tok
            b.readers = []
        for b in reads:
            if b not in writes:
                b.readers.append(tok)
        return op

    def op(self, eng, fn, reads=(), writes=()):
        rd = [r.b if isinstance(r, TT_) else r for r in reads if not getattr(r, "is_psum", False)]
        wr = [w.b if isinstance(w, TT_) else w for w in writes]
        wr += [r.b for r in reads if getattr(r, "is_psum", False) and r.b not in wr]
        return self._record(eng, fn, rd, wr)

    def dma(self, queue, out, in_, tile, reads=(), writes=(), **kw):
        def fn(e):
            return e.dma_start(out=out, in_=in_, **kw)
        return self._record(queue, fn, [r.b if isinstance(r, TT_) else r for r in reads],
                            [w.b if isinstance(w, TT_) else w for w in writes], dma_group=self.tile_sem(tile, queue))

    def _simulate(self, name):
        if not hasattr(self, "simvals"):
            self.simvals = {}
        vals = self.simvals
        pc = {e: 0 for e in self.ENGS}
        progress = True
        while progress:
            progress = False
            for e in self.ENGS:
                ops = self.ops[e]
                while pc[e] < len(ops):
                    op = ops[pc[e]]
                    ok = True
                    for w in op.waits:
                        if w[0] == "op":
                            s_, v = w[1].semval
                            if vals.get(id(s_), 0) < v:
                                ok = False
                        else:
                            if vals.get(id(w[1]), 0) < 16 * w[2]:
                                ok = False
                    if not ok:
                        break
                    if op.dma_group is not None:
                        vals[id(op.dma_group)] = vals.get(id(op.dma_group), 0) + 16
                    elif op.signal:
                        vals[id(op.semval[0])] = vals.get(id(op.semval[0]), 0) + 1
                        assert vals[id(op.semval[0])] == op.semval[1], (name, e, pc[e])
                    pc[e] += 1
                    progress = True
        for e in self.ENGS:
            if pc[e] < len(self.ops[e]):
                op = self.ops[e][pc[e]]
                desc = []
                for w in op.waits:
                    if w[0] == "op":
                        desc.append(("op", w[1].eng, w[1].idx, w[1].semval[1], vals.get(id(w[1].semval[0]), 0)))
                    else:
                        desc.append(("dma", w[1].name, 16 * w[2], vals.get(id(w[1]), 0)))
                raise RuntimeError(f"DEADLOCK in phase {name}: engine {e} stuck at op {pc[e]}/{len(self.ops[e])} waits={desc}")

    def end_phase(self, name):
        nc = self.nc
        fin = Op("sp", lambda e: e.nop(), self.base["sp"] + len(self.ops["sp"]))
        for g in self.used_dsems:
            if g.count > self.waited_dma["sp"].get(g, 0):
                fin.waits.append(("dma", g, g.count, self.gen))
                self.waited_dma["sp"][g] = g.count
        self.ops["sp"].append(fin)
        self.phase_eng_sems = []
        for e in self.ENGS:
            epoch = 0
            cnt = 0
            sem = None
            for op in self.ops[e]:
                if op.signal and op.dma_group is None:
                    if sem is None or cnt >= SEM_EPOCH:
                        lst = self.eng_sems.setdefault(e, [])
                        if epoch >= len(lst):
                            lst.append(self.new_sem(f"e_{e}_{epoch}"))
                        sem = lst[epoch]
                        epoch += 1
                        cnt = 0
                        self.phase_eng_sems.append(sem)
                    cnt += 1
                    op.semval = (sem, cnt)
        self._simulate(name)
        with nc.Block() as block:
            def run(e, handle):
                for op in self.ops[e]:
                    for w in op.waits:
                        if w[0] == "op":
                            s, v = w[1].semval
                            handle.wait_ge(s, v)
                        else:
                            handle.wait_ge(w[1].sem, 16 * w[2])
                    ins = op.fn(handle)
                    self.ninst += 1
                    if op.dma_group is not None:
                        ins.then_inc(op.dma_group.sem, 16)
                    elif op.signal:
                        ins.then_inc(op.semval[0], 1)

            @block.tensor
            def _(h):
                run("pe", h)

            @block.scalar
            def _(h):
                run("act", h)

            @block.vector
            def _(h):
                run("dve", h)

            @block.gpsimd
            def _(h):
                run("pool", h)

            @block.sync
            def _(h):
                run("sp", h)
        used_eng_sems = list(self.phase_eng_sems)
        dsems = list(self.used_dsems)
        with nc.Block() as block2:
            @block2.sync
            def _(h):
                for g in dsems:
                    if g.queue != "pool":
                        h.sem_clear(g.sem)
                for s_ in used_eng_sems:
                    h.sem_clear(s_)
        if hasattr(self, "simvals"):
            self.simvals = {}
        for e in self.ENGS:
            self.base[e] += len(self.ops[e])
            self.ops[e] = []
            self.cur_cnt[e] = 0
        for e in self.ENGS:
            for x in self.ENGS:
                self.waited_ops[e][x] = self.base[x] - 1
            self.waited_dma[e] = {}
        for b in self.bufs:
            b.last_w = None
            b.readers = []
        for g in dsems:
            assert 16 * g.count < 60000, (g.name, g.count)
            if g.queue != "pool":
                g.count = 0
                self.free_dsems.setdefault(g.queue, []).append(g)
        self.used_dsems = []
        self.gen += 1


class Pool:
    def __init__(self, alloc, name, shape, dt, n):
        self.items = [TT_(alloc(f"{name}{i}", shape, dt), f"{name}{i}") for i in range(n)]
        self.i = 0

    def next(self):
        t = self.items[self.i % len(self.items)]
        self.i += 1
        return t


class K:
    pass


def build_program(debug_outs=(), stop_after=None, n_layers=2, n_exp=32):
    nc = bass.Bass("TRN2", target_bir_lowering=False)
    k = K()
    k.nc = nc

    def din(name, shape, dt=F32):
        return nc.dram_tensor(name, list(shape), dt, kind="ExternalInput").ap()

    def dscr(name, shape, dt=F32):
        kind = "ExternalOutput" if name in debug_outs else "Internal"
        return nc.dram_tensor(name, list(shape), dt, kind=kind).ap()

    I = K()
    I.xall = din("xall", [TALL, D])
    I.cvec = din("cvec", [128, 16])
    I.w_ada = din("w_ada", [2, D, 6 * D])
    I.b_ada = din("b_ada", [2, 6 * D])
    I.norm1 = din("norm1", [2, D])
    I.norm2 = din("norm2", [2, D])
    I.final_norm = din("final_norm", [D])
    I.w_in = din("w_in_p", [2, D, NCOLP])
    I.gla_wa2 = din("gla_wa2", [2, 2, 16, 512])
    I.gla_ba = din("gla_ba", [2, 2, 512])
    I.gla_norm = din("gla_norm", [2, D])
    I.ret_norm = din("ret_norm", [2, D])
    I.convw = din("convw", [2, 128, 8, 5])
    I.lruw = din("lruw", [2, 128, 32, 256])
    I.lruv = din("lruv", [2, 128, 48])
    I.w_branch = din("w_branch", [2, 3, D, D])
    I.w_out = din("w_out", [2, D, D])
    I.w_router = din("w_router", [2, D, 32])
    I.b_router = din("b_router", [2, 32])
    I.w_gu = din("w_gu_d", [2, n_exp, D, 2048])
    I.bgu = din("bgu", [2, 128, 32 * 16])
    I.w_down = din("w_down", [2, n_exp, D, D])
    I.b_down = din("b_down", [2, 32, D])
    I.ident = din("ident", [128, 128])
    I.tri = din("tri", [2, 128, 128])
    I.maskT = din("maskT", [2, 128, 512])
    I.cosT = din("cosT", [128, TALL])
    I.sinT = din("sinT", [128, TALL])
    I.cos2 = din("cos2", [TALL, 512])
    I.sin2 = din("sin2", [TALL, 512])
    I.rtab = din("rtab", [2, 3, 128, 512])
    I.rdec = din("rdec", [2, 128, 4])
    out = nc.dram_tensor("out", [NLAT, D], F32, kind="ExternalOutput").ap()

    S = K()
    S.modv = dscr("modv", [2, 6 * D])
    S.X = dscr("X", [TALL, D])
    S.QT = dscr("QT", [512, TALL], BF16)
    S.KT = dscr("KT", [512, TALL], BF16)
    S.LRT = dscr("LRT", [32, TALL], BF16)
    S.P6T = dscr("P6T", [D, TALL], F32)
    S.P7T = dscr("P7T", [D, TALL], BF16)
    S.RQT = dscr("RQT", [512, TALL], BF16)
    S.RKT = dscr("RKT", [512, TALL], BF16)
    S.P12T = dscr("P12T", [3 * D, TALL], BF16)
    S.Kt = dscr("Kt", [TALL, 512], BF16)
    S.Vt = dscr("Vt", [TALL, D], BF16)
    S.P3 = dscr("P3", [TALL, D], BF16)
    S.RKt = dscr("RKt", [TALL, 512], BF16)
    S.RVt = dscr("RVt", [TALL, D], BF16)
    S.P11 = dscr("P11", [TALL, D], BF16)
    S.OG = [dscr("OGf", [TALL, D]), dscr("OGb", [TALL, D])]
    S.OR = [dscr("ORf", [TALL, D]), dscr("ORb", [TALL, D])]
    S.LRUT = dscr("LRUT", [D, TALL], BF16)
    S.XN2T = dscr("XN2T", [D, TALL], BF16)
    S.GLAT = dscr("GLAT", [D, TALL], BF16)
    S.RETT = dscr("RETT", [D, TALL], BF16)
    S.WR = dscr("WR", [TALL, 64])
    S.XNT = dscr("XNT", [D, TALL], BF16) if "XNT" in debug_outs else None
    S.XNT2 = dscr("XNT2", [D, TALL], BF16) if "XNT2" in debug_outs else None
    k.debug_outs = debug_outs

    with ExitStack() as gst:
        P = Prog(nc, gst)
        k.P = P
        B = K()
        for n in ["modv", "X", "QT", "KT", "LRT", "P6T", "P7T", "RQT", "RKT", "P12T", "Kt", "Vt", "P3", "RKt", "RVt",
                  "P11", "OGf", "OGb", "ORf", "ORb", "LRUT", "XN2T", "WR", "out", "GLAT", "RETT"]:
            setattr(B, n, P.buf(n))
        G = K()
        for n in ["ld0", "ld1", "ld2", "ld3", "w0", "w1", "w2", "st0", "st1", "st2", "st3", "out"]:
            setattr(G, n, None)
        k.I, k.S, k.B, k.G, k.out = I, S, B, G, out

        k.stop_after = stop_after
        for li in range(n_layers):
            last = li == 1
            x_src = I.xall if li == 0 else S.X
            seq = [("ada", lambda: phase_ada(k, li)), ("ab", lambda: phase_ab(k, li, x_src)), ("c1", lambda: phase_c1(k, li)),
                   ("c2", lambda: phase_c2(k, li)), ("d1", lambda: phase_d1(k, li, last)), ("d2", lambda: phase_d2(k, li, x_src, last)),
                   ("f", lambda: phase_f(k, li, last))]
            done = False
            for name, fn in seq:
                fn()
                if stop_after == (name, li) or (name == "ab" and stop_after == ("a", li)):
                    done = True
                    break
            if done:
                break
    k.ninst = P.ninst
    return nc, k


def mk_helpers(k, st):
    nc, P = k.nc, k.P

    def uniq(name):
        k.uid = getattr(k, "uid", 0) + 1
        return f"{name}_u{k.uid}"

    def sb(name, shape, dt=F32):
        return TT_(st.enter_context(nc.sbuf_tensor(uniq(name), list(shape), dt)), name)

    def ps(name, shape, dt=F32):
        t = TT_(st.enter_context(nc.psum_tensor(uniq(name), list(shape), dt)), name)
        t.is_psum = True
        return t

    def sb_raw(name, shape, dt=F32):
        return st.enter_context(nc.sbuf_tensor(name, list(shape), dt))

    def MM(out, out_ap, lhsT, rhs, rd, start=True, stop=True):
        P.op("pe", lambda e: e.matmul(out_ap, lhsT=lhsT, rhs=rhs, start=start, stop=stop), reads=rd, writes=[out])

    def TR(out, out_ap, in_ap, ident_ap, rd):
        P.op("pe", lambda e: e.transpose(out=out_ap, in_=in_ap, identity=ident_ap), reads=rd, writes=[out])

    def ACT(out, out_ap, in_ap, func, rd, bias=None, scale=None, accum=None, extra_w=()):
        kw = {}
        if bias is not None:
            kw["bias"] = bias
        if scale is not None:
            kw["scale"] = scale
        if accum is not None:
            kw["accum_out"] = accum
        P.op("act", lambda e: e.activation(out=out_ap, in_=in_ap, func=func, **kw), reads=rd, writes=[out] + list(extra_w))

    def TT(eng, out, out_ap, in0, in1, op, rd):
        P.op(eng, lambda e: e.tensor_tensor(out=out_ap, in0=in0, in1=in1, op=op), reads=rd, writes=[out])

    def TS(eng, out, out_ap, in0, s1, s2, op0, op1, rd, accum=None, extra_w=()):
        if op1 is None:
            P.op(eng, lambda e: e.tensor_scalar(out=out_ap, in0=in0, scalar1=s1, scalar2=None, op0=op0), reads=rd, writes=[out])
        elif accum is not None:
            P.op(eng, lambda e: e.tensor_scalar(out=out_ap, in0=in0, scalar1=s1, scalar2=s2, op0=op0, op1=op1, accum_out=accum),
                 reads=rd, writes=[out] + list(extra_w))
        else:
            P.op(eng, lambda e: e.tensor_scalar(out=out_ap, in0=in0, scalar1=s1, scalar2=s2, op0=op0, op1=op1), reads=rd, writes=[out])

    def STT(eng, out, out_ap, in0, scalar, in1, op0, op1, rd):
        P.op(eng, lambda e: e.scalar_tensor_tensor(out=out_ap, in0=in0, scalar=scalar, in1=in1, op0=op0, op1=op1),
             reads=rd, writes=[out])

    def CP(eng, out, out_ap, in_ap, rd):
        if eng == "act":
            P.op("act", lambda e: e.copy(out=out_ap, in_=in_ap), reads=rd, writes=[out])
        else:
            P.op(eng, lambda e: e.tensor_copy(out=out_ap, in_=in_ap), reads=rd, writes=[out])

    def RECIP(out, out_ap, in_ap, rd):
        P.op("dve", lambda e: e.reciprocal(out=out_ap, in_=in_ap), reads=rd, writes=[out])

    def MEMSET(eng, out, out_ap, val):
        P.op(eng, lambda e: e.memset(out_ap, val), reads=[], writes=[out])

    def LD(queue, dst, dst_ap, src_ap, group, src_bufs=(), **kw):
        P.dma(queue, dst_ap, src_ap, dst, reads=list(src_bufs), writes=[dst], **kw)

    def ST(queue, dst_ap, src, src_ap, group, dst_buf, **kw):
        P.dma(queue, dst_ap, src_ap, src, reads=[src], writes=[dst_buf], **kw)

    h = K()
    for n, f in list(locals().items()):
        if callable(f) and n not in ("h",):
            setattr(h, n, f)
    return h


def bcast_row(ap_row, n=128):
    return ap_row.partition_broadcast(n)


def phase_ada(k, li):
    I, S, B, G, P = k.I, k.S, k.B, k.G, k.P
    with ExitStack() as st:
        h = mk_helpers(k, st)
        cv = h.sb("ada_cv", [128, 16])
        sc = h.sb("ada_sc", [128, 16])
        sg = h.sb("ada_sg", [128, 16])
        brow = h.sb("ada_brow", [1, 6 * D])
        rows = [h.sb(f"ada_row{v}", [1, 6 * D]) for v in range(2)]
        wpool = Pool(h.sb, "ada_w", [128, 8, 512], F32, 2)
        pm = [Pool(h.ps, f"ada_pm{v}_", [1, 512], F32, 2) for v in range(2)]
        h.LD("sp", cv, cv[:], I.cvec, G.ld0)
        h.LD("sp", brow, brow[:], I.b_ada[li:li + 1, :], G.ld0)
        h.ACT(sg, sg[:], cv[:], AF.Sigmoid, [cv])
        h.TT("dve", sc, sc[:], cv[:], sg[:], ALU.mult, [cv, sg])
        wv = I.w_ada[li].rearrange("(kc p) n -> p kc n", p=128)
        for j in range(12):
            w = wpool.next()
            h.LD("sp", w, w[:], wv[:, :, j * 512:(j + 1) * 512], G.w0)
            for v in range(2):
                p = pm[v].next()
                for kc in range(8):
                    h.MM(p, p[:], sc[:, v * 8 + kc:v * 8 + kc + 1], w[:, kc, :], [sc, w], start=(kc == 0), stop=(kc == 7))
                h.TT("dve", rows[v], rows[v][:, j * 512:(j + 1) * 512], p[:], brow[:, j * 512:(j + 1) * 512], ALU.add, [p, brow])
        for v in range(2):
            h.ST("sp", S.modv[v:v + 1, :], rows[v], rows[v][:], G.st0, B.modv)
        P.end_phase(f"ada{li}")


def rms_mod_tile(h, k, xt, G_, S_, out_t, out_ap, tmp, stat):
    h.ACT(tmp, tmp[:], xt[:], AF.Square, [xt], accum=stat[:, 0:1], extra_w=[stat])
    h.TS("dve", stat, stat[:, 1:2], stat[:, 0:1], 1.0 / D, EPS, ALU.mult, ALU.add, [stat])
    h.ACT(stat, stat[:, 2:3], stat[:, 1:2], AF.Sqrt, [stat])
    h.RECIP(stat, stat[:, 3:4], stat[:, 2:3], [stat])
    h.STT("dve", tmp, tmp[:], xt[:], stat[:, 3:4], G_[:], ALU.mult, ALU.mult, [xt, stat, G_])
    h.TT("dve", out_t, out_ap, tmp[:], S_[:], ALU.add, [tmp, S_])


def load_mod_tiles(h, k, li, normw, idx_shift, idx_scale, names):
    I, S, B, G = k.I, k.S, k.B, k.G
    nb = h.sb(names + "_nb", [128, D])
    h.LD("sp", nb, nb[:], bcast_row(normw[li]), G.ld0)
    Gs, Ss = [], []
    for v in range(2):
        g = h.sb(f"{names}_G{v}", [128, D])
        s = h.sb(f"{names}_S{v}", [128, D])
        h.LD("sp", g, g[:], bcast_row(S.modv[v, idx_scale * D:(idx_scale + 1) * D]), G.ld0, [B.modv])
        h.LD("sp", s, s[:], bcast_row(S.modv[v, idx_shift * D:(idx_shift + 1) * D]), G.ld0, [B.modv])
        h.STT("dve", g, g[:], g[:], 1.0, nb[:], ALU.add, ALU.mult, [g, nb])
        Gs.append(g)
        Ss.append(s)
    return Gs, Ss


def phase_ab(k, li, x_src):
    I, S, B, G, P = k.I, k.S, k.B, k.G, k.P
    with ExitStack() as st:
        h = mk_helpers(k, st)
        xnT = h.sb("xnT", [128, 8, TALL], BF16)
        identf = h.sb("identf", [128, 128])
        identb = h.sb("identb", [128, 128], BF16)
        h.LD("sp", identf, identf[:], I.ident, G.ld0)
        h.CP("dve", identb, identb[:], identf[:], [identf])
        with ExitStack() as st2:
            h2 = mk_helpers(k, st2)
            Gs, Ss = load_mod_tiles(h2, k, li, I.norm1, 0, 1, "a")
            xpool = Pool(h2.sb, "a_x", [128, D], F32, 2)
            tmp = h2.sb("a_tmp", [128, D])
            xnp = Pool(h2.sb, "a_xn", [128, D], BF16, 2)
            stat = Pool(h2.sb, "a_stat", [128, 4], F32, 2)
            ptr = Pool(h2.ps, "a_ptr", [128, 8, 128], BF16, 2)
            for i in range(NT):
                v = 1 if i < 2 else 0
                xt = xpool.next()
                h2.LD("sp", xt, xt[:], x_src[i * 128:(i + 1) * 128, :], G.ld1, [B.X] if li > 0 else [])
                xn = xnp.next()
                rms_mod_tile(h2, k, xt, Gs[v], Ss[v], xn, xn[:], tmp, stat.next())
                p = ptr.next()
                for kc in range(8):
                    h2.TR(p, p[:, kc, :], xn[:, kc * 128:(kc + 1) * 128], identb[:], [xn, identb])
                h2.CP("act" if i % 2 == 0 else "dve", xnT, xnT[:, :, i * 128:(i + 1) * 128], p[:], [p])
            if S.XNT is not None:
                h2.ST("sp", S.XNT.rearrange("(kc p) t -> p kc t", p=128), xnT, xnT[:], G.st3, P.buf("XNT"))
            P.end_phase(f"a{li}")
        if k.stop_after == ("a", li):
            return
        with ExitStack() as st2:
            h2 = mk_helpers(k, st2)
            cosT = h2.sb("b_cosT", [128, TALL])
            sinT = h2.sb("b_sinT", [128, TALL])
            h2.LD("sp", cosT, cosT[:], I.cosT, G.ld0)
            h2.LD("sp", sinT, sinT[:], I.sinT, G.ld0)
            wpool = Pool(h2.sb, "b_w", [128, 8, 512], BF16, 3)
            stage_bf = Pool(h2.sb, "b_stb", [128, TALL], BF16, 2)
            stage_f = Pool(h2.sb, "b_stf", [128, TALL], F32, 1)
            stage_tm = Pool(h2.sb, "b_sttm", [128, 512], BF16, 3)
            t1p = Pool(h2.sb, "b_t1", [128, 512], F32, 2)
            t2p = Pool(h2.sb, "b_t2", [128, 512], F32, 2)
            c2p = Pool(h2.sb, "b_c2", [128, 512], F32, 2)
            s2p = Pool(h2.sb, "b_s2", [128, 512], F32, 2)
            pp = Pool(h2.ps, "b_p", [128, 512], F32, 6)
            wv = I.w_in[li].rearrange("(kc p) n -> p kc n", p=128)
            wgi = [0]

            def load_w(c0, ncol):
                w = wpool.next()
                g = [G.w0, G.w1, G.w2][wgi[0] % 3]
                wgi[0] += 1
                h2.LD("pool", w, w[:, :, 0:ncol], wv[:, :, c0:c0 + ncol], g)
                return w

            tbs = [(t0, min(512, TALL - t0)) for t0 in range(0, TALL, 512)]
            evi = [0]

            def ev_eng():
                evi[0] += 1
                return "act" if evi[0] % 2 == 0 else "dve"

            def fm_job(c0, ncol, dst, dst_buf, f32=False, swap_c0=None):
                for s0 in range(0, ncol, 512):
                    nc_ = min(512, ncol - s0)
                    w = load_w(c0 + s0, nc_)
                    ws = load_w(swap_c0 + s0, nc_) if swap_c0 is not None else None
                    for sub in range(0, nc_, 128):
                        m = min(128, nc_ - sub)
                        stg = (stage_f if f32 else stage_bf).next()
                        for (t0, tn) in tbs:
                            p = pp.next()
                            for kc in range(8):
                                h2.MM(p, p[0:m, 0:tn], w[:, kc, sub:sub + m], xnT[:, kc, t0:t0 + tn], [w, xnT],
                                      start=(kc == 0), stop=(kc == 7))
                            if ws is None:
                                h2.CP(ev_eng(), stg, stg[0:m, t0:t0 + tn], p[0:m, 0:tn], [p])
                            else:
                                p2 = pp.next()
                                for kc in range(8):
                                    h2.MM(p2, p2[0:m, 0:tn], ws[:, kc, sub:sub + m], xnT[:, kc, t0:t0 + tn], [ws, xnT],
                                          start=(kc == 0), stop=(kc == 7))
                                t1 = t1p.next()
                                t2 = t2p.next()
                                h2.TT("dve", t1, t1[:, 0:tn], p[:, 0:tn], cosT[:, t0:t0 + tn], ALU.mult, [p, cosT])
                                h2.TT("dve", t2, t2[:, 0:tn], p2[:, 0:tn], sinT[:, t0:t0 + tn], ALU.mult, [p2, sinT])
                                h2.TT("dve", stg, stg[:, t0:t0 + tn], t1[:, 0:tn], t2[:, 0:tn], ALU.add, [t1, t2])
                        r0 = s0 + sub
                        h2.ST("sp", dst[r0:r0 + m, :], stg, stg[0:m, :], G.st0, dst_buf)

            def tm_job(c0, ncol, dst, dst_buf, dcol0=0, rope=False):
                for s0 in range(0, ncol, 512):
                    w = load_w(c0 + s0, 512)
                    for i in range(NT):
                        p = pp.next()
                        for kc in range(8):
                            h2.MM(p, p[:], xnT[:, kc, i * 128:(i + 1) * 128], w[:, kc, :], [w, xnT], start=(kc == 0), stop=(kc == 7))
                        stg = stage_tm.next()
                        if not rope:
                            h2.CP(ev_eng(), stg, stg[:], p[:], [p])
                        else:
                            c2 = c2p.next()
                            s2 = s2p.next()
                            h2.LD("sp", c2, c2[:], I.cos2[i * 128:(i + 1) * 128, :], G.ld2)
                            h2.LD("sp", s2, s2[:], I.sin2[i * 128:(i + 1) * 128, :], G.ld2)
                            t1 = t1p.next()
                            t2 = t2p.next()
                            h2.TT("dve", t1, t1[:], p[:], c2[:], ALU.mult, [p, c2])
                            pv = p[:].rearrange("p (h two s) -> p h two s", h=4, two=2)
                            t2v = t2[:].rearrange("p (h two s) -> p h two s", h=4, two=2)
                            s2v = s2[:].rearrange("p (h two s) -> p h two s", h=4, two=2)
                            h2.TT("dve", t2, t2v[:, :, 0, :], pv[:, :, 1, :], s2v[:, :, 0, :], ALU.mult, [p, s2])
                            h2.TT("dve", t2, t2v[:, :, 1, :], pv[:, :, 0, :], s2v[:, :, 1, :], ALU.mult, [p, s2])
                            h2.TT("dve", stg, stg[:], t1[:], t2[:], ALU.add, [t1, t2])
                        h2.ST("sp", dst[i * 128:(i + 1) * 128, dcol0 + s0:dcol0 + s0 + 512], stg, stg[:], G.st1, dst_buf)

            import os
            sel = os.environ.get("BJOBS")
            sel = sel.split(",") if sel else None
            jobs = [
                ("QT", lambda: fm_job(OFF["q"], 512, S.QT, B.QT)),
                ("KT", lambda: fm_job(OFF["k"], 512, S.KT, B.KT)),
                ("LRT", lambda: fm_job(OFF["lrf"], 32, S.LRT, B.LRT)),
                ("Kt", lambda: tm_job(OFF["k"], 512, S.Kt, B.Kt)),
                ("Vt", lambda: tm_job(OFF["v"], 1024, S.Vt, B.Vt)),
                ("RQT", lambda: fm_job(OFF["rq"], 512, S.RQT, B.RQT, swap_c0=OFF["rqs"])),
                ("RKT", lambda: fm_job(OFF["rk"], 512, S.RKT, B.RKT, swap_c0=OFF["rks"])),
                ("RKt", lambda: tm_job(OFF["rk"], 512, S.RKt, B.RKt, rope=True)),
                ("RVt", lambda: tm_job(OFF["rv"], 1024, S.RVt, B.RVt)),
                ("P6T", lambda: fm_job(OFF["p6"], 1024, S.P6T, B.P6T, f32=True)),
                ("P7T", lambda: fm_job(OFF["p7"], 1024, S.P7T, B.P7T)),
                ("P3", lambda: tm_job(OFF["p3"], 1024, S.P3, B.P3)),
                ("P11", lambda: tm_job(OFF["p11"], 1024, S.P11, B.P11)),
                ("P12T", lambda: fm_job(OFF["p12"], 3072, S.P12T, B.P12T)),
            ]
            for jn, jf in jobs:
                if sel is None or jn in sel:
                    jf()
            if S.XNT2 is not None:
                h2.ST("sp", S.XNT2.rearrange("(kc p) t -> p kc t", p=128), xnT, xnT[:], G.st3, P.buf("XNT2"))
            P.end_phase(f"b{li}")


def phase_c1(k, li):
    I, S, B, G, P = k.I, k.S, k.B, k.G, k.P
    SCALE = 128.0 ** -0.5
    with ExitStack() as st:
        h = mk_helpers(k, st)
        tri = [h.sb(f"c_tri{d}", [128, 128]) for d in range(2)]
        maskT = [h.sb(f"c_mask{d}", [128, 512]) for d in range(2)]
        rtab = [[h.sb(f"c_rtab{d}{j}", [128, 512]) for j in range(3)] for d in range(2)]
        rdec = [h.sb(f"c_rdec{d}", [128, 4]) for d in range(2)]
        for d in range(2):
            h.LD("sp", tri[d], tri[d][:], I.tri[d], G.ld0)
            h.LD("sp", maskT[d], maskT[d][:], I.maskT[d], G.ld0)
            h.LD("sp", rdec[d], rdec[d][:], I.rdec[d], G.ld0)
            for j in range(3):
                h.LD("sp", rtab[d][j], rtab[d][j][:], I.rtab[d, j], G.ld0)
        wa2 = h.sb("c_wa2", [16, 2, 512], BF16)
        ba = h.sb("c_ba", [1, 2, 512], BF16)
        ones = h.sb("c_ones", [1, 128], BF16)
        wa2f = h.sb("c_wa2f", [16, 2, 512])
        baf = h.sb("c_baf", [1, 2, 512])
        for d_ in range(2):
            h.LD("sp", wa2f, wa2f[:, d_, :], I.gla_wa2[li, d_], G.w0)
            h.LD("sp", baf, baf[:, d_, :], I.gla_ba[li, d_:d_ + 1, :], G.w0)
        h.CP("dve", wa2, wa2[:], wa2f[:], [wa2f])
        h.CP("dve", ba, ba[:], baf[:], [baf])
        h.MEMSET("dve", ones, ones[:], 1.0)
        Sf = {}
        Sb = {}
        for kind in range(2):
            for d in range(2):
                Sf[kind, d] = h.sb(f"c_S{kind}{d}", [128, 1024])
                Sb[kind, d] = h.sb(f"c_Sb{kind}{d}", [128, 1024], BF16)
                h.MEMSET("dve", Sf[kind, d], Sf[kind, d][:], 0.0)
                h.MEMSET("dve", Sb[kind, d], Sb[kind, d][:], 0.0)
        NB = 3
        qTp = Pool(h.sb, "c_qT", [128, 512], BF16, NB)
        kTp = Pool(h.sb, "c_kT", [128, 512], BF16, NB)
        ktp = Pool(h.sb, "c_kt", [128, 512], BF16, NB)
        vtp = Pool(h.sb, "c_vt", [128, 1024], BF16, NB)
        lrp = Pool(h.sb, "c_lr", [16, 128], BF16, NB)
        e1p = Pool(h.sb, "c_e1", [128, 512], F32, 2)
        spp = Pool(h.sb, "c_sp", [128, 512], F32, 2)
        ektmp = Pool(h.sb, "c_ektm", [128, 512], F32, 2)
        eqtp = Pool(h.sb, "c_eqt", [128, 512], F32, 2)
        ektp = Pool(h.sb, "c_ekt", [128, 512], F32, 2)
        kinvp = Pool(h.sb, "c_kinv", [128, 512], BF16, 2)
        qdecp = Pool(h.sb, "c_qdec", [128, 512], BF16, 2)
        kinvTp = Pool(h.sb, "c_kinvT", [128, 512], BF16, 2)
        scTp = Pool(h.sb, "c_scT", [128, 512], BF16, 2)
        osbp = Pool(h.sb, "c_osb", [128, 1024], F32, 2)
        px = h.ps("c_px", [128, 512])
        pG = h.ps("c_pG", [128, 512])
        pGT = h.ps("c_pGT", [128, 512])
        psc = h.ps("c_psc", [128, 512])
        po = h.ps("c_po", [128, 1024])
        pkv = h.ps("c_pkv", [128, 1024])

        def tile(kind, d, i, cnt):
            QT, KT, Kt, Vt = (S.QT, S.KT, S.Kt, S.Vt) if kind == 0 else (S.RQT, S.RKT, S.RKt, S.RVt)
            bQT, bKT, bKt, bVt = (B.QT, B.KT, B.Kt, B.Vt) if kind == 0 else (B.RQT, B.RKT, B.RKt, B.RVt)
            O = (S.OG if kind == 0 else S.OR)[d]
            bO = getattr(B, ("OG" if kind == 0 else "OR") + ("f" if d == 0 else "b"))
            ts_ = slice(i * 128, (i + 1) * 128)
            qT = qTp.next()
            kT = kTp.next()
            kt = ktp.next()
            vt = vtp.next()
            gl = [G.ld1, G.ld2, G.ld3][cnt % 3]
            h.LD("sp", qT, qT[:].rearrange("p (h c) -> p h c", h=4), QT.rearrange("(h p) t -> p h t", p=128)[:, :, ts_], gl, [bQT])
            h.LD("sp", kT, kT[:].rearrange("p (h c) -> p h c", h=4), KT.rearrange("(h p) t -> p h t", p=128)[:, :, ts_], gl, [bKT])
            h.LD("sp", kt, kt[:], Kt[ts_, :], gl, [bKt])
            h.LD("sp", vt, vt[:], Vt[ts_, :], gl, [bVt])
            if kind == 0:
                lr = lrp.next()
                h.LD("sp", lr, lr[:], S.LRT[d * 16:(d + 1) * 16, ts_], gl, [B.LRT])
                h.MM(px, px[:], lr[:], wa2[:, d, :], [lr, wa2], start=True, stop=False)
                h.MM(px, px[:], ones[:], ba[:, d, :], [ones, ba], start=False, stop=True)
                e1 = e1p.next()
                h.ACT(e1, e1[:], px[:], AF.Exp, [px], scale=-1.0)
                sp = spp.next()
                h.ACT(sp, sp[:], e1[:], AF.Ln, [e1], bias=1.0)
                h.MM(pG, pG[:], tri[d][:], sp[:], [tri[d], sp])
                for hh in range(4):
                    h.MM(pGT, pGT[:, hh * 128:(hh + 1) * 128], sp[:, hh * 128:(hh + 1) * 128], tri[d][:], [tri[d], sp])
                EkTM = ektmp.next()
                h.ACT(EkTM, EkTM[:], pG[:], AF.Exp, [pG], scale=1.0 / 16)
                EqT = eqtp.next()
                h.ACT(EqT, EqT[:], pGT[:], AF.Exp, [pGT], scale=-1.0 / 16)
                EkT = ektp.next()
                h.ACT(EkT, EkT[:], pGT[:], AF.Exp, [pGT], scale=1.0 / 16)
                lastc = 127 if d == 0 else 0
                dec = [EqT[:, hh * 128 + lastc:hh * 128 + lastc + 1] for hh in range(4)]
                dec_t = EqT
            else:
                EqT, EkT, EkTM = rtab[d]
                dec = [rdec[d][:, hh:hh + 1] for hh in range(4)]
                dec_t = rdec[d]
            kinv = kinvp.next()
            h.TT("dve", kinv, kinv[:], kt[:], EkTM[:], ALU.mult, [kt, EkTM])
            qdec = qdecp.next()
            h.STT("dve", qdec, qdec[:], qT[:], SCALE, EqT[:], ALU.mult, ALU.mult, [qT, EqT])
            kinvT = kinvTp.next()
            h.TT("dve", kinvT, kinvT[:], kT[:], EkT[:], ALU.mult, [kT, EkT])
            for hh in range(4):
                hs = slice(hh * 128, (hh + 1) * 128)
                h.MM(psc, psc[:, hs], kinvT[:, hs], qdec[:, hs], [kinvT, qdec])
            scT = scTp.next()
            h.TT("dve", scT, scT[:], psc[:], maskT[d][:], ALU.mult, [psc, maskT[d]])
            sbf = Sb[kind, d]
            sf = Sf[kind, d]
            for hh in range(4):
                hs = slice(hh * 128, (hh + 1) * 128)
                vs = slice(hh * 256, (hh + 1) * 256)
                h.MM(po, po[:, vs], scT[:, hs], vt[:, vs], [scT, vt], start=True, stop=False)
                h.MM(po, po[:, vs], qdec[:, hs], sbf[:, vs], [qdec, sbf], start=False, stop=True)
            osb = osbp.next()
            h.CP("act", osb, osb[:], po[:], [po])
            h.ST("sp", O[ts_, :], osb, osb[:], G.st0 if d == 0 else G.st1, bO)
            for hh in range(4):
                hs = slice(hh * 128, (hh + 1) * 128)
                vs = slice(hh * 256, (hh + 1) * 256)
                h.MM(pkv, pkv[:, vs], kinv[:, hs], vt[:, vs], [kinv, vt])
            h.TT("dve", sf, sf[:], sf[:], pkv[:], ALU.add, [sf, pkv])
            for hh in range(4):
                vs = slice(hh * 256, (hh + 1) * 256)
                h.TS("dve", sf, sf[:, vs], sf[:, vs], dec[hh], None, ALU.mult, None, [sf, dec_t])
            h.CP("act", sbf, sbf[:], sf[:], [sf])

        fwd = list(range(NT))
        bwd = [1, 0] + list(range(NT - 1, 1, -1))
        cnt = 0
        import os
        kinds = [int(x) for x in os.environ.get("C1KINDS", "0,1").split(",")]
        nsteps = int(os.environ.get("C1N", NT))
        for s in range(nsteps):
            for kind in kinds:
                tile(kind, 0, fwd[s], cnt)
                cnt += 1
                tile(kind, 1, bwd[s], cnt)
                cnt += 1
        P.end_phase(f"c1{li}")


def rev_ap(ap2d, c0, n):
    a = ap2d[:, c0:c0 + n]
    return AP(a.tensor, a.offset + (n - 1) * a.ap[-1][0], [list(a.ap[0]), [-a.ap[-1][0], n]])


def phase_c2(k, li):
    I, S, B, G, P = k.I, k.S, k.B, k.G, k.P
    with ExitStack() as st:
        h = mk_helpers(k, st)
        convw = h.sb("l_convw", [128, 8, 5])
        lruw = h.sb("l_w", [128, 32, 256], BF16)
        lruv = h.sb("l_v", [128, 48])
        c8 = h.sb("l_c8", [128, 32])
        tmpv = h.sb("l_tmpv", [128, 16])
        h.LD("sp", convw, convw[:], I.convw[li], G.ld0)
        for q_ in range(8):
            h.LD("pool", lruw, lruw[:, q_ * 4:(q_ + 1) * 4, :], I.lruw[li, :, q_ * 4:(q_ + 1) * 4, :], G.w0)
        h.LD("sp", lruv, lruv[:], I.lruv[li], G.ld0)
        h.ACT(tmpv, tmpv[:], lruv[:, 32:48], AF.Exp, [lruv], scale=-1.0)
        h.ACT(tmpv, tmpv[:], tmpv[:], AF.Ln, [tmpv], bias=1.0)
        h.TS("dve", c8, c8[:, 0:16], tmpv[:], -8.0, None, ALU.mult, None, [tmpv])
        h.TS("dve", c8, c8[:, 16:32], tmpv[:], -16.0, None, ALU.mult, None, [tmpv])
        NPAD = TALL + 8
        xc = [h.sb(f"l_xc{j}", [128, TALL]) for j in range(2)]
        xcb = [h.sb(f"l_xcb{j}", [128, TALL], BF16) for j in range(2)]
        a_t = h.sb("l_a", [128, NPAD])
        b_t = h.sb("l_b", [128, TALL])
        hf = h.sb("l_hf", [128, TALL])
        hb = h.sb("l_hb", [128, TALL])
        p7 = h.sb("l_p7", [128, TALL], BF16)
        gl = a_t
        ob = h.sb("l_ob", [128, TALL], BF16)
        rp = Pool(h.sb, "l_r", [128, 512], F32, 2)
        ip = Pool(h.sb, "l_i", [128, 512], F32, 2)
        a2p = Pool(h.sb, "l_a2", [128, 512], F32, 2)
        pr = Pool(h.ps, "l_pr", [128, 512], F32, 3)
        pi = Pool(h.ps, "l_pi", [128, 512], F32, 3)
        tbs = [(t0, min(512, TALL - t0)) for t0 in range(0, TALL, 512)]
        CO, LO = 2, 261
        import os
        c2stop = int(os.environ.get("C2STOP", 99))
        if c2stop <= 1:
            P.end_phase(f"c2{li}")
            return
        for g in range(4):
            for j in range(2):
                cc = 2 * g + j
                xp = a_t
                h.MEMSET("dve", xp, xp[:, 0:2], 0.0)
                h.MEMSET("dve", xp, xp[:, 258:261], 0.0)
                h.MEMSET("dve", xp, xp[:, NPAD - 3:NPAD], 0.0)
                h.LD("sp", xp, xp[:, CO:CO + NCTX], S.P6T[cc * 128:(cc + 1) * 128, 0:NCTX], G.ld1, [B.P6T])
                h.LD("sp", xp, xp[:, LO:LO + NLAT], S.P6T[cc * 128:(cc + 1) * 128, NCTX:TALL], G.ld1, [B.P6T])
                for (o0, d0, n) in ((CO, 0, NCTX), (LO, NCTX, NLAT)):
                    h.TS("dve", xc[j], xc[j][:, d0:d0 + n], xp[:, o0 - 2:o0 - 2 + n], convw[:, cc, 0:1], convw[:, cc, 4:5],
                         ALU.mult, ALU.add, [xp, convw])
                    for tap in range(1, 4):
                        h.STT("dve", xc[j], xc[j][:, d0:d0 + n], xp[:, o0 - 2 + tap:o0 - 2 + tap + n], convw[:, cc, tap:tap + 1],
                              xc[j][:, d0:d0 + n], ALU.mult, ALU.add, [xp, convw, xc[j]])
                h.CP("act", xcb[j], xcb[j][:], xc[j][:], [xc[j]])
            if c2stop <= 2:
                P.end_phase(f"c2{li}")
                return
            for j in range(2):
                cc = 2 * g + j
                h.LD("sp", p7, p7[:], S.P7T[cc * 128:(cc + 1) * 128, :], G.ld2, [B.P7T])
                for d in range(2):
                    for (t0, tn) in tbs:
                        pr_ = pr.next()
                        pi_ = pi.next()
                        for gate, pt in ((0, pr_), (1, pi_)):
                            for ic in range(2):
                                widx = ((d * 2 + gate) * 4 + g) * 2 + ic
                                h.MM(pt, pt[:, 0:tn], lruw[:, widx, j * 128:(j + 1) * 128], xcb[ic][:, t0:t0 + tn], [lruw, xcb[ic]],
                                     start=(ic == 0), stop=(ic == 1))
                        r = rp.next()
                        ii = ip.next()
                        a2 = a2p.next()
                        vcol = d * 8 + cc
                        h.ACT(r, r[:, 0:tn], pr_[:, 0:tn], AF.Sigmoid, [pr_, lruv], bias=lruv[:, vcol:vcol + 1])
                        h.ACT(ii, ii[:, 0:tn], pi_[:, 0:tn], AF.Sigmoid, [pi_, lruv], bias=lruv[:, 16 + vcol:16 + vcol + 1])
                        h.ACT(a_t, a_t[:, t0:t0 + tn], r[:, 0:tn], AF.Exp, [r, c8], scale=c8[:, vcol:vcol + 1])
                        h.ACT(a2, a2[:, 0:tn], r[:, 0:tn], AF.Exp, [r, c8], scale=c8[:, 16 + vcol:16 + vcol + 1])
                        h.ACT(a2, a2[:, 0:tn], a2[:, 0:tn], AF.Sqrt, [a2], scale=-1.0, bias=1.0)
                        h.TT("dve", ii, ii[:, 0:tn], ii[:, 0:tn], xc[j][:, t0:t0 + tn], ALU.mult, [ii, xc[j]])
                        h.TT("dve", b_t, b_t[:, t0:t0 + tn], ii[:, 0:tn], a2[:, 0:tn], ALU.mult, [ii, a2])
                    if c2stop <= 3:
                        continue
                    if d == 0:
                        P.op("dve", lambda e: e.tensor_tensor_scan(out=hf[:], data0=a_t[:, 0:TALL], data1=b_t[:], initial=0.0,
                                                                   op0=ALU.mult, op1=ALU.add), reads=[a_t, b_t], writes=[hf])
                    else:
                        P.op("dve", lambda e: e.tensor_tensor_scan(out=rev_ap(hb[:], 0, NCTX), data0=rev_ap(a_t[:], 0, NCTX),
                                                                   data1=rev_ap(b_t[:], 0, NCTX), initial=0.0,
                                                                   op0=ALU.mult, op1=ALU.add), reads=[a_t, b_t], writes=[hb])
                        P.op("dve", lambda e: e.tensor_tensor_scan(out=rev_ap(hb[:], NCTX, NLAT), data0=rev_ap(a_t[:], NCTX, NLAT),
                                                                   data1=rev_ap(b_t[:], NCTX, NLAT), initial=hb[:, 0:1],
                                                                   op0=ALU.mult, op1=ALU.add), reads=[a_t, b_t, hb], writes=[hb])
                h.TT("dve", hf, hf[:], hf[:], hb[:], ALU.add, [hf, hb])
                if c2stop <= 4:
                    P.end_phase(f"c2{li}")
                    return
                glv = gl[:, 0:TALL]
                h.TT("dve", gl, glv, p7[:], p7[:], ALU.mult, [p7])
                h.TS("dve", gl, glv, glv, 0.044715, 1.0, ALU.mult, ALU.add, [gl])
                h.TT("dve", gl, glv, glv, p7[:], ALU.mult, [gl, p7])
                h.ACT(gl, glv, glv, AF.Sigmoid, [gl], scale=1.5957691216057308)
                h.TT("dve", gl, glv, glv, p7[:], ALU.mult, [gl, p7])
                h.TT("dve", ob, ob[:], glv, hf[:], ALU.mult, [gl, hf])
                h.ST("sp", S.LRUT[cc * 128:(cc + 1) * 128, :], ob, ob[:], G.st0, B.LRUT)
        P.end_phase(f"c2{li}")


def phase_d1(k, li, last):
    I, S, B, G, P = k.I, k.S, k.B, k.G, k.P
    t_first = 2 if last else 0
    with ExitStack() as st:
        h = mk_helpers(k, st)
        identf = h.sb("d_identf", [128, 128])
        identb = h.sb("d_identb", [128, 128], BF16)
        h.LD("sp", identf, identf[:], I.ident, G.ld0)
        h.CP("dve", identb, identb[:], identf[:], [identf])
        gn = h.sb("d_gn", [128, D])
        rn = h.sb("d_rn", [128, D])
        h.LD("sp", gn, gn[:], bcast_row(I.gla_norm[li]), G.ld0)
        h.LD("sp", rn, rn[:], bcast_row(I.ret_norm[li]), G.ld0)
        oa = Pool(h.sb, "d_oa", [128, D], F32, 3)
        obp = Pool(h.sb, "d_ob", [128, D], F32, 3)
        gp = Pool(h.sb, "d_g", [128, D], BF16, 3)
        sqp = Pool(h.sb, "d_sq", [128, D], F32, 2)
        stat = Pool(h.sb, "d_stat", [128, 16], F32, 3)
        nb = Pool(h.sb, "d_nb", [128, D], BF16, 2)
        oT = Pool(h.sb, "d_oT", [128, 8, 128], BF16, 3)
        ptr = Pool(h.ps, "d_ptr", [128, 8, 128], BF16, 3)

        def headnorm(o, gate_src, bsrc, normw, center, i, dst, bdst):
            stt = stat.next()
            sq = sqp.next()
            ov = o[:].rearrange("p (h e) -> p h e", h=4)
            if center:
                P.op("dve", lambda e: e.tensor_reduce(out=stt[:, 0:4], in_=ov, axis=AX.X, op=ALU.add), reads=[o], writes=[stt])
                h.TS("dve", stt, stt[:, 0:4], stt[:, 0:4], -1.0 / 256, None, ALU.mult, None, [stt])
                for hh in range(4):
                    h.TS("dve", o, o[:, hh * 256:(hh + 1) * 256], o[:, hh * 256:(hh + 1) * 256], stt[:, hh:hh + 1], None, ALU.add, None, [o, stt])
            h.TT("dve", sq, sq[:], o[:], o[:], ALU.mult, [o])
            P.op("dve", lambda e: e.tensor_reduce(out=stt[:, 4:8], in_=sq[:].rearrange("p (h e) -> p h e", h=4), axis=AX.X, op=ALU.add),
                 reads=[sq], writes=[stt])
            h.TS("dve", stt, stt[:, 8:12], stt[:, 4:8], 1.0 / 256, EPS, ALU.mult, ALU.add, [stt])
            h.ACT(stt, stt[:, 8:12], stt[:, 8:12], AF.Sqrt, [stt])
            h.RECIP(stt, stt[:, 12:16], stt[:, 8:12], [stt])
            g = gp.next()
            h.LD("sp", g, g[:], gate_src[i * 128:(i + 1) * 128, :], G.ld2, [bsrc])
            h.ACT(sq, sq[:], g[:], AF.Sigmoid, [g])
            h.TT("dve", sq, sq[:], sq[:], g[:], ALU.mult, [sq, g])
            h.TT("dve", sq, sq[:], sq[:], normw[:], ALU.mult, [sq, normw])
            n_ = nb.next()
            for hh in range(4):
                vs = slice(hh * 256, (hh + 1) * 256)
                h.STT("dve", n_, n_[:, vs], o[:, vs], stt[:, 12 + hh:13 + hh], sq[:, vs], ALU.mult, ALU.mult, [o, stt, sq])
            p = ptr.next()
            for kc in range(8):
                h.TR(p, p[:, kc, :], n_[:, kc * 128:(kc + 1) * 128], identb[:], [n_, identb])
            t_ = oT.next()
            h.CP("act", t_, t_[:], p[:], [p])
            h.ST("sp", dst.rearrange("(kc p) t -> p kc t", p=128)[:, :, i * 128:(i + 1) * 128], t_, t_[:], G.st1, bdst)

        for i in range(t_first, NT):
            o1 = oa.next()
            o2 = obp.next()
            h.LD("sp", o1, o1[:], S.OG[0][i * 128:(i + 1) * 128, :], G.ld1, [B.OGf])
            h.LD("sp", o2, o2[:], S.OG[1][i * 128:(i + 1) * 128, :], G.ld1, [B.OGb])
            h.TT("dve", o1, o1[:], o1[:], o2[:], ALU.add, [o1, o2])
            headnorm(o1, S.P3, B.P3, gn, False, i, S.GLAT, B.GLAT)
            o1 = oa.next()
            o2 = obp.next()
            h.LD("sp", o1, o1[:], S.OR[0][i * 128:(i + 1) * 128, :], G.ld3, [B.ORf])
            h.LD("sp", o2, o2[:], S.OR[1][i * 128:(i + 1) * 128, :], G.ld3, [B.ORb])
            h.TT("dve", o1, o1[:], o1[:], o2[:], ALU.add, [o1, o2])
            headnorm(o1, S.P11, B.P11, rn, True, i, S.RETT, B.RETT)
        P.end_phase(f"d1{li}")


def phase_d2(k, li, x_src, last):
    I, S, B, G, P = k.I, k.S, k.B, k.G, k.P
    t_first = 2 if last else 0
    with ExitStack() as st:
        h = mk_helpers(k, st)
        identf = h.sb("e_identf", [128, 128])
        identb = h.sb("e_identb", [128, 128], BF16)
        h.LD("sp", identf, identf[:], I.ident, G.ld0)
        h.CP("dve", identb, identb[:], identf[:], [identf])
        wbr = [h.sb(f"e_wbr{j}", [128, 8, D], BF16) for j in range(3)]
        wout = h.sb("e_wout", [128, 8, D], BF16)
        for j in range(3):
            h.LD("pool", wbr[j], wbr[j][:], I.w_branch[li, j].rearrange("(kc p) n -> p kc n", p=128), G.w0)
        h.LD("pool", wout, wout[:], I.w_out[li].rearrange("(kc p) n -> p kc n", p=128), G.w0)
        wr = h.sb("e_wr", [128, 8, 32])
        h.LD("sp", wr, wr[:], I.w_router[li].rearrange("(kc p) n -> p kc n", p=128), G.ld0)
        wrh = h.sb("e_wrh", [128, 8, 32], BF16)
        wrl = h.sb("e_wrl", [128, 8, 32], BF16)
        h.CP("dve", wrh, wrh[:], wr[:], [wr])
        h.TT("dve", wr, wr[:], wr[:], wrh[:], ALU.subtract, [wr, wrh])
        h.CP("dve", wrl, wrl[:], wr[:], [wr])
        brb = h.sb("e_brb", [128, 32])
        h.LD("sp", brb, brb[:], bcast_row(I.b_router[li]), G.ld0)
        G2, S2 = load_mod_tiles(h, k, li, I.norm2, 3, 4, "e")
        gate1 = []
        for v in range(2):
            g = h.sb(f"e_gate{v}", [128, D])
            h.LD("sp", g, g[:], bcast_row(S.modv[v, 2 * D:3 * D]), G.ld0, [B.modv])
            gate1.append(g)
        srcT = [Pool(h.sb, f"e_src{j}_", [128, 8, 512], BF16, 1) for j in range(3)]
        p12p = Pool(h.sb, "e_p12", [128, 3, 512], BF16, 2)
        sg = Pool(h.sb, "e_sg", [128, 512], F32, 3)
        mrg = Pool(h.sb, "e_mrg", [128, 512], F32, 2)
        mT = h.sb("e_mT", [128, 8, 512], BF16)
        stat = Pool(h.sb, "e_stat", [128, 16], F32, 3)
        xp = Pool(h.sb, "e_x", [128, D], F32, 2)
        tmp = h.sb("e_tmp", [128, D])
        xn2 = h.sb("e_xn2", [128, D])
        xh = h.sb("e_xh", [128, D], BF16)
        xl = h.sb("e_xl", [128, D], BF16)
        xn2Tb = Pool(h.sb, "e_xn2Tb", [128, 8, 128], BF16, 2)
        xlTp = Pool(h.sb, "e_xlT", [128, 8, 128], BF16, 2)
        lg = Pool(h.sb, "e_lg", [128, 32], F32, 2)
        m8 = Pool(h.sb, "e_m8", [128, 8], F32, 2)
        wro = Pool(h.sb, "e_wro", [128, 64], F32, 2)
        ptr = Pool(h.ps, "e_ptr", [128, 8, 128], BF16, 2)
        plg = h.ps("e_plg", [128, 512])
        pbr = Pool(h.ps, "e_pbr", [128, 512], F32, 3)
        pout = h.ps("e_pout", [128, D])
        srcD = [(S.GLAT, B.GLAT), (S.LRUT, B.LRUT), (S.RETT, B.RETT)]
        groups = []
        t = t_first
        while t < NT:
            n = min(4, NT - t)
            groups.append((t, n))
            t += n
        import os
        d2stop = float(os.environ.get("D2STOP", 99))
        if d2stop <= 1:
            P.end_phase(f"d2{li}")
            return
        for (g0, gn_) in groups:
            ntok = gn_ * 128
            c0 = g0 * 128
            srcs = []
            for j in range(3):
                t_ = srcT[j].next()
                h.LD("sp", t_, t_[:, :, 0:ntok], srcD[j][0].rearrange("(kc p) t -> p kc t", p=128)[:, :, c0:c0 + ntok], G.ld3, [srcD[j][1]])
                srcs.append(t_)
            for dc in range(8):
                m = mrg.next()
                p12 = p12p.next()
                h.LD("sp", p12, p12[:, :, 0:ntok], S.P12T.rearrange("(j r) t -> r j t", j=3)[dc * 128:(dc + 1) * 128, :, c0:c0 + ntok], G.ld2, [B.P12T])
                for j in range(3):
                    pb = pbr.next()
                    for kc in range(8):
                        h.MM(pb, pb[:, 0:ntok], wbr[j][:, kc, dc * 128:(dc + 1) * 128], srcs[j][:, kc, 0:ntok], [wbr[j], srcs[j]],
                             start=(kc == 0), stop=(kc == 7))
                    s_ = sg.next()
                    h.ACT(s_, s_[:, 0:ntok], p12[:, j, 0:ntok], AF.Sigmoid, [p12])
                    if j == 0:
                        h.TT("dve", m, m[:, 0:ntok], pb[:, 0:ntok], s_[:, 0:ntok], ALU.mult, [pb, s_])
                    else:
                        h.TT("dve", s_, s_[:, 0:ntok], pb[:, 0:ntok], s_[:, 0:ntok], ALU.mult, [pb, s_])
                        if j == 1:
                            h.TT("dve", m, m[:, 0:ntok], m[:, 0:ntok], s_[:, 0:ntok], ALU.add, [m, s_])
                        else:
                            h.TT("dve", mT, mT[:, dc, 0:ntok], m[:, 0:ntok], s_[:, 0:ntok], ALU.add, [m, s_])
            if d2stop <= 2:
                P.end_phase(f"d2{li}")
                return
            for tl in range(gn_):
                i = g0 + tl
                v = 1 if i < 2 else 0
                for half in range(2):
                    for kc in range(8):
                        h.MM(pout, pout[:, half * 512:(half + 1) * 512], mT[:, kc, tl * 128:(tl + 1) * 128],
                             wout[:, kc, half * 512:(half + 1) * 512], [mT, wout], start=(kc == 0), stop=(kc == 7))
                xt = xp.next()
                h.LD("sp", xt, xt[:], x_src[i * 128:(i + 1) * 128, :], G.ld1, [B.X] if li > 0 else [])
                h.TT("dve", tmp, tmp[:], pout[:], gate1[v][:], ALU.mult, [pout, gate1[v]])
                h.TT("dve", xt, xt[:], xt[:], tmp[:], ALU.add, [xt, tmp])
                h.ST("sp", S.X[i * 128:(i + 1) * 128, :], xt, xt[:], G.st0, B.X)
                if d2stop <= 3:
                    P.end_phase(f"d2{li}")
                    return
                rms_mod_tile(h, k, xt, G2[v], S2[v], xn2, xn2[:], tmp, stat.next())
                if d2stop <= 3.2:
                    P.end_phase(f"d2{li}")
                    return
                xb = xn2Tb.next()
                xlT = xlTp.next()
                h.CP("act", xh, xh[:], xn2[:], [xn2])
                h.TT("dve", xl, xl[:], xn2[:], xh[:], ALU.subtract, [xn2, xh])
                for src_, dst_ in ((xh, xb), (xl, xlT)):
                    p = ptr.next()
                    for kc in range(8):
                        h.TR(p, p[:, kc, :], src_[:, kc * 128:(kc + 1) * 128], identb[:], [src_, identb])
                    h.CP("act" if src_ is xh else "dve", dst_, dst_[:], p[:], [p])
                if d2stop <= 3.5:
                    P.end_phase(f"d2{li}")
                    return
                h.ST("sp", S.XN2T.rearrange("(kc p) t -> p kc t", p=128)[:, :, i * 128:(i + 1) * 128], xb, xb[:], G.st1, B.XN2T)
                if d2stop <= 4:
                    P.end_phase(f"d2{li}")
                    return
                nmm = 0
                for (a_, w_t) in ((xb, wrh), (xlT, wrh), (xb, wrl)):
                    for kc in range(8):
                        h.MM(plg, plg[:, 0:32], a_[:, kc, :], w_t[:, kc, :], [a_, w_t], start=(nmm == 0), stop=(nmm == 23))
                        nmm += 1
                l_ = lg.next()
                h.TT("dve", l_, l_[:], plg[:, 0:32], brb[:], ALU.add, [plg, brb])
                m_ = m8.next()
                P.op("dve", lambda e, m_=m_, l_=l_: e.max(out=m_[:], in_=l_[:]), reads=[l_], writes=[m_])
                w_ = wro.next()
                stt = stat.next()
                h.TS("dve", w_, w_[:, 32:64], l_[:], m_[:, 3:4], None, ALU.is_ge, None, [l_, m_])
                h.TS("dve", stt, stt[:, 0:1], m_[:, 0:1], -1.0, None, ALU.mult, None, [m_])
                h.ACT(l_, l_[:], l_[:], AF.Exp, [l_, stt], bias=stt[:, 0:1])
                h.TT("dve", l_, l_[:], l_[:], w_[:, 32:64], ALU.mult, [l_, w_])
                P.op("dve", lambda e, stt=stt, l_=l_: e.tensor_reduce(out=stt[:, 1:2], in_=l_[:], axis=AX.X, op=ALU.add), reads=[l_], writes=[stt])
                h.RECIP(stt, stt[:, 2:3], stt[:, 1:2], [stt])
                h.TS("dve", w_, w_[:, 0:32], l_[:], stt[:, 2:3], None, ALU.mult, None, [l_, stt])
                h.ST("sp", S.WR[i * 128:(i + 1) * 128, :], w_, w_[:], G.st2, B.WR)
        P.end_phase(f"d2{li}")


def phase_f(k, li, last):
    I, S, B, G, P = k.I, k.S, k.B, k.G, k.P
    out = k.out
    t_first = 2 if last else 0
    tiles = list(range(t_first, NT))
    if last:
        groups = [tiles[j:j + 8] for j in range(0, 32, 8)]
    else:
        groups = [tiles[0:9], tiles[9:18], tiles[18:26], tiles[26:34]]
    with ExitStack() as st:
        h = mk_helpers(k, st)
        ones = h.sb("f_ones", [1, 128], BF16)
        h.MEMSET("dve", ones, ones[:], 1.0)
        bgu = h.sb("f_bgu", [128, 32 * 16])
        h.LD("sp", bgu, bgu[:], I.bgu[li], G.ld0)
        bdf = Pool(h.sb, "f_bdf", [1, D], F32, 2)
        bdb = Pool(h.sb, "f_bdb", [1, D], BF16, 2)
        gate2 = []
        for v in range(2):
            g = h.sb(f"f_gate{v}", [128, D])
            h.LD("sp", g, g[:], bcast_row(S.modv[v, 5 * D:6 * D]), G.ld0, [B.modv])
            gate2.append(g)
        fnb = None
        if last:
            fnb = h.sb("f_fnb", [128, D])
            h.LD("sp", fnb, fnb[:], bcast_row(I.final_norm), G.ld0)
        wgu = Pool(h.sb, "f_wgu", [128, 8, 1024], BF16, 3)
        wdn = Pool(h.sb, "f_wdn", [128, 4, D], BF16, 3)
        xT = h.sb("f_xT", [128, 8, 9 * 128], BF16)
        acc = h.sb("f_acc", [128, 9, D])
        wr = h.sb("f_wr", [128, 9, 64])
        gcp = Pool(h.sb, "f_gc", [128, 512], F32, 2)
        ucp = Pool(h.sb, "f_uc", [128, 512], F32, 2)
        sip = Pool(h.sb, "f_si", [128, 512], F32, 2)
        actT = Pool(h.sb, "f_actT", [128, 4, 512], BF16, 2)
        xo = Pool(h.sb, "f_xo", [128, D], F32, 2)
        tmp = h.sb("f_tmp", [128, D])
        stat = Pool(h.sb, "f_stat", [128, 4], F32, 2)
        pg = Pool(h.ps, "f_pg", [128, 512], F32, 2)
        pu = Pool(h.ps, "f_pu", [128, 512], F32, 2)
        pd = Pool(h.ps, "f_pd", [128, D], F32, 2)
        wgi = [0]
        for grp in groups:
            ng = len(grp)
            c0 = grp[0] * 128
            ntok = ng * 128
            h.LD("sp", xT, xT[:, :, 0:ntok], S.XN2T.rearrange("(kc p) t -> p kc t", p=128)[:, :, c0:c0 + ntok], G.ld1, [B.XN2T])
            h.LD("sp", wr, wr[:, 0:ng, :], S.WR[c0:c0 + ntok, :].rearrange("(n p) e -> p n e", p=128), G.ld1, [B.WR])
            h.MEMSET("dve", acc, acc[:, 0:ng, :], 0.0)
            subs = [(s0, min(4, ng - s0)) for s0 in range(0, ng, 4)]
            for e in range(32):
                bf_ = bdf.next()
                bb_ = bdb.next()
                h.LD("sp", bf_, bf_[:], I.b_down[li, e:e + 1, :], G.ld0)
                h.CP("dve", bb_, bb_[:], bf_[:], [bf_])
                for hf_ in range(2):
                    wg_ = wgu.next()
                    wd_ = wdn.next()
                    g1 = [G.w0, G.w1, G.w2][wgi[0] % 3]
                    wgi[0] += 1
                    src = I.w_gu[li, e].rearrange("(kc p) n -> p kc n", p=128)
                    h.LD("pool", wg_, wg_[:, :, 0:512], src[:, :, hf_ * 512:(hf_ + 1) * 512], g1)
                    h.LD("pool", wg_, wg_[:, :, 512:1024], src[:, :, 1024 + hf_ * 512:1024 + (hf_ + 1) * 512], g1)
                    h.LD("pool", wd_, wd_[:], I.w_down[li, e, hf_ * 512:(hf_ + 1) * 512, :].rearrange("(fc p) n -> p fc n", p=128), g1)
                    for (s0, sn) in subs:
                        tn = sn * 128
                        tc0 = s0 * 128
                        aT = actT.next()
                        for fl in range(4):
                            fc = hf_ * 4 + fl
                            pg_ = pg.next()
                            pu_ = pu.next()
                            for kc in range(8):
                                h.MM(pg_, pg_[:, 0:tn], wg_[:, kc, fl * 128:(fl + 1) * 128], xT[:, kc, tc0:tc0 + tn], [wg_, xT],
                                     start=(kc == 0), stop=(kc == 7))
                            for kc in range(8):
                                h.MM(pu_, pu_[:, 0:tn], wg_[:, kc, 512 + fl * 128:512 + (fl + 1) * 128], xT[:, kc, tc0:tc0 + tn], [wg_, xT],
                                     start=(kc == 0), stop=(kc == 7))
                            gc = gcp.next()
                            uc = ucp.next()
                            si = sip.next()
                            bcol = e * 16 + fc
                            h.TS("dve", gc, gc[:, 0:tn], pg_[:, 0:tn], bgu[:, bcol:bcol + 1], 7.0, ALU.add, ALU.min, [pg_, bgu])
                            h.ACT(uc, uc[:, 0:tn], pu_[:, 0:tn], AF.Identity, [pu_, bgu], bias=bgu[:, bcol + 8:bcol + 9])
                            h.ACT(si, si[:, 0:tn], gc[:, 0:tn], AF.Sigmoid, [gc], scale=1.702)
                            h.TS("dve", uc, uc[:, 0:tn], uc[:, 0:tn], 7.0, -7.0, ALU.min, ALU.max, [uc])
                            h.TT("dve", gc, gc[:, 0:tn], gc[:, 0:tn], si[:, 0:tn], ALU.mult, [gc, si])
                            h.STT("dve", aT, aT[:, fl, 0:tn], uc[:, 0:tn], 1.0, gc[:, 0:tn], ALU.add, ALU.mult, [uc, gc])
                        for tl in range(sn):
                            ti = s0 + tl
                            pd_ = pd.next()
                            for half in range(2):
                                if hf_ == 0:
                                    h.MM(pd_, pd_[:, half * 512:(half + 1) * 512], ones[:], bb_[:, half * 512:(half + 1) * 512],
                                         [ones, bb_], start=True, stop=False)
                                for fl in range(4):
                                    h.MM(pd_, pd_[:, half * 512:(half + 1) * 512], aT[:, fl, tl * 128:(tl + 1) * 128],
                                         wd_[:, fl, half * 512:(half + 1) * 512], [aT, wd_], start=(fl == 0 and hf_ == 1), stop=(fl == 3))
                            h.STT("dve", acc, acc[:, ti, :], pd_[:], wr[:, ti, e:e + 1], acc[:, ti, :], ALU.mult, ALU.add, [pd_, wr, acc])
            for tl in range(ng):
                i = grp[tl]
                v = 1 if i < 2 else 0
                xt = xo.next()
                h.LD("sp", xt, xt[:], S.X[i * 128:(i + 1) * 128, :], G.ld2, [B.X])
                h.TT("dve", tmp, tmp[:], acc[:, tl, :], gate2[v][:], ALU.mult, [acc, gate2[v]])
                h.TT("dve", xt, xt[:], xt[:], tmp[:], ALU.add, [xt, tmp])
                if not last:
                    h.ST("sp", S.X[i * 128:(i + 1) * 128, :], xt, xt[:], G.st0, B.X)
                else:
                    stt = stat.next()
                    h.ACT(tmp, tmp[:], xt[:], AF.Square, [xt], accum=stt[:, 0:1], extra_w=[stt])
                    h.TS("dve", stt, stt[:, 1:2], stt[:, 0:1], 1.0 / D, EPS, ALU.mult, ALU.add, [stt])
                    h.ACT(stt, stt[:, 2:3], stt[:, 1:2], AF.Sqrt, [stt])
                    h.RECIP(stt, stt[:, 3:4], stt[:, 2:3], [stt])
                    h.STT("dve", xt, xt[:], xt[:], stt[:, 3:4], fnb[:], ALU.mult, ALU.mult, [xt, stt, fnb])
                    h.ST("sp", out[(i - 2) * 128:(i - 1) * 128, :], xt, xt[:], G.out, B.out)
        P.end_phase(f"f{li}")


def host_constants():
    f32 = np.float32
    c = {}
    c["ident"] = np.eye(128, dtype=f32)
    s = np.arange(128)[:, None]
    cc = np.arange(128)[None, :]
    c["tri"] = np.stack([(s <= cc), (s >= cc)]).astype(f32)
    c["maskT"] = np.stack([np.tile((s <= cc), (1, 4)), np.tile((s > cc), (1, 4))]).astype(f32)
    rows = NLAT // 64
    row = np.repeat(np.arange(rows), 64).astype(np.float64)
    col = np.tile(np.arange(64), rows).astype(np.float64)
    nf = 32
    inv = (10000.0 ** (-np.arange(nf, dtype=np.float32) / np.float32(nf))).astype(np.float32)
    ang = np.concatenate([row[:, None].astype(f32) * inv, col[:, None].astype(f32) * inv], axis=-1).astype(f32)
    cos = np.concatenate([np.ones((NCTX, 64), f32), np.cos(ang).astype(f32)], 0)
    sin = np.concatenate([np.zeros((NCTX, 64), f32), np.sin(ang).astype(f32)], 0)
    cos128 = np.concatenate([cos, cos], 1)
    sin128 = np.concatenate([-sin, sin], 1)
    c["cosT"] = np.ascontiguousarray(cos128.T)
    c["sinT"] = np.ascontiguousarray(sin128.T)
    c["cos2"] = np.ascontiguousarray(np.tile(cos128, (1, 4)))
    c["sin2"] = np.ascontiguousarray(np.tile(sin128, (1, 4)))
    gamma = 1.0 - 2.0 ** (-5.0 - np.arange(4, dtype=np.float64))
    lg = np.log(gamma)
    pos = np.arange(128, dtype=np.float64)
    rtab = np.zeros((2, 3, 128, 512), f32)
    rdec = np.zeros((2, 128, 4), f32)
    for d in range(2):
        steps = (pos + 1) if d == 0 else (128 - pos)
        for hh in range(4):
            G_ = steps * lg[hh]
            rtab[d, 0, :, hh * 128:(hh + 1) * 128] = np.exp(G_)[None, :]
            rtab[d, 1, :, hh * 128:(hh + 1) * 128] = np.exp(-G_)[None, :]
            rtab[d, 2, :, hh * 128:(hh + 1) * 128] = np.exp(-G_)[:, None]
            rdec[d, :, hh] = np.exp(128 * lg[hh])
    c["rtab"] = rtab
    c["rdec"] = rdec
    return c


def host_weights(inp):
    f32 = np.float32
    w = {}
    w_in = inp["w_in"]
    rq = w_in[:, :, OFF["rq"]:OFF["rq"] + 512].reshape(2, D, 4, 2, 64)[:, :, :, ::-1, :].reshape(2, D, 512)
    rk = w_in[:, :, OFF["rk"]:OFF["rk"] + 512].reshape(2, D, 4, 2, 64)[:, :, :, ::-1, :].reshape(2, D, 512)
    w["w_in_p"] = np.ascontiguousarray(np.concatenate([w_in, rq, rk], axis=2))
    for n in ["w_ada", "b_ada", "norm1", "norm2", "final_norm", "gla_wa2", "gla_ba", "gla_norm", "ret_norm", "w_branch",
              "w_out", "w_router", "b_router", "w_down", "b_down"]:
        w[n] = np.ascontiguousarray(inp[n])
    cw = np.concatenate([inp["lru_conv_w"], inp["lru_conv_b"][:, None, :]], axis=1)
    w["convw"] = np.ascontiguousarray(cw.reshape(2, 5, 8, 128).transpose(0, 3, 2, 1))
    lw = np.stack([inp["lru_wa"], inp["lru_wi"]], axis=2)
    lw = lw.reshape(2, 2, 2, 4, 2, 128, 256).transpose(0, 5, 1, 2, 3, 4, 6)
    w["lruw"] = np.ascontiguousarray(lw.reshape(2, 128, 32, 256))
    lv = np.stack([inp["lru_ba"], inp["lru_bi"], inp["lru_lam"]], axis=1)
    lv = lv.reshape(2, 3, 2, 8, 128).transpose(0, 4, 1, 2, 3)
    w["lruv"] = np.ascontiguousarray(lv.reshape(2, 128, 48))
    wg = inp["w_gu"].reshape(2, 32, D, 1024, 2).transpose(0, 1, 2, 4, 3)
    w["w_gu_d"] = np.ascontiguousarray(wg.reshape(2, 32, D, 2048))
    bg = inp["b_gu"].reshape(2, 32, 8, 128, 2).transpose(0, 3, 1, 4, 2)
    w["bgu"] = np.ascontiguousarray(bg.reshape(2, 128, 32 * 16))
    return w


_CACHE = {}


def kernel(**inputs):
    inp = {k_: np.asarray(v) for k_, v in inputs.items()}
    if "prog" not in _CACHE:
        _CACHE["prog"] = build_program()
    nc, k = _CACHE["prog"]
    consts = host_constants()
    wts = host_weights(inp)
    in_maps = []
    for b in range(8):
        m = dict(consts)
        m.update(wts)
        m["xall"] = np.ascontiguousarray(np.concatenate([inp["ctx"][b], inp["x"][b]], axis=0))
        cv = np.concatenate([inp["c"][b].reshape(8, 128).T, inp["c_ctx"].reshape(8, 128).T], axis=1)
        m["cvec"] = np.ascontiguousarray(cv.astype(np.float32))
        in_maps.append(m)
    res = run_bass_kernel_spmd(nc, in_maps, core_ids=list(range(8)))
    return np.stack([np.asarray(r["out"]) for r in res.results], axis=0).astype(np.float32)
```

```python
import numpy as np
import ml_dtypes
from contextlib import ExitStack
import concourse.bass as bass
import concourse.mybir as mybir
from concourse.ap import AP
from concourse.bass_utils import run_bass_kernel_spmd

F32 = mybir.dt.float32
BF16 = mybir.dt.bfloat16
AF = mybir.ActivationFunctionType
ALU = mybir.AluOpType
AX = mybir.AxisListType

SEM_EPOCH = 30000
D = 1024
NCTX = 256
NLAT = 4096
TALL = NCTX + NLAT
NT = TALL // 128
EPS = 1e-6
NCOLP = 11296 + 1024
OFF = dict(q=0, k=512, v=1024, p3=2048, lrf=3072, lrb=3088, p6=3104, p7=4128, rq=5152, rk=5664, rv=6176,
           p11=7200, p12=8224, rqs=11296, rks=11808)


class Buf:
    __slots__ = ("name", "last_w", "readers")

    def __init__(self, name):
        self.name = name
        self.last_w = None
        self.readers = []


class DmaGroup:
    def __init__(self, sem, name):
        self.sem = sem
        self.count = 0
        self.name = name


class Op:
    __slots__ = ("eng", "fn", "waits", "signal", "idx", "semval", "dma_group")

    def __init__(self, eng, fn, idx):
        self.eng = eng
        self.fn = fn
        self.idx = idx
        self.waits = []
        self.signal = False
        self.semval = None
        self.dma_group = None


class TT_:
    def __init__(self, h, name):
        self.h = h
        self.b = Buf(name)
        self.dsem = None

    def __getitem__(self, k):
        return self.h[k]


class Prog:
    ENGS = ("pe", "act", "dve", "pool", "sp")

    def __init__(self, nc, gstack):
        self.nc = nc
        self.gstack = gstack
        self.base = {e: 0 for e in self.ENGS}
        self.ops = {e: [] for e in self.ENGS}
        self.waited_ops = {e: {x: -1 for x in self.ENGS} for e in self.ENGS}
        self.waited_dma = {e: {} for e in self.ENGS}
        self.groups = []
        self.nsem = 0
        self.cur_sem = {e: None for e in self.ENGS}
        self.cur_cnt = {e: 0 for e in self.ENGS}
        self.bufs = []
        self.ninst = 0
        self.free_dsems = {}
        self.used_dsems = []
        self.gen = 0
        self.eng_sems = {}

    def tile_sem(self, t, queue="sp"):
        if t.dsem is None or getattr(t, "dsem_gen", -1) != self.gen:
            t.dsem_gen = self.gen
            fl = self.free_dsems.setdefault(queue, [])
            if fl:
                t.dsem = fl.pop()
            else:
                t.dsem = DmaGroup(self.new_sem(f"d{self.nsem}"), f"d{self.nsem}")
                t.dsem.queue = queue
            self.used_dsems.append(t.dsem)
        assert t.dsem.queue == queue, "tile DMA'd from two queue types"
        return t.dsem

    def new_sem(self, name):
        self.nsem += 1
        return self.gstack.enter_context(self.nc.semaphore(name))

    def group(self, name):
        g = DmaGroup(self.new_sem("g_" + name), name)
        self.groups.append(g)
        return g

    def buf(self, name):
        b = Buf(name)
        self.bufs.append(b)
        return b

    def _add_dep(self, op, tok):
        if tok is None:
            return
        E = op.eng
        if tok[0] == "op":
            x = tok[1]
            if x.eng == E and E == "pe":
                return
            if self.waited_ops[E][x.eng] >= x.idx:
                return
            self.waited_ops[E][x.eng] = x.idx
            x.signal = True
            op.waits.append(tok)
        else:
            _, g, cnt, gen = tok
            if gen != self.gen:
                return
            if self.waited_dma[E].get(g, 0) >= cnt:
                return
            self.waited_dma[E][g] = cnt
            op.waits.append(tok)

    def _record(self, eng, fn, reads, writes, dma_group=None):
        op = Op(eng, fn, self.base[eng] + len(self.ops[eng]))
        self.ops[eng].append(op)
        for b in reads:
            self._add_dep(op, b.last_w)
        for b in writes:
            self._add_dep(op, b.last_w)
            for r in b.readers:
                self._add_dep(op, r)
        if dma_group is not None:
            dma_group.count += 1
            op.dma_group = dma_group
            tok = ("dma", dma_group, dma_group.count, self.gen)
        else:
            tok = ("op", op)
        for b in writes:
            b.last_w = tok
            b.readers = []
        for b in reads:
            if b not in writes:
                b.readers.append(tok)
        return op

    def op(self, eng, fn, reads=(), writes=()):
        rd = [r.b if isinstance(r, TT_) else r for r in reads if not getattr(r, "is_psum", False)]
        wr = [w.b if isinstance(w, TT_) else w for w in writes]
        wr += [r.b for r in reads if getattr(r, "is_psum", False) and r.b not in wr]
        return self._record(eng, fn, rd, wr)

    def dma(self, queue, out, in_, tile, reads=(), writes=(), **kw):
        def fn(e):
            return e.dma_start(out=out, in_=in_, **kw)
        return self._record(queue, fn, [r.b if isinstance(r, TT_) else r for r in reads],
                            [w.b if isinstance(w, TT_) else w for w in writes], dma_group=self.tile_sem(tile, queue))

    def _simulate(self, name):
        if not hasattr(self, "simvals"):
            self.simvals = {}
        vals = self.simvals
        pc = {e: 0 for e in self.ENGS}
        progress = True
        while progress:
            progress = False
            for e in self.ENGS:
                ops = self.ops[e]
                while pc[e] < len(ops):
                    op = ops[pc[e]]
                    ok = True
                    for w in op.waits:
                        if w[0] == "op":
                            s_, v = w[1].semval
                            if vals.get(id(s_), 0) < v:
                                ok = False
                        else:
                            if vals.get(id(w[1]), 0) < 16 * w[2]:
                                ok = False
                    if not ok:
                        break
                    if op.dma_group is not None:
                        vals[id(op.dma_group)] = vals.get(id(op.dma_group), 0) + 16
                    elif op.signal:
                        vals[id(op.semval[0])] = vals.get(id(op.semval[0]), 0) + 1
                        assert vals[id(op.semval[0])] == op.semval[1], (name, e, pc[e])
                    pc[e] += 1
                    progress = True
        for e in self.ENGS:
            if pc[e] < len(self.ops[e]):
                op = self.ops[e][pc[e]]
                desc = []
                for w in op.waits:
                    if w[0] == "op":
                        desc.append(("op", w[1].eng, w[1].idx, w[1].semval[1], vals.get(id(w[1].semval[0]), 0)))
                    else:
                        desc.append(("dma", w[1].name, 16 * w[2], vals.get(id(w[1]), 0)))
                raise RuntimeError(f"DEADLOCK in phase {name}: engine {e} stuck at op {pc[e]}/{len(self.ops[e])} waits={desc}")

    def end_phase(self, name):
        nc = self.nc
        fin = Op("sp", lambda e: e.nop(), self.base["sp"] + len(self.ops["sp"]))
        for g in self.used_dsems:
            if g.count > self.waited_dma["sp"].get(g, 0):
                fin.waits.append(("dma", g, g.count, self.gen))
                self.waited_dma["sp"][g] = g.count
        self.ops["sp"].append(fin)
        self.phase_eng_sems = []
        for e in self.ENGS:
            epoch = 0
            cnt = 0
            sem = None
            for op in self.ops[e]:
                if op.signal and op.dma_group is None:
                    if sem is None or cnt >= SEM_EPOCH:
                        lst = self.eng_sems.setdefault(e, [])
                        if epoch >= len(lst):
                            lst.append(self.new_sem(f"e_{e}_{epoch}"))
                        sem = lst[epoch]
                        epoch += 1
                        cnt = 0
                        self.phase_eng_sems.append(sem)
                    cnt += 1
                    op.semval = (sem, cnt)
        self._simulate(name)
        with nc.Block() as block:
            def run(e, handle):
                for op in self.ops[e]:
                    for w in op.waits:
                        if w[0] == "op":
                            s, v = w[1].semval
                            handle.wait_ge(s, v)
                        else:
                            handle.wait_ge(w[1].sem, 16 * w[2])
                    ins = op.fn(handle)
                    self.ninst += 1
                    if op.dma_group is not None:
                        ins.then_inc(op.dma_group.sem, 16)
                    elif op.signal:
                        ins.then_inc(op.semval[0], 1)

            @block.tensor
            def _(h):
                run("pe", h)

            @block.scalar
            def _(h):
                run("act", h)

            @block.vector
            def _(h):
                run("dve", h)

            @block.gpsimd
            def _(h):
                run("pool", h)

            @block.sync
            def _(h):
                run("sp", h)
        used_eng_sems = list(self.phase_eng_sems)
        dsems = list(self.used_dsems)
        with nc.Block() as block2:
            @block2.sync
            def _(h):
                for g in dsems:
                    if g.queue != "pool":
                        h.sem_clear(g.sem)
                for s_ in used_eng_sems:
                    h.sem_clear(s_)
        if hasattr(self, "simvals"):
            self.simvals = {}
        for e in self.ENGS:
            self.base[e] += len(self.ops[e])
            self.ops[e] = []
            self.cur_cnt[e] = 0
        for e in self.ENGS:
            for x in self.ENGS:
                self.waited_ops[e][x] = self.base[x] - 1
            self.waited_dma[e] = {}
        for b in self.bufs:
            b.last_w = None
            b.readers = []
        for g in dsems:
            assert 16 * g.count < 60000, (g.name, g.count)
            if g.queue != "pool":
                g.count = 0
                self.free_dsems.setdefault(g.queue, []).append(g)
        self.used_dsems = []
        self.gen += 1


class Pool:
    def __init__(self, alloc, name, shape, dt, n):
        self.items = [TT_(alloc(f"{name}{i}", shape, dt), f"{name}{i}") for i in range(n)]
        self.i = 0

    def next(self):
        t = self.items[self.i % len(self.items)]
        self.i += 1
        return t


class K:
    pass


def build_program(debug_outs=(), stop_after=None, n_layers=2, n_exp=32):
    nc = bass.Bass("TRN2", target_bir_lowering=False)
    k = K()
    k.nc = nc

    def din(name, shape, dt=F32):
        return nc.dram_tensor(name, list(shape), dt, kind="ExternalInput").ap()

    def dscr(name, shape, dt=F32):
        kind = "ExternalOutput" if name in debug_outs else "Internal"
        return nc.dram_tensor(name, list(shape), dt, kind=kind).ap()

    I = K()
    I.xall = din("xall", [TALL, D])
    I.cvec = din("cvec", [128, 16])
    I.w_ada = din("w_ada", [2, D, 6 * D])
    I.b_ada = din("b_ada", [2, 6 * D])
    I.norm1 = din("norm1", [2, D])
    I.norm2 = din("norm2", [2, D])
    I.final_norm = din("final_norm", [D])
    I.w_in = din("w_in_p", [2, D, NCOLP])
    I.gla_wa2 = din("gla_wa2", [2, 2, 16, 512])
    I.gla_ba = din("gla_ba", [2, 2, 512])
    I.gla_norm = din("gla_norm", [2, D])
    I.ret_norm = din("ret_norm", [2, D])
    I.convw = din("convw", [2, 128, 8, 5])
    I.lruw = din("lruw", [2, 128, 32, 256])
    I.lruv = din("lruv", [2, 128, 48])
    I.w_branch = din("w_branch", [2, 3, D, D])
    I.w_out = din("w_out", [2, D, D])
    I.w_router = din("w_router", [2, D, 32])
    I.b_router = din("b_router", [2, 32])
    I.w_gu = din("w_gu_d", [2, n_exp, D, 2048])
    I.bgu = din("bgu", [2, 128, 32 * 16])
    I.w_down = din("w_down", [2, n_exp, D, D])
    I.b_down = din("b_down", [2, 32, D])
    I.ident = din("ident", [128, 128])
    I.tri = din("tri", [2, 128, 128])
    I.maskT = din("maskT", [2, 128, 512])
    I.cosT = din("cosT", [128, TALL])
    I.sinT = din("sinT", [128, TALL])
    I.cos2 = din("cos2", [TALL, 512])
    I.sin2 = din("sin2", [TALL, 512])
    I.rtab = din("rtab", [2, 3, 128, 512])
    I.rdec = din("rdec", [2, 128, 4])
    out = nc.dram_tensor("out", [NLAT, D], F32, kind="ExternalOutput").ap()

    S = K()
    S.modv = dscr("modv", [2, 6 * D])
    S.X = dscr("X", [TALL, D])
    S.QT = dscr("QT", [512, TALL], BF16)
    S.KT = dscr("KT", [512, TALL], BF16)
    S.LRT = dscr("LRT", [32, TALL], BF16)
    S.P6T = dscr("P6T", [D, TALL], F32)
    S.P7T = dscr("P7T", [D, TALL], BF16)
    S.RQT = dscr("RQT", [512, TALL], BF16)
    S.RKT = dscr("RKT", [512, TALL], BF16)
    S.P12T = dscr("P12T", [3 * D, TALL], BF16)
    S.Kt = dscr("Kt", [TALL, 512], BF16)
    S.Vt = dscr("Vt", [TALL, D], BF16)
    S.P3 = dscr("P3", [TALL, D], BF16)
    S.RKt = dscr("RKt", [TALL, 512], BF16)
    S.RVt = dscr("RVt", [TALL, D], BF16)
    S.P11 = dscr("P11", [TALL, D], BF16)
    S.OG = [dscr("OGf", [TALL, D]), dscr("OGb", [TALL, D])]
    S.OR = [dscr("ORf", [TALL, D]), dscr("ORb", [TALL, D])]
    S.LRUT = dscr("LRUT", [D, TALL], BF16)
    S.XN2T = dscr("XN2T", [D, TALL], BF16)
    S.ACC0 = dscr("ACC0", [TALL, D])
    S.GLAT = dscr("GLAT", [D, TALL], BF16)
    S.RETT = dscr("RETT", [D, TALL], BF16)
    S.WR = dscr("WR", [TALL, 64])
    S.XNT = dscr("XNT", [D, TALL], BF16) if "XNT" in debug_outs else None
    S.XNT2 = dscr("XNT2", [D, TALL], BF16) if "XNT2" in debug_outs else None
    k.debug_outs = debug_outs

    with ExitStack() as gst:
        P = Prog(nc, gst)
        k.P = P
        B = K()
        for n in ["modv", "X", "QT", "KT", "LRT", "P6T", "P7T", "RQT", "RKT", "P12T", "Kt", "Vt", "P3", "RKt", "RVt",
                  "P11", "OGf", "OGb", "ORf", "ORb", "LRUT", "XN2T", "WR", "out", "GLAT", "RETT", "ACC0"]:
            setattr(B, n, P.buf(n))
        G = K()
        for n in ["ld0", "ld1", "ld2", "ld3", "w0", "w1", "w2", "st0", "st1", "st2", "st3", "out"]:
            setattr(G, n, None)
        k.I, k.S, k.B, k.G, k.out = I, S, B, G, out

        k.stop_after = stop_after
        for li in range(n_layers):
            last = li == 1
            x_src = I.xall if li == 0 else S.X
            seq = [("ada", lambda: phase_ada(k, li)), ("ab", lambda: phase_ab(k, li, x_src)), ("c1", lambda: phase_c1(k, li)),
                   ("c2", lambda: phase_c2(k, li)), ("d1", lambda: phase_d1(k, li, last)), ("d2", lambda: phase_d2(k, li, x_src, last)),
                   ("f", lambda: phase_f(k, li, last))]
            done = False
            for name, fn in seq:
                fn()
                if stop_after == (name, li) or (name == "ab" and stop_after == ("a", li)):
                    done = True
                    break
            if done:
                break
    k.ninst = P.ninst
    return nc, k


def mk_helpers(k, st):
    nc, P = k.nc, k.P

    def uniq(name):
        k.uid = getattr(k, "uid", 0) + 1
        return f"{name}_u{k.uid}"

    def sb(name, shape, dt=F32):
        return TT_(st.enter_context(nc.sbuf_tensor(uniq(name), list(shape), dt)), name)

    def ps(name, shape, dt=F32):
        t = TT_(st.enter_context(nc.psum_tensor(uniq(name), list(shape), dt)), name)
        t.is_psum = True
        return t

    def sb_raw(name, shape, dt=F32):
        return st.enter_context(nc.sbuf_tensor(name, list(shape), dt))

    def MM(out, out_ap, lhsT, rhs, rd, start=True, stop=True):
        P.op("pe", lambda e: e.matmul(out_ap, lhsT=lhsT, rhs=rhs, start=start, stop=stop), reads=rd, writes=[out])

    def TR(out, out_ap, in_ap, ident_ap, rd):
        P.op("pe", lambda e: e.transpose(out=out_ap, in_=in_ap, identity=ident_ap), reads=rd, writes=[out])

    def ACT(out, out_ap, in_ap, func, rd, bias=None, scale=None, accum=None, extra_w=()):
        kw = {}
        if bias is not None:
            kw["bias"] = bias
        if scale is not None:
            kw["scale"] = scale
        if accum is not None:
            kw["accum_out"] = accum
        P.op("act", lambda e: e.activation(out=out_ap, in_=in_ap, func=func, **kw), reads=rd, writes=[out] + list(extra_w))

    def TT(eng, out, out_ap, in0, in1, op, rd):
        P.op(eng, lambda e: e.tensor_tensor(out=out_ap, in0=in0, in1=in1, op=op), reads=rd, writes=[out])

    def TS(eng, out, out_ap, in0, s1, s2, op0, op1, rd, accum=None, extra_w=()):
        if op1 is None:
            P.op(eng, lambda e: e.tensor_scalar(out=out_ap, in0=in0, scalar1=s1, scalar2=None, op0=op0), reads=rd, writes=[out])
        elif accum is not None:
            P.op(eng, lambda e: e.tensor_scalar(out=out_ap, in0=in0, scalar1=s1, scalar2=s2, op0=op0, op1=op1, accum_out=accum),
                 reads=rd, writes=[out] + list(extra_w))
        else:
            P.op(eng, lambda e: e.tensor_scalar(out=out_ap, in0=in0, scalar1=s1, scalar2=s2, op0=op0, op1=op1), reads=rd, writes=[out])

    def STT(eng, out, out_ap, in0, scalar, in1, op0, op1, rd):
        P.op(eng, lambda e: e.scalar_tensor_tensor(out=out_ap, in0=in0, scalar=scalar, in1=in1, op0=op0, op1=op1),
             reads=rd, writes=[out])

    def CP(eng, out, out_ap, in_ap, rd):
        if eng == "act":
            P.op("act", lambda e: e.copy(out=out_ap, in_=in_ap), reads=rd, writes=[out])
        else:
            P.op(eng, lambda e: e.tensor_copy(out=out_ap, in_=in_ap), reads=rd, writes=[out])

    def RECIP(out, out_ap, in_ap, rd):
        P.op("dve", lambda e: e.reciprocal(out=out_ap, in_=in_ap), reads=rd, writes=[out])

    def MEMSET(eng, out, out_ap, val):
        P.op(eng, lambda e: e.memset(out_ap, val), reads=[], writes=[out])

    def LD(queue, dst, dst_ap, src_ap, group, src_bufs=(), **kw):
        P.dma(queue, dst_ap, src_ap, dst, reads=list(src_bufs), writes=[dst], **kw)

    def ST(queue, dst_ap, src, src_ap, group, dst_buf, **kw):
        P.dma(queue, dst_ap, src_ap, src, reads=[src], writes=[dst_buf], **kw)

    h = K()
    for n, f in list(locals().items()):
        if callable(f) and n not in ("h",):
            setattr(h, n, f)
    return h


def bcast_row(ap_row, n=128):
    return ap_row.partition_broadcast(n)


def phase_ada(k, li):
    I, S, B, G, P = k.I, k.S, k.B, k.G, k.P
    with ExitStack() as st:
        h = mk_helpers(k, st)
        cv = h.sb("ada_cv", [128, 16])
        sc = h.sb("ada_sc", [128, 16])
        sg = h.sb("ada_sg", [128, 16])
        brow = h.sb("ada_brow", [1, 6 * D])
        rows = [h.sb(f"ada_row{v}", [1, 6 * D]) for v in range(2)]
        wpool = Pool(h.sb, "ada_w", [128, 8, 512], F32, 2)
        pm = [Pool(h.ps, f"ada_pm{v}_", [1, 512], F32, 2) for v in range(2)]
        h.LD("sp", cv, cv[:], I.cvec, G.ld0)
        h.LD("sp", brow, brow[:], I.b_ada[li:li + 1, :], G.ld0)
        h.ACT(sg, sg[:], cv[:], AF.Sigmoid, [cv])
        h.TT("dve", sc, sc[:], cv[:], sg[:], ALU.mult, [cv, sg])
        wv = I.w_ada[li].rearrange("(kc p) n -> p kc n", p=128)
        for j in range(12):
            w = wpool.next()
            h.LD("sp", w, w[:], wv[:, :, j * 512:(j + 1) * 512], G.w0)
            for v in range(2):
                p = pm[v].next()
                for kc in range(8):
                    h.MM(p, p[:], sc[:, v * 8 + kc:v * 8 + kc + 1], w[:, kc, :], [sc, w], start=(kc == 0), stop=(kc == 7))
                h.TT("dve", rows[v], rows[v][:, j * 512:(j + 1) * 512], p[:], brow[:, j * 512:(j + 1) * 512], ALU.add, [p, brow])
        for v in range(2):
            h.ST("sp", S.modv[v:v + 1, :], rows[v], rows[v][:], G.st0, B.modv)
        P.end_phase(f"ada{li}")


def rms_mod_tile(h, k, xt, G_, S_, out_t, out_ap, tmp, stat):
    h.ACT(tmp, tmp[:], xt[:], AF.Square, [xt], accum=stat[:, 0:1], extra_w=[stat])
    h.TS("dve", stat, stat[:, 1:2], stat[:, 0:1], 1.0 / D, EPS, ALU.mult, ALU.add, [stat])
    h.ACT(stat, stat[:, 2:3], stat[:, 1:2], AF.Sqrt, [stat])
    h.RECIP(stat, stat[:, 3:4], stat[:, 2:3], [stat])
    h.STT("dve", tmp, tmp[:], xt[:], stat[:, 3:4], G_[:], ALU.mult, ALU.mult, [xt, stat, G_])
    h.TT("dve", out_t, out_ap, tmp[:], S_[:], ALU.add, [tmp, S_])


def load_mod_tiles(h, k, li, normw, idx_shift, idx_scale, names):
    I, S, B, G = k.I, k.S, k.B, k.G
    nb = h.sb(names + "_nb", [128, D])
    h.LD("sp", nb, nb[:], bcast_row(normw[li]), G.ld0)
    Gs, Ss = [], []
    for v in range(2):
        g = h.sb(f"{names}_G{v}", [128, D])
        s = h.sb(f"{names}_S{v}", [128, D])
        h.LD("sp", g, g[:], bcast_row(S.modv[v, idx_scale * D:(idx_scale + 1) * D]), G.ld0, [B.modv])
        h.LD("sp", s, s[:], bcast_row(S.modv[v, idx_shift * D:(idx_shift + 1) * D]), G.ld0, [B.modv])
        h.STT("dve", g, g[:], g[:], 1.0, nb[:], ALU.add, ALU.mult, [g, nb])
        Gs.append(g)
        Ss.append(s)
    return Gs, Ss


def phase_ab(k, li, x_src):
    I, S, B, G, P = k.I, k.S, k.B, k.G, k.P
    with ExitStack() as st:
        h = mk_helpers(k, st)
        xnT = h.sb("xnT", [128, 8, TALL], BF16)
        identf = h.sb("identf", [128, 128])
        identb = h.sb("identb", [128, 128], BF16)
        h.LD("sp", identf, identf[:], I.ident, G.ld0)
        h.CP("dve", identb, identb[:], identf[:], [identf])
        with ExitStack() as st2:
            h2 = mk_helpers(k, st2)
            Gs, Ss = load_mod_tiles(h2, k, li, I.norm1, 0, 1, "a")
            xpool = Pool(h2.sb, "a_x", [128, D], F32, 2)
            tmp = h2.sb("a_tmp", [128, D])
            xnp = Pool(h2.sb, "a_xn", [128, D], BF16, 2)
            stat = Pool(h2.sb, "a_stat", [128, 4], F32, 2)
            ptr = Pool(h2.ps, "a_ptr", [128, 8, 128], BF16, 2)
            for i in range(NT):
                v = 1 if i < 2 else 0
                xt = xpool.next()
                h2.LD("sp", xt, xt[:], x_src[i * 128:(i + 1) * 128, :], G.ld1, [B.X] if li > 0 else [])
                xn = xnp.next()
                rms_mod_tile(h2, k, xt, Gs[v], Ss[v], xn, xn[:], tmp, stat.next())
                p = ptr.next()
                for kc in range(8):
                    h2.TR(p, p[:, kc, :], xn[:, kc * 128:(kc + 1) * 128], identb[:], [xn, identb])
                h2.CP("act" if i % 2 == 0 else "dve", xnT, xnT[:, :, i * 128:(i + 1) * 128], p[:], [p])
            if S.XNT is not None:
                h2.ST("sp", S.XNT.rearrange("(kc p) t -> p kc t", p=128), xnT, xnT[:], G.st3, P.buf("XNT"))
            P.end_phase(f"a{li}")
        if k.stop_after == ("a", li):
            return
        with ExitStack() as st2:
            h2 = mk_helpers(k, st2)
            cosT = h2.sb("b_cosT", [128, TALL])
            sinT = h2.sb("b_sinT", [128, TALL])
            h2.LD("sp", cosT, cosT[:], I.cosT, G.ld0)
            h2.LD("sp", sinT, sinT[:], I.sinT, G.ld0)
            wpool = Pool(h2.sb, "b_w", [128, 8, 512], BF16, 3)
            stage_bf = Pool(h2.sb, "b_stb", [128, TALL], BF16, 2)
            stage_f = Pool(h2.sb, "b_stf", [128, TALL], F32, 1)
            stage_tm = Pool(h2.sb, "b_sttm", [128, 512], BF16, 3)
            t1p = Pool(h2.sb, "b_t1", [128, 512], F32, 2)
            t2p = Pool(h2.sb, "b_t2", [128, 512], F32, 2)
            c2p = Pool(h2.sb, "b_c2", [128, 512], F32, 2)
            s2p = Pool(h2.sb, "b_s2", [128, 512], F32, 2)
            pp = Pool(h2.ps, "b_p", [128, 512], F32, 6)
            wv = I.w_in[li].rearrange("(kc p) n -> p kc n", p=128)
            wgi = [0]

            def load_w(c0, ncol):
                w = wpool.next()
                g = [G.w0, G.w1, G.w2][wgi[0] % 3]
                wgi[0] += 1
                h2.LD("pool", w, w[:, :, 0:ncol], wv[:, :, c0:c0 + ncol], g)
                return w

            tbs = [(t0, min(512, TALL - t0)) for t0 in range(0, TALL, 512)]
            evi = [0]

            def ev_eng():
                evi[0] += 1
                return "act" if evi[0] % 2 == 0 else "dve"

            def fm_job(c0, ncol, dst, dst_buf, f32=False, swap_c0=None):
                for s0 in range(0, ncol, 512):
                    nc_ = min(512, ncol - s0)
                    w = load_w(c0 + s0, nc_)
                    ws = load_w(swap_c0 + s0, nc_) if swap_c0 is not None else None
                    for sub in range(0, nc_, 128):
                        m = min(128, nc_ - sub)
                        stg = (stage_f if f32 else stage_bf).next()
                        for (t0, tn) in tbs:
                            p = pp.next()
                            for kc in range(8):
                                h2.MM(p, p[0:m, 0:tn], w[:, kc, sub:sub + m], xnT[:, kc, t0:t0 + tn], [w, xnT],
                                      start=(kc == 0), stop=(kc == 7))
                            if ws is None:
                                h2.CP(ev_eng(), stg, stg[0:m, t0:t0 + tn], p[0:m, 0:tn], [p])
                            else:
                                p2 = pp.next()
                                for kc in range(8):
                                    h2.MM(p2, p2[0:m, 0:tn], ws[:, kc, sub:sub + m], xnT[:, kc, t0:t0 + tn], [ws, xnT],
                                          start=(kc == 0), stop=(kc == 7))
                                t1 = t1p.next()
                                t2 = t2p.next()
                                h2.TT("dve", t1, t1[:, 0:tn], p[:, 0:tn], cosT[:, t0:t0 + tn], ALU.mult, [p, cosT])
                                h2.TT("dve", t2, t2[:, 0:tn], p2[:, 0:tn], sinT[:, t0:t0 + tn], ALU.mult, [p2, sinT])
                                h2.TT("dve", stg, stg[:, t0:t0 + tn], t1[:, 0:tn], t2[:, 0:tn], ALU.add, [t1, t2])
                        r0 = s0 + sub
                        h2.ST("sp", dst[r0:r0 + m, :], stg, stg[0:m, :], G.st0, dst_buf)

            def tm_job(c0, ncol, dst, dst_buf, dcol0=0, rope=False):
                for s0 in range(0, ncol, 512):
                    w = load_w(c0 + s0, 512)
                    for i in range(NT):
                        p = pp.next()
                        for kc in range(8):
                            h2.MM(p, p[:], xnT[:, kc, i * 128:(i + 1) * 128], w[:, kc, :], [w, xnT], start=(kc == 0), stop=(kc == 7))
                        stg = stage_tm.next()
                        if not rope:
                            h2.CP(ev_eng(), stg, stg[:], p[:], [p])
                        else:
                            c2 = c2p.next()
                            s2 = s2p.next()
                            h2.LD("sp", c2, c2[:], I.cos2[i * 128:(i + 1) * 128, :], G.ld2)
                            h2.LD("sp", s2, s2[:], I.sin2[i * 128:(i + 1) * 128, :], G.ld2)
                            t1 = t1p.next()
                            t2 = t2p.next()
                            h2.TT("dve", t1, t1[:], p[:], c2[:], ALU.mult, [p, c2])
                            pv = p[:].rearrange("p (h two s) -> p h two s", h=4, two=2)
                            t2v = t2[:].rearrange("p (h two s) -> p h two s", h=4, two=2)
                            s2v = s2[:].rearrange("p (h two s) -> p h two s", h=4, two=2)
                            h2.TT("dve", t2, t2v[:, :, 0, :], pv[:, :, 1, :], s2v[:, :, 0, :], ALU.mult, [p, s2])
                            h2.TT("dve", t2, t2v[:, :, 1, :], pv[:, :, 0, :], s2v[:, :, 1, :], ALU.mult, [p, s2])
                            h2.TT("dve", stg, stg[:], t1[:], t2[:], ALU.add, [t1, t2])
                        h2.ST("sp", dst[i * 128:(i + 1) * 128, dcol0 + s0:dcol0 + s0 + 512], stg, stg[:], G.st1, dst_buf)

            import os
            sel = os.environ.get("BJOBS")
            sel = sel.split(",") if sel else None
            jobs = [
                ("QT", lambda: fm_job(OFF["q"], 512, S.QT, B.QT)),
                ("KT", lambda: fm_job(OFF["k"], 512, S.KT, B.KT)),
                ("LRT", lambda: fm_job(OFF["lrf"], 32, S.LRT, B.LRT)),
                ("Kt", lambda: tm_job(OFF["k"], 512, S.Kt, B.Kt)),
                ("Vt", lambda: tm_job(OFF["v"], 1024, S.Vt, B.Vt)),
                ("RQT", lambda: fm_job(OFF["rq"], 512, S.RQT, B.RQT, swap_c0=OFF["rqs"])),
                ("RKT", lambda: fm_job(OFF["rk"], 512, S.RKT, B.RKT, swap_c0=OFF["rks"])),
                ("RKt", lambda: tm_job(OFF["rk"], 512, S.RKt, B.RKt, rope=True)),
                ("RVt", lambda: tm_job(OFF["rv"], 1024, S.RVt, B.RVt)),
                ("P6T", lambda: fm_job(OFF["p6"], 1024, S.P6T, B.P6T, f32=True)),
                ("P7T", lambda: fm_job(OFF["p7"], 1024, S.P7T, B.P7T)),
                ("P3", lambda: tm_job(OFF["p3"], 1024, S.P3, B.P3)),
                ("P11", lambda: tm_job(OFF["p11"], 1024, S.P11, B.P11)),
                ("P12T", lambda: fm_job(OFF["p12"], 3072, S.P12T, B.P12T)),
            ]
            for jn, jf in jobs:
                if sel is None or jn in sel:
                    jf()
            if S.XNT2 is not None:
                h2.ST("sp", S.XNT2.rearrange("(kc p) t -> p kc t", p=128), xnT, xnT[:], G.st3, P.buf("XNT2"))
            P.end_phase(f"b{li}")


def phase_c1(k, li):
    I, S, B, G, P = k.I, k.S, k.B, k.G, k.P
    SCALE = 128.0 ** -0.5
    with ExitStack() as st:
        h = mk_helpers(k, st)
        tri = [h.sb(f"c_tri{d}", [128, 128]) for d in range(2)]
        maskT = [h.sb(f"c_mask{d}", [128, 512]) for d in range(2)]
        rtab = [[h.sb(f"c_rtab{d}{j}", [128, 512]) for j in range(3)] for d in range(2)]
        rdec = [h.sb(f"c_rdec{d}", [128, 4]) for d in range(2)]
        for d in range(2):
            h.LD("sp", tri[d], tri[d][:], I.tri[d], G.ld0)
            h.LD("sp", maskT[d], maskT[d][:], I.maskT[d], G.ld0)
            h.LD("sp", rdec[d], rdec[d][:], I.rdec[d], G.ld0)
            for j in range(3):
                h.LD("sp", rtab[d][j], rtab[d][j][:], I.rtab[d, j], G.ld0)
        wa2 = h.sb("c_wa2", [16, 2, 512], BF16)
        ba = h.sb("c_ba", [1, 2, 512], BF16)
        ones = h.sb("c_ones", [1, 128], BF16)
        wa2f = h.sb("c_wa2f", [16, 2, 512])
        baf = h.sb("c_baf", [1, 2, 512])
        for d_ in range(2):
            h.LD("sp", wa2f, wa2f[:, d_, :], I.gla_wa2[li, d_], G.w0)
            h.LD("sp", baf, baf[:, d_, :], I.gla_ba[li, d_:d_ + 1, :], G.w0)
        h.CP("dve", wa2, wa2[:], wa2f[:], [wa2f])
        h.CP("dve", ba, ba[:], baf[:], [baf])
        h.MEMSET("dve", ones, ones[:], 1.0)
        Sf = {}
        Sb = {}
        for kind in range(2):
            for d in range(2):
                Sf[kind, d] = h.sb(f"c_S{kind}{d}", [128, 1024])
                Sb[kind, d] = h.sb(f"c_Sb{kind}{d}", [128, 1024], BF16)
                h.MEMSET("dve", Sf[kind, d], Sf[kind, d][:], 0.0)
                h.MEMSET("dve", Sb[kind, d], Sb[kind, d][:], 0.0)
        NB = 3
        qTp = Pool(h.sb, "c_qT", [128, 512], BF16, NB)
        kTp = Pool(h.sb, "c_kT", [128, 512], BF16, NB)
        ktp = Pool(h.sb, "c_kt", [128, 512], BF16, NB)
        vtp = Pool(h.sb, "c_vt", [128, 1024], BF16, NB)
        lrp = Pool(h.sb, "c_lr", [16, 128], BF16, NB)
        e1p = Pool(h.sb, "c_e1", [128, 512], F32, 2)
        spp = Pool(h.sb, "c_sp", [128, 512], F32, 2)
        ektmp = Pool(h.sb, "c_ektm", [128, 512], F32, 2)
        eqtp = Pool(h.sb, "c_eqt", [128, 512], F32, 2)
        ektp = Pool(h.sb, "c_ekt", [128, 512], F32, 2)
        kinvp = Pool(h.sb, "c_kinv", [128, 512], BF16, 2)
        qdecp = Pool(h.sb, "c_qdec", [128, 512], BF16, 2)
        kinvTp = Pool(h.sb, "c_kinvT", [128, 512], BF16, 2)
        scTp = Pool(h.sb, "c_scT", [128, 512], BF16, 2)
        osbp = Pool(h.sb, "c_osb", [128, 1024], F32, 2)
        px = h.ps("c_px", [128, 512])
        pG = h.ps("c_pG", [128, 512])
        pGT = h.ps("c_pGT", [128, 512])
        psc = h.ps("c_psc", [128, 512])
        po = h.ps("c_po", [128, 1024])
        pkv = h.ps("c_pkv", [128, 1024])

        def tile(kind, d, i, cnt):
            QT, KT, Kt, Vt = (S.QT, S.KT, S.Kt, S.Vt) if kind == 0 else (S.RQT, S.RKT, S.RKt, S.RVt)
            bQT, bKT, bKt, bVt = (B.QT, B.KT, B.Kt, B.Vt) if kind == 0 else (B.RQT, B.RKT, B.RKt, B.RVt)
            O = (S.OG if kind == 0 else S.OR)[d]
            bO = getattr(B, ("OG" if kind == 0 else "OR") + ("f" if d == 0 else "b"))
            ts_ = slice(i * 128, (i + 1) * 128)
            qT = qTp.next()
            kT = kTp.next()
            kt = ktp.next()
            vt = vtp.next()
            gl = [G.ld1, G.ld2, G.ld3][cnt % 3]
            h.LD("sp", qT, qT[:].rearrange("p (h c) -> p h c", h=4), QT.rearrange("(h p) t -> p h t", p=128)[:, :, ts_], gl, [bQT])
            h.LD("sp", kT, kT[:].rearrange("p (h c) -> p h c", h=4), KT.rearrange("(h p) t -> p h t", p=128)[:, :, ts_], gl, [bKT])
            h.LD("sp", kt, kt[:], Kt[ts_, :], gl, [bKt])
            h.LD("sp", vt, vt[:], Vt[ts_, :], gl, [bVt])
            if kind == 0:
                lr = lrp.next()
                h.LD("sp", lr, lr[:], S.LRT[d * 16:(d + 1) * 16, ts_], gl, [B.LRT])
                h.MM(px, px[:], lr[:], wa2[:, d, :], [lr, wa2], start=True, stop=False)
                h.MM(px, px[:], ones[:], ba[:, d, :], [ones, ba], start=False, stop=True)
                e1 = e1p.next()
                h.ACT(e1, e1[:], px[:], AF.Exp, [px], scale=-1.0)
                sp = spp.next()
                h.ACT(sp, sp[:], e1[:], AF.Ln, [e1], bias=1.0)
                h.MM(pG, pG[:], tri[d][:], sp[:], [tri[d], sp])
                for hh in range(4):
                    h.MM(pGT, pGT[:, hh * 128:(hh + 1) * 128], sp[:, hh * 128:(hh + 1) * 128], tri[d][:], [tri[d], sp])
                EkTM = ektmp.next()
                h.ACT(EkTM, EkTM[:], pG[:], AF.Exp, [pG], scale=1.0 / 16)
                EqT = eqtp.next()
                h.ACT(EqT, EqT[:], pGT[:], AF.Exp, [pGT], scale=-1.0 / 16)
                EkT = ektp.next()
                h.ACT(EkT, EkT[:], pGT[:], AF.Exp, [pGT], scale=1.0 / 16)
                lastc = 127 if d == 0 else 0
                dec = [EqT[:, hh * 128 + lastc:hh * 128 + lastc + 1] for hh in range(4)]
                dec_t = EqT
            else:
                EqT, EkT, EkTM = rtab[d]
                dec = [rdec[d][:, hh:hh + 1] for hh in range(4)]
                dec_t = rdec[d]
            kinv = kinvp.next()
            h.TT("dve", kinv, kinv[:], kt[:], EkTM[:], ALU.mult, [kt, EkTM])
            qdec = qdecp.next()
            h.STT("dve", qdec, qdec[:], qT[:], SCALE, EqT[:], ALU.mult, ALU.mult, [qT, EqT])
            kinvT = kinvTp.next()
            h.TT("dve", kinvT, kinvT[:], kT[:], EkT[:], ALU.mult, [kT, EkT])
            for hh in range(4):
                hs = slice(hh * 128, (hh + 1) * 128)
                h.MM(psc, psc[:, hs], kinvT[:, hs], qdec[:, hs], [kinvT, qdec])
            scT = scTp.next()
            h.TT("dve", scT, scT[:], psc[:], maskT[d][:], ALU.mult, [psc, maskT[d]])
            sbf = Sb[kind, d]
            sf = Sf[kind, d]
            for hh in range(4):
                hs = slice(hh * 128, (hh + 1) * 128)
                vs = slice(hh * 256, (hh + 1) * 256)
                h.MM(po, po[:, vs], scT[:, hs], vt[:, vs], [scT, vt], start=True, stop=False)
                h.MM(po, po[:, vs], qdec[:, hs], sbf[:, vs], [qdec, sbf], start=False, stop=True)
            osb = osbp.next()
            h.CP("act", osb, osb[:], po[:], [po])
            h.ST("sp", O[ts_, :], osb, osb[:], G.st0 if d == 0 else G.st1, bO)
            for hh in range(4):
                hs = slice(hh * 128, (hh + 1) * 128)
                vs = slice(hh * 256, (hh + 1) * 256)
                h.MM(pkv, pkv[:, vs], kinv[:, hs], vt[:, vs], [kinv, vt])
            h.TT("dve", sf, sf[:], sf[:], pkv[:], ALU.add, [sf, pkv])
            for hh in range(4):
                vs = slice(hh * 256, (hh + 1) * 256)
                h.TS("dve", sf, sf[:, vs], sf[:, vs], dec[hh], None, ALU.mult, None, [sf, dec_t])
            h.CP("act", sbf, sbf[:], sf[:], [sf])

        fwd = list(range(NT))
        bwd = [1, 0] + list(range(NT - 1, 1, -1))
        cnt = 0
        import os
        kinds = [int(x) for x in os.environ.get("C1KINDS", "0,1").split(",")]
        nsteps = int(os.environ.get("C1N", NT))
        for s in range(nsteps):
            for kind in kinds:
                tile(kind, 0, fwd[s], cnt)
                cnt += 1
                tile(kind, 1, bwd[s], cnt)
                cnt += 1
        P.end_phase(f"c1{li}")


def rev_ap(ap2d, c0, n):
    a = ap2d[:, c0:c0 + n]
    return AP(a.tensor, a.offset + (n - 1) * a.ap[-1][0], [list(a.ap[0]), [-a.ap[-1][0], n]])


def phase_c2(k, li):
    I, S, B, G, P = k.I, k.S, k.B, k.G, k.P
    with ExitStack() as st:
        h = mk_helpers(k, st)
        convw = h.sb("l_convw", [128, 8, 5])
        lruw = h.sb("l_w", [128, 32, 256], BF16)
        lruv = h.sb("l_v", [128, 48])
        c8 = h.sb("l_c8", [128, 32])
        tmpv = h.sb("l_tmpv", [128, 16])
        h.LD("sp", convw, convw[:], I.convw[li], G.ld0)
        for q_ in range(8):
            h.LD("pool", lruw, lruw[:, q_ * 4:(q_ + 1) * 4, :], I.lruw[li, :, q_ * 4:(q_ + 1) * 4, :], G.w0)
        h.LD("sp", lruv, lruv[:], I.lruv[li], G.ld0)
        h.ACT(tmpv, tmpv[:], lruv[:, 32:48], AF.Exp, [lruv], scale=-1.0)
        h.ACT(tmpv, tmpv[:], tmpv[:], AF.Ln, [tmpv], bias=1.0)
        h.TS("dve", c8, c8[:, 0:16], tmpv[:], -8.0, None, ALU.mult, None, [tmpv])
        h.TS("dve", c8, c8[:, 16:32], tmpv[:], -16.0, None, ALU.mult, None, [tmpv])
        NPAD = TALL + 8
        xc = [h.sb(f"l_xc{j}", [128, TALL]) for j in range(2)]
        xcb = [h.sb(f"l_xcb{j}", [128, TALL], BF16) for j in range(2)]
        a_t = h.sb("l_a", [128, NPAD])
        b_t = h.sb("l_b", [128, TALL])
        hf = h.sb("l_hf", [128, TALL])
        hb = h.sb("l_hb", [128, TALL])
        p7 = h.sb("l_p7", [128, TALL], BF16)
        gl = a_t
        ob = h.sb("l_ob", [128, TALL], BF16)
        rp = Pool(h.sb, "l_r", [128, 512], F32, 2)
        ip = Pool(h.sb, "l_i", [128, 512], F32, 2)
        a2p = Pool(h.sb, "l_a2", [128, 512], F32, 2)
        pr = Pool(h.ps, "l_pr", [128, 512], F32, 3)
        pi = Pool(h.ps, "l_pi", [128, 512], F32, 3)
        tbs = [(t0, min(512, TALL - t0)) for t0 in range(0, TALL, 512)]
        CO, LO = 2, 261
        import os
        c2stop = int(os.environ.get("C2STOP", 99))
        if c2stop <= 1:
            P.end_phase(f"c2{li}")
            return
        for g in range(4):
            for j in range(2):
                cc = 2 * g + j
                xp = a_t
                h.MEMSET("dve", xp, xp[:, 0:2], 0.0)
                h.MEMSET("dve", xp, xp[:, 258:261], 0.0)
                h.MEMSET("dve", xp, xp[:, NPAD - 3:NPAD], 0.0)
                h.LD("sp", xp, xp[:, CO:CO + NCTX], S.P6T[cc * 128:(cc + 1) * 128, 0:NCTX], G.ld1, [B.P6T])
                h.LD("sp", xp, xp[:, LO:LO + NLAT], S.P6T[cc * 128:(cc + 1) * 128, NCTX:TALL], G.ld1, [B.P6T])
                for (o0, d0, n) in ((CO, 0, NCTX), (LO, NCTX, NLAT)):
                    h.TS("dve", xc[j], xc[j][:, d0:d0 + n], xp[:, o0 - 2:o0 - 2 + n], convw[:, cc, 0:1], convw[:, cc, 4:5],
                         ALU.mult, ALU.add, [xp, convw])
                    for tap in range(1, 4):
                        h.STT("dve", xc[j], xc[j][:, d0:d0 + n], xp[:, o0 - 2 + tap:o0 - 2 + tap + n], convw[:, cc, tap:tap + 1],
                              xc[j][:, d0:d0 + n], ALU.mult, ALU.add, [xp, convw, xc[j]])
                h.CP("act", xcb[j], xcb[j][:], xc[j][:], [xc[j]])
            if c2stop <= 2:
                P.end_phase(f"c2{li}")
                return
            for j in range(2):
                cc = 2 * g + j
                h.LD("sp", p7, p7[:], S.P7T[cc * 128:(cc + 1) * 128, :], G.ld2, [B.P7T])
                for d in range(2):
                    for (t0, tn) in tbs:
                        pr_ = pr.next()
                        pi_ = pi.next()
                        for gate, pt in ((0, pr_), (1, pi_)):
                            for ic in range(2):
                                widx = ((d * 2 + gate) * 4 + g) * 2 + ic
                                h.MM(pt, pt[:, 0:tn], lruw[:, widx, j * 128:(j + 1) * 128], xcb[ic][:, t0:t0 + tn], [lruw, xcb[ic]],
                                     start=(ic == 0), stop=(ic == 1))
                        r = rp.next()
                        ii = ip.next()
                        a2 = a2p.next()
                        vcol = d * 8 + cc
                        h.ACT(r, r[:, 0:tn], pr_[:, 0:tn], AF.Sigmoid, [pr_, lruv], bias=lruv[:, vcol:vcol + 1])
                        h.ACT(ii, ii[:, 0:tn], pi_[:, 0:tn], AF.Sigmoid, [pi_, lruv], bias=lruv[:, 16 + vcol:16 + vcol + 1])
                        h.ACT(a_t, a_t[:, t0:t0 + tn], r[:, 0:tn], AF.Exp, [r, c8], scale=c8[:, vcol:vcol + 1])
                        h.ACT(a2, a2[:, 0:tn], r[:, 0:tn], AF.Exp, [r, c8], scale=c8[:, 16 + vcol:16 + vcol + 1])
                        h.ACT(a2, a2[:, 0:tn], a2[:, 0:tn], AF.Sqrt, [a2], scale=-1.0, bias=1.0)
                        h.TT("dve", ii, ii[:, 0:tn], ii[:, 0:tn], xc[j][:, t0:t0 + tn], ALU.mult, [ii, xc[j]])
                        h.TT("dve", b_t, b_t[:, t0:t0 + tn], ii[:, 0:tn], a2[:, 0:tn], ALU.mult, [ii, a2])
                    if c2stop <= 3:
                        continue
                    if d == 0:
                        P.op("dve", lambda e: e.tensor_tensor_scan(out=hf[:], data0=a_t[:, 0:TALL], data1=b_t[:], initial=0.0,
                                                                   op0=ALU.mult, op1=ALU.add), reads=[a_t, b_t], writes=[hf])
                    else:
                        P.op("dve", lambda e: e.tensor_tensor_scan(out=rev_ap(hb[:], 0, NCTX), data0=rev_ap(a_t[:], 0, NCTX),
                                                                   data1=rev_ap(b_t[:], 0, NCTX), initial=0.0,
                                                                   op0=ALU.mult, op1=ALU.add), reads=[a_t, b_t], writes=[hb])
                        P.op("dve", lambda e: e.tensor_tensor_scan(out=rev_ap(hb[:], NCTX, NLAT), data0=rev_ap(a_t[:], NCTX, NLAT),
                                                                   data1=rev_ap(b_t[:], NCTX, NLAT), initial=hb[:, 0:1],
                                                                   op0=ALU.mult, op1=ALU.add), reads=[a_t, b_t, hb], writes=[hb])
                h.TT("dve", hf, hf[:], hf[:], hb[:], ALU.add, [hf, hb])
                if c2stop <= 4:
                    P.end_phase(f"c2{li}")
                    return
                glv = gl[:, 0:TALL]
                h.TT("dve", gl, glv, p7[:], p7[:], ALU.mult, [p7])
                h.TS("dve", gl, glv, glv, 0.044715, 1.0, ALU.mult, ALU.add, [gl])
                h.TT("dve", gl, glv, glv, p7[:], ALU.mult, [gl, p7])
                h.ACT(gl, glv, glv, AF.Sigmoid, [gl], scale=1.5957691216057308)
                h.TT("dve", gl, glv, glv, p7[:], ALU.mult, [gl, p7])
                h.TT("dve", ob, ob[:], glv, hf[:], ALU.mult, [gl, hf])
                h.ST("sp", S.LRUT[cc * 128:(cc + 1) * 128, :], ob, ob[:], G.st0, B.LRUT)
        P.end_phase(f"c2{li}")


def phase_d1(k, li, last):
    I, S, B, G, P = k.I, k.S, k.B, k.G, k.P
    t_first = 2 if last else 0
    with ExitStack() as st:
        h = mk_helpers(k, st)
        identf = h.sb("d_identf", [128, 128])
        identb = h.sb("d_identb", [128, 128], BF16)
        h.LD("sp", identf, identf[:], I.ident, G.ld0)
        h.CP("dve", identb, identb[:], identf[:], [identf])
        gn = h.sb("d_gn", [128, D])
        rn = h.sb("d_rn", [128, D])
        h.LD("sp", gn, gn[:], bcast_row(I.gla_norm[li]), G.ld0)
        h.LD("sp", rn, rn[:], bcast_row(I.ret_norm[li]), G.ld0)
        oa = Pool(h.sb, "d_oa", [128, D], F32, 3)
        obp = Pool(h.sb, "d_ob", [128, D], F32, 3)
        gp = Pool(h.sb, "d_g", [128, D], BF16, 3)
        sqp = Pool(h.sb, "d_sq", [128, D], F32, 2)
        stat = Pool(h.sb, "d_stat", [128, 16], F32, 3)
        nb = Pool(h.sb, "d_nb", [128, D], BF16, 2)
        oT = Pool(h.sb, "d_oT", [128, 8, 128], BF16, 3)
        ptr = Pool(h.ps, "d_ptr", [128, 8, 128], BF16, 3)

        def headnorm(o, gate_src, bsrc, normw, center, i, dst, bdst):
            stt = stat.next()
            sq = sqp.next()
            ov = o[:].rearrange("p (h e) -> p h e", h=4)
            if center:
                P.op("dve", lambda e: e.tensor_reduce(out=stt[:, 0:4], in_=ov, axis=AX.X, op=ALU.add), reads=[o], writes=[stt])
                h.TS("dve", stt, stt[:, 0:4], stt[:, 0:4], -1.0 / 256, None, ALU.mult, None, [stt])
                for hh in range(4):
                    h.TS("dve", o, o[:, hh * 256:(hh + 1) * 256], o[:, hh * 256:(hh + 1) * 256], stt[:, hh:hh + 1], None, ALU.add, None, [o, stt])
            h.TT("dve", sq, sq[:], o[:], o[:], ALU.mult, [o])
            P.op("dve", lambda e: e.tensor_reduce(out=stt[:, 4:8], in_=sq[:].rearrange("p (h e) -> p h e", h=4), axis=AX.X, op=ALU.add),
                 reads=[sq], writes=[stt])
            h.TS("dve", stt, stt[:, 8:12], stt[:, 4:8], 1.0 / 256, EPS, ALU.mult, ALU.add, [stt])
            h.ACT(stt, stt[:, 8:12], stt[:, 8:12], AF.Sqrt, [stt])
            h.RECIP(stt, stt[:, 12:16], stt[:, 8:12], [stt])
            g = gp.next()
            h.LD("sp", g, g[:], gate_src[i * 128:(i + 1) * 128, :], G.ld2, [bsrc])
            h.ACT(sq, sq[:], g[:], AF.Sigmoid, [g])
            h.TT("dve", sq, sq[:], sq[:], g[:], ALU.mult, [sq, g])
            h.TT("dve", sq, sq[:], sq[:], normw[:], ALU.mult, [sq, normw])
            n_ = nb.next()
            for hh in range(4):
                vs = slice(hh * 256, (hh + 1) * 256)
                h.STT("dve", n_, n_[:, vs], o[:, vs], stt[:, 12 + hh:13 + hh], sq[:, vs], ALU.mult, ALU.mult, [o, stt, sq])
            p = ptr.next()
            for kc in range(8):
                h.TR(p, p[:, kc, :], n_[:, kc * 128:(kc + 1) * 128], identb[:], [n_, identb])
            t_ = oT.next()
            h.CP("act", t_, t_[:], p[:], [p])
            h.ST("sp", dst.rearrange("(kc p) t -> p kc t", p=128)[:, :, i * 128:(i + 1) * 128], t_, t_[:], G.st1, bdst)

        for i in range(t_first, NT):
            o1 = oa.next()
            o2 = obp.next()
            h.LD("sp", o1, o1[:], S.OG[0][i * 128:(i + 1) * 128, :], G.ld1, [B.OGf])
            h.LD("sp", o2, o2[:], S.OG[1][i * 128:(i + 1) * 128, :], G.ld1, [B.OGb])
            h.TT("dve", o1, o1[:], o1[:], o2[:], ALU.add, [o1, o2])
            headnorm(o1, S.P3, B.P3, gn, False, i, S.GLAT, B.GLAT)
            o1 = oa.next()
            o2 = obp.next()
            h.LD("sp", o1, o1[:], S.OR[0][i * 128:(i + 1) * 128, :], G.ld3, [B.ORf])
            h.LD("sp", o2, o2[:], S.OR[1][i * 128:(i + 1) * 128, :], G.ld3, [B.ORb])
            h.TT("dve", o1, o1[:], o1[:], o2[:], ALU.add, [o1, o2])
            headnorm(o1, S.P11, B.P11, rn, True, i, S.RETT, B.RETT)
        P.end_phase(f"d1{li}")


def phase_d2(k, li, x_src, last):
    I, S, B, G, P = k.I, k.S, k.B, k.G, k.P
    t_first = 2 if last else 0
    with ExitStack() as st:
        h = mk_helpers(k, st)
        identf = h.sb("e_identf", [128, 128])
        identb = h.sb("e_identb", [128, 128], BF16)
        h.LD("sp", identf, identf[:], I.ident, G.ld0)
        h.CP("dve", identb, identb[:], identf[:], [identf])
        wbr = [h.sb(f"e_wbr{j}", [128, 8, D], BF16) for j in range(3)]
        wout = h.sb("e_wout", [128, 8, D], BF16)
        for j in range(3):
            h.LD("pool", wbr[j], wbr[j][:], I.w_branch[li, j].rearrange("(kc p) n -> p kc n", p=128), G.w0)
        h.LD("pool", wout, wout[:], I.w_out[li].rearrange("(kc p) n -> p kc n", p=128), G.w0)
        wr = h.sb("e_wr", [128, 8, 32])
        h.LD("sp", wr, wr[:], I.w_router[li].rearrange("(kc p) n -> p kc n", p=128), G.ld0)
        wrh = h.sb("e_wrh", [128, 8, 32], BF16)
        wrl = h.sb("e_wrl", [128, 8, 32], BF16)
        h.CP("dve", wrh, wrh[:], wr[:], [wr])
        h.TT("dve", wr, wr[:], wr[:], wrh[:], ALU.subtract, [wr, wrh])
        h.CP("dve", wrl, wrl[:], wr[:], [wr])
        brb = h.sb("e_brb", [128, 32])
        h.LD("sp", brb, brb[:], bcast_row(I.b_router[li]), G.ld0)
        bdf = h.sb("e_bdf", [32, D])
        bdh = h.sb("e_bdh", [32, D], BF16)
        bdl = h.sb("e_bdl", [32, D], BF16)
        h.LD("sp", bdf, bdf[:], I.b_down[li], G.ld0)
        h.CP("dve", bdh, bdh[:], bdf[:], [bdf])
        h.TT("dve", bdf, bdf[:], bdf[:], bdh[:], ALU.subtract, [bdf, bdh])
        h.CP("dve", bdl, bdl[:], bdf[:], [bdf])
        whl = Pool(h.sb, "e_whl", [128, 64], BF16, 2)
        wT = Pool(h.sb, "e_wT", [32, 2, 128], BF16, 2)
        a0p = Pool(h.sb, "e_a0", [128, D], F32, 2)
        G2, S2 = load_mod_tiles(h, k, li, I.norm2, 3, 4, "e")
        gate1 = []
        for v in range(2):
            g = h.sb(f"e_gate{v}", [128, D])
            h.LD("sp", g, g[:], bcast_row(S.modv[v, 2 * D:3 * D]), G.ld0, [B.modv])
            gate1.append(g)
        srcT = [Pool(h.sb, f"e_src{j}_", [128, 8, 512], BF16, 1) for j in range(3)]
        p12p = Pool(h.sb, "e_p12", [128, 3, 512], BF16, 2)
        sg = Pool(h.sb, "e_sg", [128, 512], F32, 3)
        mrg = Pool(h.sb, "e_mrg", [128, 512], F32, 2)
        mT = h.sb("e_mT", [128, 8, 512], BF16)
        stat = Pool(h.sb, "e_stat", [128, 16], F32, 3)
        xp = Pool(h.sb, "e_x", [128, D], F32, 2)
        tmp = h.sb("e_tmp", [128, D])
        xn2 = h.sb("e_xn2", [128, D])
        xh = h.sb("e_xh", [128, D], BF16)
        xl = h.sb("e_xl", [128, D], BF16)
        xn2Tb = Pool(h.sb, "e_xn2Tb", [128, 8, 128], BF16, 2)
        xlTp = Pool(h.sb, "e_xlT", [128, 8, 128], BF16, 2)
        lg = Pool(h.sb, "e_lg", [128, 32], F32, 2)
        m8 = Pool(h.sb, "e_m8", [128, 8], F32, 2)
        wro = Pool(h.sb, "e_wro", [128, 64], F32, 2)
        ptr = Pool(h.ps, "e_ptr", [128, 8, 128], BF16, 2)
        plg = h.ps("e_plg", [128, 512])
        pbr = Pool(h.ps, "e_pbr", [128, 512], F32, 3)
        pout = h.ps("e_pout", [128, D])
        srcD = [(S.GLAT, B.GLAT), (S.LRUT, B.LRUT), (S.RETT, B.RETT)]
        groups = []
        t = t_first
        while t < NT:
            n = min(4, NT - t)
            groups.append((t, n))
            t += n
        import os
        d2stop = float(os.environ.get("D2STOP", 99))
        if d2stop <= 1:
            P.end_phase(f"d2{li}")
            return
        for (g0, gn_) in groups:
            ntok = gn_ * 128
            c0 = g0 * 128
            srcs = []
            for j in range(3):
                t_ = srcT[j].next()
                h.LD("sp", t_, t_[:, :, 0:ntok], srcD[j][0].rearrange("(kc p) t -> p kc t", p=128)[:, :, c0:c0 + ntok], G.ld3, [srcD[j][1]])
                srcs.append(t_)
            for dc in range(8):
                m = mrg.next()
                p12 = p12p.next()
                h.LD("sp", p12, p12[:, :, 0:ntok], S.P12T.rearrange("(j r) t -> r j t", j=3)[dc * 128:(dc + 1) * 128, :, c0:c0 + ntok], G.ld2, [B.P12T])
                for j in range(3):
                    pb = pbr.next()
                    for kc in range(8):
                        h.MM(pb, pb[:, 0:ntok], wbr[j][:, kc, dc * 128:(dc + 1) * 128], srcs[j][:, kc, 0:ntok], [wbr[j], srcs[j]],
                             start=(kc == 0), stop=(kc == 7))
                    s_ = sg.next()
                    h.ACT(s_, s_[:, 0:ntok], p12[:, j, 0:ntok], AF.Sigmoid, [p12])
                    if j == 0:
                        h.TT("dve", m, m[:, 0:ntok], pb[:, 0:ntok], s_[:, 0:ntok], ALU.mult, [pb, s_])
                    else:
                        h.TT("dve", s_, s_[:, 0:ntok], pb[:, 0:ntok], s_[:, 0:ntok], ALU.mult, [pb, s_])
                        if j == 1:
                            h.TT("dve", m, m[:, 0:ntok], m[:, 0:ntok], s_[:, 0:ntok], ALU.add, [m, s_])
                        else:
                            h.TT("dve", mT, mT[:, dc, 0:ntok], m[:, 0:ntok], s_[:, 0:ntok], ALU.add, [m, s_])
            if d2stop <= 2:
                P.end_phase(f"d2{li}")
                return
            for tl in range(gn_):
                i = g0 + tl
                v = 1 if i < 2 else 0
                for half in range(2):
                    for kc in range(8):
                        h.MM(pout, pout[:, half * 512:(half + 1) * 512], mT[:, kc, tl * 128:(tl + 1) * 128],
                             wout[:, kc, half * 512:(half + 1) * 512], [mT, wout], start=(kc == 0), stop=(kc == 7))
                xt = xp.next()
                h.LD("sp", xt, xt[:], x_src[i * 128:(i + 1) * 128, :], G.ld1, [B.X] if li > 0 else [])
                h.TT("dve", tmp, tmp[:], pout[:], gate1[v][:], ALU.mult, [pout, gate1[v]])
                h.TT("dve", xt, xt[:], xt[:], tmp[:], ALU.add, [xt, tmp])
                h.ST("sp", S.X[i * 128:(i + 1) * 128, :], xt, xt[:], G.st0, B.X)
                if d2stop <= 3:
                    P.end_phase(f"d2{li}")
                    return
                rms_mod_tile(h, k, xt, G2[v], S2[v], xn2, xn2[:], tmp, stat.next())
                if d2stop <= 3.2:
                    P.end_phase(f"d2{li}")
                    return
                xb = xn2Tb.next()
                xlT = xlTp.next()
                h.CP("act", xh, xh[:], xn2[:], [xn2])
                h.TT("dve", xl, xl[:], xn2[:], xh[:], ALU.subtract, [xn2, xh])
                for src_, dst_ in ((xh, xb), (xl, xlT)):
                    p = ptr.next()
                    for kc in range(8):
                        h.TR(p, p[:, kc, :], src_[:, kc * 128:(kc + 1) * 128], identb[:], [src_, identb])
                    h.CP("act" if src_ is xh else "dve", dst_, dst_[:], p[:], [p])
                if d2stop <= 3.5:
                    P.end_phase(f"d2{li}")
                    return
                h.ST("sp", S.XN2T.rearrange("(kc p) t -> p kc t", p=128)[:, :, i * 128:(i + 1) * 128], xb, xb[:], G.st1, B.XN2T)
                if d2stop <= 4:
                    P.end_phase(f"d2{li}")
                    return
                nmm = 0
                for (a_, w_t) in ((xb, wrh), (xlT, wrh), (xb, wrl)):
                    for kc in range(8):
                        h.MM(plg, plg[:, 0:32], a_[:, kc, :], w_t[:, kc, :], [a_, w_t], start=(nmm == 0), stop=(nmm == 23))
                        nmm += 1
                l_ = lg.next()
                h.TT("dve", l_, l_[:], plg[:, 0:32], brb[:], ALU.add, [plg, brb])
                m_ = m8.next()
                P.op("dve", lambda e, m_=m_, l_=l_: e.max(out=m_[:], in_=l_[:]), reads=[l_], writes=[m_])
                w_ = wro.next()
                stt = stat.next()
                h.TS("dve", w_, w_[:, 32:64], l_[:], m_[:, 3:4], None, ALU.is_ge, None, [l_, m_])
                h.TS("dve", stt, stt[:, 0:1], m_[:, 0:1], -1.0, None, ALU.mult, None, [m_])
                h.ACT(l_, l_[:], l_[:], AF.Exp, [l_, stt], bias=stt[:, 0:1])
                h.TT("dve", l_, l_[:], l_[:], w_[:, 32:64], ALU.mult, [l_, w_])
                P.op("dve", lambda e, stt=stt, l_=l_: e.tensor_reduce(out=stt[:, 1:2], in_=l_[:], axis=AX.X, op=ALU.add), reads=[l_], writes=[stt])
                h.RECIP(stt, stt[:, 2:3], stt[:, 1:2], [stt])
                h.TS("dve", w_, w_[:, 0:32], l_[:], stt[:, 2:3], None, ALU.mult, None, [l_, stt])
                h.ST("sp", S.WR[i * 128:(i + 1) * 128, :], w_, w_[:], G.st2, B.WR)
                wh_ = whl.next()
                h.CP("dve", wh_, wh_[:, 0:32], w_[:, 0:32], [w_])
                h.TT("dve", wh_, wh_[:, 32:64], w_[:, 0:32], wh_[:, 0:32], ALU.subtract, [w_, wh_])
                pw = ptr.next()
                h.TR(pw, pw[0:32, 0, :], wh_[:, 0:32], identb[:], [wh_, identb])
                h.TR(pw, pw[0:32, 1, :], wh_[:, 32:64], identb[:], [wh_, identb])
                wT_ = wT.next()
                h.CP("act", wT_, wT_[:], pw[0:32, 0:2, :], [pw])
                for half in range(2):
                    hs_ = slice(half * 512, (half + 1) * 512)
                    h.MM(pout, pout[:, hs_], wT_[:, 0, :], bdh[:, hs_], [wT_, bdh], start=True, stop=False)
                    h.MM(pout, pout[:, hs_], wT_[:, 1, :], bdh[:, hs_], [wT_, bdh], start=False, stop=False)
                    h.MM(pout, pout[:, hs_], wT_[:, 0, :], bdl[:, hs_], [wT_, bdl], start=False, stop=True)
                a0 = a0p.next()
                h.CP("act", a0, a0[:], pout[:], [pout])
                h.ST("sp", S.ACC0[i * 128:(i + 1) * 128, :], a0, a0[:], G.st2, B.ACC0)
        P.end_phase(f"d2{li}")


def phase_f(k, li, last):
    I, S, B, G, P = k.I, k.S, k.B, k.G, k.P
    out = k.out
    t_first = 2 if last else 0
    tiles = list(range(t_first, NT))
    if last:
        groups = [tiles[j:j + 8] for j in range(0, 32, 8)]
    else:
        groups = [tiles[0:9], tiles[9:18], tiles[18:26], tiles[26:34]]
    with ExitStack() as st:
        h = mk_helpers(k, st)
        bgu = h.sb("f_bgu", [128, 32 * 16])
        h.LD("sp", bgu, bgu[:], I.bgu[li], G.ld0)
        gate2 = []
        for v in range(2):
            g = h.sb(f"f_gate{v}", [128, D])
            h.LD("sp", g, g[:], bcast_row(S.modv[v, 5 * D:6 * D]), G.ld0, [B.modv])
            gate2.append(g)
        fnb = None
        if last:
            fnb = h.sb("f_fnb", [128, D])
            h.LD("sp", fnb, fnb[:], bcast_row(I.final_norm), G.ld0)
        wgu = Pool(h.sb, "f_wgu", [128, 8, 1024], BF16, 3)
        wdn = Pool(h.sb, "f_wdn", [128, 4, D], BF16, 3)
        xT = h.sb("f_xT", [128, 8, 9 * 128], BF16)
        acc = h.sb("f_acc", [128, 9, D])
        wr = h.sb("f_wr", [128, 9, 64])
        gcp = Pool(h.sb, "f_gc", [128, 512], F32, 2)
        ucp = Pool(h.sb, "f_uc", [128, 512], F32, 2)
        sip = Pool(h.sb, "f_si", [128, 512], F32, 2)
        actT = Pool(h.sb, "f_actT", [128, 4, 512], BF16, 2)
        xo = Pool(h.sb, "f_xo", [128, D], F32, 2)
        tmp = h.sb("f_tmp", [128, D])
        stat = Pool(h.sb, "f_stat", [128, 4], F32, 2)
        pg = Pool(h.ps, "f_pg", [128, 512], F32, 2)
        pu = Pool(h.ps, "f_pu", [128, 512], F32, 2)
        pd = Pool(h.ps, "f_pd", [128, D], F32, 2)
        wgi = [0]
        for grp in groups:
            ng = len(grp)
            c0 = grp[0] * 128
            ntok = ng * 128
            h.LD("sp", xT, xT[:, :, 0:ntok], S.XN2T.rearrange("(kc p) t -> p kc t", p=128)[:, :, c0:c0 + ntok], G.ld1, [B.XN2T])
            h.LD("sp", wr, wr[:, 0:ng, :], S.WR[c0:c0 + ntok, :].rearrange("(n p) e -> p n e", p=128), G.ld1, [B.WR])
            h.LD("sp", acc, acc[:, 0:ng, :], S.ACC0[c0:c0 + ntok, :].rearrange("(n p) d -> p n d", p=128), G.ld1, [B.ACC0])
            subs = [(s0, min(4, ng - s0)) for s0 in range(0, ng, 4)]
            def emit_gu(e, hf_, wg_, s0, sn):
                tn = sn * 128
                tc0 = s0 * 128
                aT = actT.next()
                for fl in range(4):
                    fc = hf_ * 4 + fl
                    pg_ = pg.next()
                    pu_ = pu.next()
                    for kc in range(8):
                        h.MM(pg_, pg_[:, 0:tn], wg_[:, kc, fl * 128:(fl + 1) * 128], xT[:, kc, tc0:tc0 + tn], [wg_, xT],
                             start=(kc == 0), stop=(kc == 7))
                    for kc in range(8):
                        h.MM(pu_, pu_[:, 0:tn], wg_[:, kc, 512 + fl * 128:512 + (fl + 1) * 128], xT[:, kc, tc0:tc0 + tn], [wg_, xT],
                             start=(kc == 0), stop=(kc == 7))
                    gc = gcp.next()
                    uc = ucp.next()
                    si = sip.next()
                    bcol = e * 16 + fc
                    h.TS("dve", gc, gc[:, 0:tn], pg_[:, 0:tn], bgu[:, bcol:bcol + 1], 7.0, ALU.add, ALU.min, [pg_, bgu])
                    h.ACT(uc, uc[:, 0:tn], pu_[:, 0:tn], AF.Identity, [pu_, bgu], bias=bgu[:, bcol + 8:bcol + 9])
                    h.ACT(si, si[:, 0:tn], gc[:, 0:tn], AF.Sigmoid, [gc], scale=1.702)
                    h.TS("dve", uc, uc[:, 0:tn], uc[:, 0:tn], 7.0, -7.0, ALU.min, ALU.max, [uc])
                    h.TT("dve", gc, gc[:, 0:tn], gc[:, 0:tn], si[:, 0:tn], ALU.mult, [gc, si])
                    h.STT("dve", aT, aT[:, fl, 0:tn], uc[:, 0:tn], 1.0, gc[:, 0:tn], ALU.add, ALU.mult, [uc, gc])
                return aT

            def emit_down(e, wd_, aT, s0, sn):
                for tl in range(sn):
                    ti = s0 + tl
                    pd_ = pd.next()
                    for half in range(2):
                        for fl in range(4):
                            h.MM(pd_, pd_[:, half * 512:(half + 1) * 512], aT[:, fl, tl * 128:(tl + 1) * 128],
                                 wd_[:, fl, half * 512:(half + 1) * 512], [aT, wd_], start=(fl == 0), stop=(fl == 3))
                    h.STT("dve", acc, acc[:, ti, :], pd_[:], wr[:, ti, e:e + 1], acc[:, ti, :], ALU.mult, ALU.add, [pd_, wr, acc])

            pending = None
            for e in range(32):
                for hf_ in range(2):
                    wg_ = wgu.next()
                    wd_ = wdn.next()
                    src = I.w_gu[li, e].rearrange("(kc p) n -> p kc n", p=128)
                    h.LD("pool", wg_, wg_[:, :, 0:512], src[:, :, hf_ * 512:(hf_ + 1) * 512], None)
                    h.LD("pool", wg_, wg_[:, :, 512:1024], src[:, :, 1024 + hf_ * 512:1024 + (hf_ + 1) * 512], None)
                    h.LD("pool", wd_, wd_[:], I.w_down[li, e, hf_ * 512:(hf_ + 1) * 512, :].rearrange("(fc p) n -> p fc n", p=128), None)
                    for (s0, sn) in subs:
                        aT = emit_gu(e, hf_, wg_, s0, sn)
                        if pending is not None:
                            emit_down(*pending)
                        pending = (e, wd_, aT, s0, sn)
            emit_down(*pending)
            for tl in range(ng):
                i = grp[tl]
                v = 1 if i < 2 else 0
                xt = xo.next()
                h.LD("sp", xt, xt[:], S.X[i * 128:(i + 1) * 128, :], G.ld2, [B.X])
                h.TT("dve", tmp, tmp[:], acc[:, tl, :], gate2[v][:], ALU.mult, [acc, gate2[v]])
                h.TT("dve", xt, xt[:], xt[:], tmp[:], ALU.add, [xt, tmp])
                if not last:
                    h.ST("sp", S.X[i * 128:(i + 1) * 128, :], xt, xt[:], G.st0, B.X)
                else:
                    stt = stat.next()
                    h.ACT(tmp, tmp[:], xt[:], AF.Square, [xt], accum=stt[:, 0:1], extra_w=[stt])
                    h.TS("dve", stt, stt[:, 1:2], stt[:, 0:1], 1.0 / D, EPS, ALU.mult, ALU.add, [stt])
                    h.ACT(stt, stt[:, 2:3], stt[:, 1:2], AF.Sqrt, [stt])
                    h.RECIP(stt, stt[:, 3:4], stt[:, 2:3], [stt])
                    h.STT("dve", xt, xt[:], xt[:], stt[:, 3:4], fnb[:], ALU.mult, ALU.mult, [xt, stt, fnb])
                    h.ST("sp", out[(i - 2) * 128:(i - 1) * 128, :], xt, xt[:], G.out, B.out)
        P.end_phase(f"f{li}")


def host_constants():
    f32 = np.float32
    c = {}
    c["ident"] = np.eye(128, dtype=f32)
    s = np.arange(128)[:, None]
    cc = np.arange(128)[None, :]
    c["tri"] = np.stack([(s <= cc), (s >= cc)]).astype(f32)
    c["maskT"] = np.stack([np.tile((s <= cc), (1, 4)), np.tile((s > cc), (1, 4))]).astype(f32)
    rows = NLAT // 64
    row = np.repeat(np.arange(rows), 64).astype(np.float64)
    col = np.tile(np.arange(64), rows).astype(np.float64)
    nf = 32
    inv = (10000.0 ** (-np.arange(nf, dtype=np.float32) / np.float32(nf))).astype(np.float32)
    ang = np.concatenate([row[:, None].astype(f32) * inv, col[:, None].astype(f32) * inv], axis=-1).astype(f32)
    cos = np.concatenate([np.ones((NCTX, 64), f32), np.cos(ang).astype(f32)], 0)
    sin = np.concatenate([np.zeros((NCTX, 64), f32), np.sin(ang).astype(f32)], 0)
    cos128 = np.concatenate([cos, cos], 1)
    sin128 = np.concatenate([-sin, sin], 1)
    c["cosT"] = np.ascontiguousarray(cos128.T)
    c["sinT"] = np.ascontiguousarray(sin128.T)
    c["cos2"] = np.ascontiguousarray(np.tile(cos128, (1, 4)))
    c["sin2"] = np.ascontiguousarray(np.tile(sin128, (1, 4)))
    gamma = 1.0 - 2.0 ** (-5.0 - np.arange(4, dtype=np.float64))
    lg = np.log(gamma)
    pos = np.arange(128, dtype=np.float64)
    rtab = np.zeros((2, 3, 128, 512), f32)
    rdec = np.zeros((2, 128, 4), f32)
    for d in range(2):
        steps = (pos + 1) if d == 0 else (128 - pos)
        for hh in range(4):
            G_ = steps * lg[hh]
            rtab[d, 0, :, hh * 128:(hh + 1) * 128] = np.exp(G_)[None, :]
            rtab[d, 1, :, hh * 128:(hh + 1) * 128] = np.exp(-G_)[None, :]
            rtab[d, 2, :, hh * 128:(hh + 1) * 128] = np.exp(-G_)[:, None]
            rdec[d, :, hh] = np.exp(128 * lg[hh])
    c["rtab"] = rtab
    c["rdec"] = rdec
    return c


def host_weights(inp):
    f32 = np.float32
    w = {}
    w_in = inp["w_in"]
    rq = w_in[:, :, OFF["rq"]:OFF["rq"] + 512].reshape(2, D, 4, 2, 64)[:, :, :, ::-1, :].reshape(2, D, 512)
    rk = w_in[:, :, OFF["rk"]:OFF["rk"] + 512].reshape(2, D, 4, 2, 64)[:, :, :, ::-1, :].reshape(2, D, 512)
    w["w_in_p"] = np.ascontiguousarray(np.concatenate([w_in, rq, rk], axis=2))
    for n in ["w_ada", "b_ada", "norm1", "norm2", "final_norm", "gla_wa2", "gla_ba", "gla_norm", "ret_norm", "w_branch",
              "w_out", "w_router", "b_router", "w_down", "b_down"]:
        w[n] = np.ascontiguousarray(inp[n])
    cw = np.concatenate([inp["lru_conv_w"], inp["lru_conv_b"][:, None, :]], axis=1)
    w["convw"] = np.ascontiguousarray(cw.reshape(2, 5, 8, 128).transpose(0, 3, 2, 1))
    lw = np.stack([inp["lru_wa"], inp["lru_wi"]], axis=2)
    lw = lw.reshape(2, 2, 2, 4, 2, 128, 256).transpose(0, 5, 1, 2, 3, 4, 6)
    w["lruw"] = np.ascontiguousarray(lw.reshape(2, 128, 32, 256))
    lv = np.stack([inp["lru_ba"], inp["lru_bi"], inp["lru_lam"]], axis=1)
    lv = lv.reshape(2, 3, 2, 8, 128).transpose(0, 4, 1, 2, 3)
    w["lruv"] = np.ascontiguousarray(lv.reshape(2, 128, 48))
    wg = inp["w_gu"].reshape(2, 32, D, 1024, 2).transpose(0, 1, 2, 4, 3)
    w["w_gu_d"] = np.ascontiguousarray(wg.reshape(2, 32, D, 2048))
    bg = inp["b_gu"].reshape(2, 32, 8, 128, 2).transpose(0, 3, 1, 4, 2)
    w["bgu"] = np.ascontiguousarray(bg.reshape(2, 128, 32 * 16))
    return w


_CACHE = {}


def kernel(**inputs):
    inp = {k_: np.asarray(v) for k_, v in inputs.items()}
    if "prog" not in _CACHE:
        _CACHE["prog"] = build_program()
    nc, k = _CACHE["prog"]
    consts = host_constants()
    wts = host_weights(inp)
    in_maps = []
    for b in range(8):
        m = dict(consts)
        m.update(wts)
        m["xall"] = np.ascontiguousarray(np.concatenate([inp["ctx"][b], inp["x"][b]], axis=0))
        cv = np.concatenate([inp["c"][b].reshape(8, 128).T, inp["c_ctx"].reshape(8, 128).T], axis=1)
        m["cvec"] = np.ascontiguousarray(cv.astype(np.float32))
        in_maps.append(m)
    res = run_bass_kernel_spmd(nc, in_maps, core_ids=list(range(8)))
    return np.stack([np.asarray(r["out"]) for r in res.results], axis=0).astype(np.float32)
```

```python
import numpy as np
import ml_dtypes
from contextlib import ExitStack
import concourse.bass as bass
import concourse.mybir as mybir
from concourse.ap import AP
from concourse.bass_utils import run_bass_kernel_spmd

F32 = mybir.dt.float32
BF16 = mybir.dt.bfloat16
AF = mybir.ActivationFunctionType
ALU = mybir.AluOpType
AX = mybir.AxisListType

SEM_EPOCH = 30000
SIDE_PER_STEP = 12
D = 1024
NCTX = 256
NLAT = 4096
TALL = NCTX + NLAT
NT = TALL // 128
EPS = 1e-6
NCOLP = 11296 + 1024
OFF = dict(q=0, k=512, v=1024, p3=2048, lrf=3072, lrb=3088, p6=3104, p7=4128, rq=5152, rk=5664, rv=6176,
           p11=7200, p12=8224, rqs=11296, rks=11808)


class Buf:
    __slots__ = ("name", "last_w", "readers")

    def __init__(self, name):
        self.name = name
        self.last_w = None
        self.readers = []


class DmaGroup:
    def __init__(self, sem, name):
        self.sem = sem
        self.count = 0
        self.name = name


class Op:
    __slots__ = ("eng", "fn", "waits", "signal", "idx", "semval", "dma_group")

    def __init__(self, eng, fn, idx):
        self.eng = eng
        self.fn = fn
        self.idx = idx
        self.waits = []
        self.signal = False
        self.semval = None
        self.dma_group = None


class TT_:
    def __init__(self, h, name):
        self.h = h
        self.b = Buf(name)
        self.dsem = None

    def __getitem__(self, k):
        return self.h[k]


class Prog:
    ENGS = ("pe", "act", "dve", "pool", "sp")

    def __init__(self, nc, gstack):
        self.nc = nc
        self.gstack = gstack
        self.base = {e: 0 for e in self.ENGS}
        self.ops = {e: [] for e in self.ENGS}
        self.waited_ops = {e: {x: -1 for x in self.ENGS} for e in self.ENGS}
        self.waited_dma = {e: {} for e in self.ENGS}
        self.groups = []
        self.nsem = 0
        self.cur_sem = {e: None for e in self.ENGS}
        self.cur_cnt = {e: 0 for e in self.ENGS}
        self.bufs = []
        self.ninst = 0
        self.free_dsems = {}
        self.used_dsems = []
        self.gen = 0
        self.eng_sems = {}

    def tile_sem(self, t, queue="sp"):
        if t.dsem is None or getattr(t, "dsem_gen", -1) != self.gen:
            t.dsem_gen = self.gen
            fl = self.free_dsems.setdefault(queue, [])
            if fl:
                t.dsem = fl.pop()
            else:
                t.dsem = DmaGroup(self.new_sem(f"d{self.nsem}"), f"d{self.nsem}")
                t.dsem.queue = queue
            self.used_dsems.append(t.dsem)
        assert t.dsem.queue == queue, "tile DMA'd from two queue types"
        return t.dsem

    def new_sem(self, name):
        self.nsem += 1
        return self.gstack.enter_context(self.nc.semaphore(name))

    def group(self, name):
        g = DmaGroup(self.new_sem("g_" + name), name)
        self.groups.append(g)
        return g

    def buf(self, name):
        b = Buf(name)
        self.bufs.append(b)
        return b

    def _add_dep(self, op, tok):
        if tok is None:
            return
        E = op.eng
        if tok[0] == "op":
            x = tok[1]
            if x.eng == E and E == "pe":
                return
            if self.waited_ops[E][x.eng] >= x.idx:
                return
            self.waited_ops[E][x.eng] = x.idx
            x.signal = True
            op.waits.append(tok)
        else:
            _, g, cnt, gen = tok
            if gen != self.gen:
                return
            if self.waited_dma[E].get(g, 0) >= cnt:
                return
            self.waited_dma[E][g] = cnt
            op.waits.append(tok)

    def _record(self, eng, fn, reads, writes, dma_group=None):
        op = Op(eng, fn, self.base[eng] + len(self.ops[eng]))
        self.ops[eng].append(op)
        for b in reads:
            self._add_dep(op, b.last_w)
        for b in writes:
            self._add_dep(op, b.last_w)
            for r in b.readers:
                self._add_dep(op, r)
        if dma_group is not None:
            dma_group.count += 1
            op.dma_group = dma_group
            tok = ("dma", dma_group, dma_group.count, self.gen)
        else:
            tok = ("op", op)
        for b in writes:
            b.last_w = tok
            b.readers = []
        for b in reads:
            if b not in writes:
                b.readers.append(tok)
        return op

    def op(self, eng, fn, reads=(), writes=()):
        rd = [r.b if isinstance(r, TT_) else r for r in reads if not getattr(r, "is_psum", False)]
        wr = [w.b if isinstance(w, TT_) else w for w in writes]
        wr += [r.b for r in reads if getattr(r, "is_psum", False) and r.b not in wr]
        return self._record(eng, fn, rd, wr)

    def dma(self, queue, out, in_, tile, reads=(), writes=(), **kw):
        def fn(e):
            return e.dma_start(out=out, in_=in_, **kw)
        return self._record(queue, fn, [r.b if isinstance(r, TT_) else r for r in reads],
                            [w.b if isinstance(w, TT_) else w for w in writes], dma_group=self.tile_sem(tile, queue))

    def _simulate(self, name):
        if not hasattr(self, "simvals"):
            self.simvals = {}
        vals = self.simvals
        pc = {e: 0 for e in self.ENGS}
        progress = True
        while progress:
            progress = False
            for e in self.ENGS:
                ops = self.ops[e]
                while pc[e] < len(ops):
                    op = ops[pc[e]]
                    ok = True
                    for w in op.waits:
                        if w[0] == "op":
                            s_, v = w[1].semval
                            if vals.get(id(s_), 0) < v:
                                ok = False
                        else:
                            if vals.get(id(w[1]), 0) < 16 * w[2]:
                                ok = False
                    if not ok:
                        break
                    if op.dma_group is not None:
                        vals[id(op.dma_group)] = vals.get(id(op.dma_group), 0) + 16
                    elif op.signal:
                        vals[id(op.semval[0])] = vals.get(id(op.semval[0]), 0) + 1
                        assert vals[id(op.semval[0])] == op.semval[1], (name, e, pc[e])
                    pc[e] += 1
                    progress = True
        for e in self.ENGS:
            if pc[e] < len(self.ops[e]):
                op = self.ops[e][pc[e]]
                desc = []
                for w in op.waits:
                    if w[0] == "op":
                        desc.append(("op", w[1].eng, w[1].idx, w[1].semval[1], vals.get(id(w[1].semval[0]), 0)))
                    else:
                        desc.append(("dma", w[1].name, 16 * w[2], vals.get(id(w[1]), 0)))
                raise RuntimeError(f"DEADLOCK in phase {name}: engine {e} stuck at op {pc[e]}/{len(self.ops[e])} waits={desc}")

    def end_phase(self, name):
        nc = self.nc
        fin = Op("sp", lambda e: e.nop(), self.base["sp"] + len(self.ops["sp"]))
        for g in self.used_dsems:
            if g.count > self.waited_dma["sp"].get(g, 0):
                fin.waits.append(("dma", g, g.count, self.gen))
                self.waited_dma["sp"][g] = g.count
        self.ops["sp"].append(fin)
        self.phase_eng_sems = []
        for e in self.ENGS:
            epoch = 0
            cnt = 0
            sem = None
            for op in self.ops[e]:
                if op.signal and op.dma_group is None:
                    if sem is None or cnt >= SEM_EPOCH:
                        lst = self.eng_sems.setdefault(e, [])
                        if epoch >= len(lst):
                            lst.append(self.new_sem(f"e_{e}_{epoch}"))
                        sem = lst[epoch]
                        epoch += 1
                        cnt = 0
                        self.phase_eng_sems.append(sem)
                    cnt += 1
                    op.semval = (sem, cnt)
        self._simulate(name)
        with nc.Block() as block:
            def run(e, handle):
                for op in self.ops[e]:
                    for w in op.waits:
                        if w[0] == "op":
                            s, v = w[1].semval
                            handle.wait_ge(s, v)
                        else:
                            handle.wait_ge(w[1].sem, 16 * w[2])
                    ins = op.fn(handle)
                    self.ninst += 1
                    if op.dma_group is not None:
                        ins.then_inc(op.dma_group.sem, 16)
                    elif op.signal:
                        ins.then_inc(op.semval[0], 1)

            @block.tensor
            def _(h):
                run("pe", h)

            @block.scalar
            def _(h):
                run("act", h)

            @block.vector
            def _(h):
                run("dve", h)

            @block.gpsimd
            def _(h):
                run("pool", h)

            @block.sync
            def _(h):
                run("sp", h)
        used_eng_sems = list(self.phase_eng_sems)
        dsems = list(self.used_dsems)
        with nc.Block() as block2:
            @block2.sync
            def _(h):
                for g in dsems:
                    if g.queue != "pool":
                        h.sem_clear(g.sem)
                for s_ in used_eng_sems:
                    h.sem_clear(s_)
        if hasattr(self, "simvals"):
            self.simvals = {}
        for e in self.ENGS:
            self.base[e] += len(self.ops[e])
            self.ops[e] = []
            self.cur_cnt[e] = 0
        for e in self.ENGS:
            for x in self.ENGS:
                self.waited_ops[e][x] = self.base[x] - 1
            self.waited_dma[e] = {}
        for b in self.bufs:
            b.last_w = None
            b.readers = []
        for g in dsems:
            assert 16 * g.count < 60000, (g.name, g.count)
            if g.queue != "pool":
                g.count = 0
                self.free_dsems.setdefault(g.queue, []).append(g)
        self.used_dsems = []
        self.gen += 1


class Pool:
    def __init__(self, alloc, name, shape, dt, n):
        self.items = [TT_(alloc(f"{name}{i}", shape, dt), f"{name}{i}") for i in range(n)]
        self.i = 0

    def next(self):
        t = self.items[self.i % len(self.items)]
        self.i += 1
        return t


class K:
    pass


def build_program(debug_outs=(), stop_after=None, n_layers=2, n_exp=32):
    nc = bass.Bass("TRN2", target_bir_lowering=False)
    k = K()
    k.nc = nc

    def din(name, shape, dt=F32):
        return nc.dram_tensor(name, list(shape), dt, kind="ExternalInput").ap()

    def dscr(name, shape, dt=F32):
        kind = "ExternalOutput" if name in debug_outs else "Internal"
        return nc.dram_tensor(name, list(shape), dt, kind=kind).ap()

    I = K()
    I.xall = din("xall", [TALL, D])
    I.cvec = din("cvec", [128, 16])
    I.w_ada = din("w_ada", [2, D, 6 * D])
    I.b_ada = din("b_ada", [2, 6 * D])
    I.norm1 = din("norm1", [2, D])
    I.norm2 = din("norm2", [2, D])
    I.final_norm = din("final_norm", [D])
    I.w_in = din("w_in_p", [2, D, NCOLP])
    I.gla_wa2 = din("gla_wa2", [2, 2, 16, 512])
    I.gla_ba = din("gla_ba", [2, 2, 512])
    I.gla_norm = din("gla_norm", [2, D])
    I.ret_norm = din("ret_norm", [2, D])
    I.convw = din("convw", [2, 128, 8, 5])
    I.lruw = din("lruw", [2, 128, 32, 256])
    I.lruv = din("lruv", [2, 128, 48])
    I.w_branch = din("w_branch", [2, 3, D, D])
    I.w_out = din("w_out", [2, D, D])
    I.w_router = din("w_router", [2, D, 32])
    I.b_router = din("b_router", [2, 32])
    I.w_gu = din("w_gu_d", [2, n_exp, D, 2048])
    I.bgu = din("bgu", [2, 128, 32 * 16])
    I.w_down = din("w_down", [2, n_exp, D, D])
    I.b_down = din("b_down", [2, 32, D])
    I.ident = din("ident", [128, 128])
    I.tri = din("tri", [2, 128, 128])
    I.maskT = din("maskT", [2, 128, 512])
    I.cosT = din("cosT", [128, TALL])
    I.sinT = din("sinT", [128, TALL])
    I.cos2 = din("cos2", [TALL, 512])
    I.sin2 = din("sin2", [TALL, 512])
    I.rtab = din("rtab", [2, 3, 128, 512])
    I.rdec = din("rdec", [2, 128, 4])
    out = nc.dram_tensor("out", [NLAT, D], F32, kind="ExternalOutput").ap()

    S = K()
    S.modv = dscr("modv", [2, 6 * D])
    S.X = dscr("X", [TALL, D])
    S.QT = dscr("QT", [512, TALL], BF16)
    S.KT = dscr("KT", [512, TALL], BF16)
    S.LRT = dscr("LRT", [32, TALL], BF16)
    S.P6T = dscr("P6T", [D, TALL], F32)
    S.P7T = dscr("P7T", [D, TALL], BF16)
    S.RQT = dscr("RQT", [512, TALL], BF16)
    S.RKT = dscr("RKT", [512, TALL], BF16)
    S.P12T = dscr("P12T", [3 * D, TALL], BF16)
    S.Kt = dscr("Kt", [TALL, 512], BF16)
    S.Vt = dscr("Vt", [TALL, D], BF16)
    S.P3 = dscr("P3", [TALL, D], BF16)
    S.RKt = dscr("RKt", [TALL, 512], BF16)
    S.RVt = dscr("RVt", [TALL, D], BF16)
    S.P11 = dscr("P11", [TALL, D], BF16)
    S.OG = [dscr("OGf", [TALL, D]), dscr("OGb", [TALL, D])]
    S.OR = [dscr("ORf", [TALL, D]), dscr("ORb", [TALL, D])]
    S.LRUT = dscr("LRUT", [D, TALL], BF16)
    S.XN2T = dscr("XN2T", [D, TALL], BF16)
    S.ACC0 = dscr("ACC0", [TALL, D])
    S.GLAT = dscr("GLAT", [D, TALL], BF16)
    S.RETT = dscr("RETT", [D, TALL], BF16)
    S.WR = dscr("WR", [TALL, 64])
    S.XNT = dscr("XNT", [D, TALL], BF16) if "XNT" in debug_outs else None
    S.XNT2 = dscr("XNT2", [D, TALL], BF16) if "XNT2" in debug_outs else None
    k.debug_outs = debug_outs

    with ExitStack() as gst:
        P = Prog(nc, gst)
        k.P = P
        B = K()
        for n in ["modv", "X", "QT", "KT", "LRT", "P6T", "P7T", "RQT", "RKT", "P12T", "Kt", "Vt", "P3", "RKt", "RVt",
                  "P11", "OGf", "OGb", "ORf", "ORb", "LRUT", "XN2T", "WR", "out", "GLAT", "RETT", "ACC0"]:
            setattr(B, n, P.buf(n))
        G = K()
        for n in ["ld0", "ld1", "ld2", "ld3", "w0", "w1", "w2", "st0", "st1", "st2", "st3", "out"]:
            setattr(G, n, None)
        k.I, k.S, k.B, k.G, k.out = I, S, B, G, out

        k.stop_after = stop_after
        for li in range(n_layers):
            last = li == 1
            x_src = I.xall if li == 0 else S.X
            seq = [("ada", lambda: phase_ada(k, li)), ("ab", lambda: phase_ab(k, li, x_src)), ("c1", lambda: phase_c1(k, li, side=lambda h_: c2_body(k, li, h_))), ("d1", lambda: phase_d1(k, li, last)), ("d2", lambda: phase_d2(k, li, x_src, last)),
                   ("f", lambda: phase_f(k, li, last))]
            done = False
            for name, fn in seq:
                fn()
                if stop_after == (name, li) or (name == "ab" and stop_after == ("a", li)):
                    done = True
                    break
            if done:
                break
    k.ninst = P.ninst
    return nc, k


def mk_helpers(k, st):
    nc, P = k.nc, k.P

    def uniq(name):
        k.uid = getattr(k, "uid", 0) + 1
        return f"{name}_u{k.uid}"

    def sb(name, shape, dt=F32):
        return TT_(st.enter_context(nc.sbuf_tensor(uniq(name), list(shape), dt)), name)

    def ps(name, shape, dt=F32):
        t = TT_(st.enter_context(nc.psum_tensor(uniq(name), list(shape), dt)), name)
        t.is_psum = True
        return t

    def sb_raw(name, shape, dt=F32):
        return st.enter_context(nc.sbuf_tensor(name, list(shape), dt))

    def MM(out, out_ap, lhsT, rhs, rd, start=True, stop=True):
        P.op("pe", lambda e: e.matmul(out_ap, lhsT=lhsT, rhs=rhs, start=start, stop=stop), reads=rd, writes=[out])

    def TR(out, out_ap, in_ap, ident_ap, rd):
        P.op("pe", lambda e: e.transpose(out=out_ap, in_=in_ap, identity=ident_ap), reads=rd, writes=[out])

    def ACT(out, out_ap, in_ap, func, rd, bias=None, scale=None, accum=None, extra_w=()):
        kw = {}
        if bias is not None:
            kw["bias"] = bias
        if scale is not None:
            kw["scale"] = scale
        if accum is not None:
            kw["accum_out"] = accum
        P.op("act", lambda e: e.activation(out=out_ap, in_=in_ap, func=func, **kw), reads=rd, writes=[out] + list(extra_w))

    def TT(eng, out, out_ap, in0, in1, op, rd):
        P.op(eng, lambda e: e.tensor_tensor(out=out_ap, in0=in0, in1=in1, op=op), reads=rd, writes=[out])

    def TS(eng, out, out_ap, in0, s1, s2, op0, op1, rd, accum=None, extra_w=()):
        if op1 is None:
            P.op(eng, lambda e: e.tensor_scalar(out=out_ap, in0=in0, scalar1=s1, scalar2=None, op0=op0), reads=rd, writes=[out])
        elif accum is not None:
            P.op(eng, lambda e: e.tensor_scalar(out=out_ap, in0=in0, scalar1=s1, scalar2=s2, op0=op0, op1=op1, accum_out=accum),
                 reads=rd, writes=[out] + list(extra_w))
        else:
            P.op(eng, lambda e: e.tensor_scalar(out=out_ap, in0=in0, scalar1=s1, scalar2=s2, op0=op0, op1=op1), reads=rd, writes=[out])

    def STT(eng, out, out_ap, in0, scalar, in1, op0, op1, rd):
        P.op(eng, lambda e: e.scalar_tensor_tensor(out=out_ap, in0=in0, scalar=scalar, in1=in1, op0=op0, op1=op1),
             reads=rd, writes=[out])

    def CP(eng, out, out_ap, in_ap, rd):
        if eng == "act":
            P.op("act", lambda e: e.copy(out=out_ap, in_=in_ap), reads=rd, writes=[out])
        else:
            P.op(eng, lambda e: e.tensor_copy(out=out_ap, in_=in_ap), reads=rd, writes=[out])

    def RECIP(out, out_ap, in_ap, rd):
        P.op("dve", lambda e: e.reciprocal(out=out_ap, in_=in_ap), reads=rd, writes=[out])

    def MEMSET(eng, out, out_ap, val):
        P.op(eng, lambda e: e.memset(out_ap, val), reads=[], writes=[out])

    def LD(queue, dst, dst_ap, src_ap, group, src_bufs=(), **kw):
        P.dma(queue, dst_ap, src_ap, dst, reads=list(src_bufs), writes=[dst], **kw)

    def ST(queue, dst_ap, src, src_ap, group, dst_buf, **kw):
        P.dma(queue, dst_ap, src_ap, src, reads=[src], writes=[dst_buf], **kw)

    h = K()
    for n, f in list(locals().items()):
        if callable(f) and n not in ("h",):
            setattr(h, n, f)
    return h


def bcast_row(ap_row, n=128):
    return ap_row.partition_broadcast(n)


def phase_ada(k, li):
    I, S, B, G, P = k.I, k.S, k.B, k.G, k.P
    with ExitStack() as st:
        h = mk_helpers(k, st)
        cv = h.sb("ada_cv", [128, 16])
        sc = h.sb("ada_sc", [128, 16])
        sg = h.sb("ada_sg", [128, 16])
        brow = h.sb("ada_brow", [1, 6 * D])
        rows = [h.sb(f"ada_row{v}", [1, 6 * D]) for v in range(2)]
        wpool = Pool(h.sb, "ada_w", [128, 8, 512], F32, 2)
        pm = [Pool(h.ps, f"ada_pm{v}_", [1, 512], F32, 2) for v in range(2)]
        h.LD("sp", cv, cv[:], I.cvec, G.ld0)
        h.LD("sp", brow, brow[:], I.b_ada[li:li + 1, :], G.ld0)
        h.ACT(sg, sg[:], cv[:], AF.Sigmoid, [cv])
        h.TT("dve", sc, sc[:], cv[:], sg[:], ALU.mult, [cv, sg])
        wv = I.w_ada[li].rearrange("(kc p) n -> p kc n", p=128)
        for j in range(12):
            w = wpool.next()
            h.LD("sp", w, w[:], wv[:, :, j * 512:(j + 1) * 512], G.w0)
            for v in range(2):
                p = pm[v].next()
                for kc in range(8):
                    h.MM(p, p[:], sc[:, v * 8 + kc:v * 8 + kc + 1], w[:, kc, :], [sc, w], start=(kc == 0), stop=(kc == 7))
                h.TT("dve", rows[v], rows[v][:, j * 512:(j + 1) * 512], p[:], brow[:, j * 512:(j + 1) * 512], ALU.add, [p, brow])
        for v in range(2):
            h.ST("sp", S.modv[v:v + 1, :], rows[v], rows[v][:], G.st0, B.modv)
        P.end_phase(f"ada{li}")


def rms_mod_tile(h, k, xt, G_, S_, out_t, out_ap, tmp, stat):
    h.ACT(tmp, tmp[:], xt[:], AF.Square, [xt], accum=stat[:, 0:1], extra_w=[stat])
    h.TS("dve", stat, stat[:, 1:2], stat[:, 0:1], 1.0 / D, EPS, ALU.mult, ALU.add, [stat])
    h.ACT(stat, stat[:, 2:3], stat[:, 1:2], AF.Sqrt, [stat])
    h.RECIP(stat, stat[:, 3:4], stat[:, 2:3], [stat])
    h.STT("dve", tmp, tmp[:], xt[:], stat[:, 3:4], G_[:], ALU.mult, ALU.mult, [xt, stat, G_])
    h.TT("dve", out_t, out_ap, tmp[:], S_[:], ALU.add, [tmp, S_])


def load_mod_tiles(h, k, li, normw, idx_shift, idx_scale, names):
    I, S, B, G = k.I, k.S, k.B, k.G
    nb = h.sb(names + "_nb", [128, D])
    h.LD("sp", nb, nb[:], bcast_row(normw[li]), G.ld0)
    Gs, Ss = [], []
    for v in range(2):
        g = h.sb(f"{names}_G{v}", [128, D])
        s = h.sb(f"{names}_S{v}", [128, D])
        h.LD("sp", g, g[:], bcast_row(S.modv[v, idx_scale * D:(idx_scale + 1) * D]), G.ld0, [B.modv])
        h.LD("sp", s, s[:], bcast_row(S.modv[v, idx_shift * D:(idx_shift + 1) * D]), G.ld0, [B.modv])
        h.STT("dve", g, g[:], g[:], 1.0, nb[:], ALU.add, ALU.mult, [g, nb])
        Gs.append(g)
        Ss.append(s)
    return Gs, Ss


def phase_ab(k, li, x_src):
    I, S, B, G, P = k.I, k.S, k.B, k.G, k.P
    with ExitStack() as st:
        h = mk_helpers(k, st)
        xnT = h.sb("xnT", [128, 8, TALL], BF16)
        identf = h.sb("identf", [128, 128])
        identb = h.sb("identb", [128, 128], BF16)
        h.LD("sp", identf, identf[:], I.ident, G.ld0)
        h.CP("dve", identb, identb[:], identf[:], [identf])
        with ExitStack() as st2:
            h2 = mk_helpers(k, st2)
            Gs, Ss = load_mod_tiles(h2, k, li, I.norm1, 0, 1, "a")
            xpool = Pool(h2.sb, "a_x", [128, D], F32, 2)
            tmp = h2.sb("a_tmp", [128, D])
            xnp = Pool(h2.sb, "a_xn", [128, D], BF16, 2)
            stat = Pool(h2.sb, "a_stat", [128, 4], F32, 2)
            ptr = Pool(h2.ps, "a_ptr", [128, 8, 128], BF16, 2)
            for i in range(NT):
                v = 1 if i < 2 else 0
                xt = xpool.next()
                h2.LD("sp", xt, xt[:], x_src[i * 128:(i + 1) * 128, :], G.ld1, [B.X] if li > 0 else [])
                xn = xnp.next()
                rms_mod_tile(h2, k, xt, Gs[v], Ss[v], xn, xn[:], tmp, stat.next())
                p = ptr.next()
                for kc in range(8):
                    h2.TR(p, p[:, kc, :], xn[:, kc * 128:(kc + 1) * 128], identb[:], [xn, identb])
                h2.CP("act" if i % 2 == 0 else "dve", xnT, xnT[:, :, i * 128:(i + 1) * 128], p[:], [p])
            if S.XNT is not None:
                h2.ST("sp", S.XNT.rearrange("(kc p) t -> p kc t", p=128), xnT, xnT[:], G.st3, P.buf("XNT"))
            P.end_phase(f"a{li}")
        if k.stop_after == ("a", li):
            return
        with ExitStack() as st2:
            h2 = mk_helpers(k, st2)
            cosT = h2.sb("b_cosT", [128, TALL])
            sinT = h2.sb("b_sinT", [128, TALL])
            h2.LD("sp", cosT, cosT[:], I.cosT, G.ld0)
            h2.LD("sp", sinT, sinT[:], I.sinT, G.ld0)
            wpool = Pool(h2.sb, "b_w", [128, 8, 512], BF16, 3)
            stage_bf = Pool(h2.sb, "b_stb", [128, TALL], BF16, 2)
            stage_f = Pool(h2.sb, "b_stf", [128, TALL], F32, 1)
            stage_tm = Pool(h2.sb, "b_sttm", [128, 512], BF16, 3)
            t1p = Pool(h2.sb, "b_t1", [128, 512], F32, 2)
            t2p = Pool(h2.sb, "b_t2", [128, 512], F32, 2)
            c2p = Pool(h2.sb, "b_c2", [128, 512], F32, 2)
            s2p = Pool(h2.sb, "b_s2", [128, 512], F32, 2)
            pp = Pool(h2.ps, "b_p", [128, 512], F32, 6)
            wv = I.w_in[li].rearrange("(kc p) n -> p kc n", p=128)
            wgi = [0]

            def load_w(c0, ncol):
                w = wpool.next()
                g = [G.w0, G.w1, G.w2][wgi[0] % 3]
                wgi[0] += 1
                h2.LD("pool", w, w[:, :, 0:ncol], wv[:, :, c0:c0 + ncol], g)
                return w

            tbs = [(t0, min(512, TALL - t0)) for t0 in range(0, TALL, 512)]
            evi = [0]

            def ev_eng():
                evi[0] += 1
                return "act" if evi[0] % 2 == 0 else "dve"

            def fm_job(c0, ncol, dst, dst_buf, f32=False, swap_c0=None):
                for s0 in range(0, ncol, 512):
                    nc_ = min(512, ncol - s0)
                    w = load_w(c0 + s0, nc_)
                    ws = load_w(swap_c0 + s0, nc_) if swap_c0 is not None else None
                    for sub in range(0, nc_, 128):
                        m = min(128, nc_ - sub)
                        stg = (stage_f if f32 else stage_bf).next()
                        for (t0, tn) in tbs:
                            p = pp.next()
                            for kc in range(8):
                                h2.MM(p, p[0:m, 0:tn], w[:, kc, sub:sub + m], xnT[:, kc, t0:t0 + tn], [w, xnT],
                                      start=(kc == 0), stop=(kc == 7))
                            if ws is None:
                                h2.CP(ev_eng(), stg, stg[0:m, t0:t0 + tn], p[0:m, 0:tn], [p])
                            else:
                                p2 = pp.next()
                                for kc in range(8):
                                    h2.MM(p2, p2[0:m, 0:tn], ws[:, kc, sub:sub + m], xnT[:, kc, t0:t0 + tn], [ws, xnT],
                                          start=(kc == 0), stop=(kc == 7))
                                t1 = t1p.next()
                                t2 = t2p.next()
                                h2.TT("dve", t1, t1[:, 0:tn], p[:, 0:tn], cosT[:, t0:t0 + tn], ALU.mult, [p, cosT])
                                h2.TT("dve", t2, t2[:, 0:tn], p2[:, 0:tn], sinT[:, t0:t0 + tn], ALU.mult, [p2, sinT])
                                h2.TT("dve", stg, stg[:, t0:t0 + tn], t1[:, 0:tn], t2[:, 0:tn], ALU.add, [t1, t2])
                        r0 = s0 + sub
                        h2.ST("sp", dst[r0:r0 + m, :], stg, stg[0:m, :], G.st0, dst_buf)

            def tm_job(c0, ncol, dst, dst_buf, dcol0=0, rope=False):
                for s0 in range(0, ncol, 512):
                    w = load_w(c0 + s0, 512)
                    for i in range(NT):
                        p = pp.next()
                        for kc in range(8):
                            h2.MM(p, p[:], xnT[:, kc, i * 128:(i + 1) * 128], w[:, kc, :], [w, xnT], start=(kc == 0), stop=(kc == 7))
                        stg = stage_tm.next()
                        if not rope:
                            h2.CP(ev_eng(), stg, stg[:], p[:], [p])
                        else:
                            c2 = c2p.next()
                            s2 = s2p.next()
                            h2.LD("sp", c2, c2[:], I.cos2[i * 128:(i + 1) * 128, :], G.ld2)
                            h2.LD("sp", s2, s2[:], I.sin2[i * 128:(i + 1) * 128, :], G.ld2)
                            t1 = t1p.next()
                            t2 = t2p.next()
                            h2.TT("dve", t1, t1[:], p[:], c2[:], ALU.mult, [p, c2])
                            pv = p[:].rearrange("p (h two s) -> p h two s", h=4, two=2)
                            t2v = t2[:].rearrange("p (h two s) -> p h two s", h=4, two=2)
                            s2v = s2[:].rearrange("p (h two s) -> p h two s", h=4, two=2)
                            h2.TT("dve", t2, t2v[:, :, 0, :], pv[:, :, 1, :], s2v[:, :, 0, :], ALU.mult, [p, s2])
                            h2.TT("dve", t2, t2v[:, :, 1, :], pv[:, :, 0, :], s2v[:, :, 1, :], ALU.mult, [p, s2])
                            h2.TT("dve", stg, stg[:], t1[:], t2[:], ALU.add, [t1, t2])
                        h2.ST("sp", dst[i * 128:(i + 1) * 128, dcol0 + s0:dcol0 + s0 + 512], stg, stg[:], G.st1, dst_buf)

            import os
            sel = os.environ.get("BJOBS")
            sel = sel.split(",") if sel else None
            jobs = [
                ("QT", lambda: fm_job(OFF["q"], 512, S.QT, B.QT)),
                ("KT", lambda: fm_job(OFF["k"], 512, S.KT, B.KT)),
                ("LRT", lambda: fm_job(OFF["lrf"], 32, S.LRT, B.LRT)),
                ("Kt", lambda: tm_job(OFF["k"], 512, S.Kt, B.Kt)),
                ("Vt", lambda: tm_job(OFF["v"], 1024, S.Vt, B.Vt)),
                ("RQT", lambda: fm_job(OFF["rq"], 512, S.RQT, B.RQT, swap_c0=OFF["rqs"])),
                ("RKT", lambda: fm_job(OFF["rk"], 512, S.RKT, B.RKT, swap_c0=OFF["rks"])),
                ("RKt", lambda: tm_job(OFF["rk"], 512, S.RKt, B.RKt, rope=True)),
                ("RVt", lambda: tm_job(OFF["rv"], 1024, S.RVt, B.RVt)),
                ("P6T", lambda: fm_job(OFF["p6"], 1024, S.P6T, B.P6T, f32=True)),
                ("P7T", lambda: fm_job(OFF["p7"], 1024, S.P7T, B.P7T)),
                ("P3", lambda: tm_job(OFF["p3"], 1024, S.P3, B.P3)),
                ("P11", lambda: tm_job(OFF["p11"], 1024, S.P11, B.P11)),
                ("P12T", lambda: fm_job(OFF["p12"], 3072, S.P12T, B.P12T)),
            ]
            for jn, jf in jobs:
                if sel is None or jn in sel:
                    jf()
            if S.XNT2 is not None:
                h2.ST("sp", S.XNT2.rearrange("(kc p) t -> p kc t", p=128), xnT, xnT[:], G.st3, P.buf("XNT2"))
            P.end_phase(f"b{li}")


def phase_c1(k, li, side=None):
    I, S, B, G, P = k.I, k.S, k.B, k.G, k.P
    SCALE = 128.0 ** -0.5
    with ExitStack() as st:
        h = mk_helpers(k, st)
        tri = [h.sb(f"c_tri{d}", [128, 128]) for d in range(2)]
        maskT = [h.sb(f"c_mask{d}", [128, 512]) for d in range(2)]
        rtab = [[h.sb(f"c_rtab{d}{j}", [128, 512]) for j in range(3)] for d in range(2)]
        rdec = [h.sb(f"c_rdec{d}", [128, 4]) for d in range(2)]
        for d in range(2):
            h.LD("sp", tri[d], tri[d][:], I.tri[d], G.ld0)
            h.LD("sp", maskT[d], maskT[d][:], I.maskT[d], G.ld0)
            h.LD("sp", rdec[d], rdec[d][:], I.rdec[d], G.ld0)
            for j in range(3):
                h.LD("sp", rtab[d][j], rtab[d][j][:], I.rtab[d, j], G.ld0)
        wa2 = h.sb("c_wa2", [16, 2, 512], BF16)
        ba = h.sb("c_ba", [1, 2, 512], BF16)
        ones = h.sb("c_ones", [1, 128], BF16)
        wa2f = h.sb("c_wa2f", [16, 2, 512])
        baf = h.sb("c_baf", [1, 2, 512])
        for d_ in range(2):
            h.LD("sp", wa2f, wa2f[:, d_, :], I.gla_wa2[li, d_], G.w0)
            h.LD("sp", baf, baf[:, d_, :], I.gla_ba[li, d_:d_ + 1, :], G.w0)
        h.CP("dve", wa2, wa2[:], wa2f[:], [wa2f])
        h.CP("dve", ba, ba[:], baf[:], [baf])
        h.MEMSET("dve", ones, ones[:], 1.0)
        Sf = {}
        Sb = {}
        for kind in range(2):
            for d in range(2):
                Sf[kind, d] = h.sb(f"c_S{kind}{d}", [128, 1024])
                Sb[kind, d] = h.sb(f"c_Sb{kind}{d}", [128, 1024], BF16)
                h.MEMSET("dve", Sf[kind, d], Sf[kind, d][:], 0.0)
                h.MEMSET("dve", Sb[kind, d], Sb[kind, d][:], 0.0)
        NB = 3
        qTp = Pool(h.sb, "c_qT", [128, 512], BF16, NB)
        kTp = Pool(h.sb, "c_kT", [128, 512], BF16, NB)
        ktp = Pool(h.sb, "c_kt", [128, 512], BF16, NB)
        vtp = Pool(h.sb, "c_vt", [128, 1024], BF16, NB)
        lrp = Pool(h.sb, "c_lr", [16, 128], BF16, NB)
        e1p = Pool(h.sb, "c_e1", [128, 512], F32, 1)
        spp = Pool(h.sb, "c_sp", [128, 512], F32, 1)
        ektmp = Pool(h.sb, "c_ektm", [128, 512], F32, 1)
        eqtp = Pool(h.sb, "c_eqt", [128, 512], F32, 1)
        ektp = Pool(h.sb, "c_ekt", [128, 512], F32, 1)
        kinvp = Pool(h.sb, "c_kinv", [128, 512], BF16, 2)
        qdecp = Pool(h.sb, "c_qdec", [128, 512], BF16, 2)
        kinvTp = Pool(h.sb, "c_kinvT", [128, 512], BF16, 2)
        scTp = Pool(h.sb, "c_scT", [128, 512], BF16, 2)
        osbp = Pool(h.sb, "c_osb", [128, 1024], F32, 2)
        px = h.ps("c_px", [128, 512])
        pG = px
        pGT = h.ps("c_pGT", [128, 512])
        psc = h.ps("c_psc", [128, 512])
        po = h.ps("c_po", [128, 1024])
        pkv = h.ps("c_pkv", [128, 1024])

        def tile(kind, d, i, cnt):
            QT, KT, Kt, Vt = (S.QT, S.KT, S.Kt, S.Vt) if kind == 0 else (S.RQT, S.RKT, S.RKt, S.RVt)
            bQT, bKT, bKt, bVt = (B.QT, B.KT, B.Kt, B.Vt) if kind == 0 else (B.RQT, B.RKT, B.RKt, B.RVt)
            O = (S.OG if kind == 0 else S.OR)[d]
            bO = getattr(B, ("OG" if kind == 0 else "OR") + ("f" if d == 0 else "b"))
            ts_ = slice(i * 128, (i + 1) * 128)
            qT = qTp.next()
            kT = kTp.next()
            kt = ktp.next()
            vt = vtp.next()
            gl = [G.ld1, G.ld2, G.ld3][cnt % 3]
            h.LD("sp", qT, qT[:].rearrange("p (h c) -> p h c", h=4), QT.rearrange("(h p) t -> p h t", p=128)[:, :, ts_], gl, [bQT])
            h.LD("sp", kT, kT[:].rearrange("p (h c) -> p h c", h=4), KT.rearrange("(h p) t -> p h t", p=128)[:, :, ts_], gl, [bKT])
            h.LD("sp", kt, kt[:], Kt[ts_, :], gl, [bKt])
            h.LD("sp", vt, vt[:], Vt[ts_, :], gl, [bVt])
            if kind == 0:
                lr = lrp.next()
                h.LD("sp", lr, lr[:], S.LRT[d * 16:(d + 1) * 16, ts_], gl, [B.LRT])
                h.MM(px, px[:], lr[:], wa2[:, d, :], [lr, wa2], start=True, stop=False)
                h.MM(px, px[:], ones[:], ba[:, d, :], [ones, ba], start=False, stop=True)
                e1 = e1p.next()
                h.ACT(e1, e1[:], px[:], AF.Exp, [px], scale=-1.0)
                sp = spp.next()
                h.ACT(sp, sp[:], e1[:], AF.Ln, [e1], bias=1.0)
                h.MM(pG, pG[:], tri[d][:], sp[:], [tri[d], sp])
                for hh in range(4):
                    h.MM(pGT, pGT[:, hh * 128:(hh + 1) * 128], sp[:, hh * 128:(hh + 1) * 128], tri[d][:], [tri[d], sp])
                EkTM = ektmp.next()
                h.ACT(EkTM, EkTM[:], pG[:], AF.Exp, [pG], scale=1.0 / 16)
                EqT = eqtp.next()
                h.ACT(EqT, EqT[:], pGT[:], AF.Exp, [pGT], scale=-1.0 / 16)
                EkT = ektp.next()
                h.ACT(EkT, EkT[:], pGT[:], AF.Exp, [pGT], scale=1.0 / 16)
                lastc = 127 if d == 0 else 0
                dec = [EqT[:, hh * 128 + lastc:hh * 128 + lastc + 1] for hh in range(4)]
                dec_t = EqT
            else:
                EqT, EkT, EkTM = rtab[d]
                dec = [rdec[d][:, hh:hh + 1] for hh in range(4)]
                dec_t = rdec[d]
            kinv = kinvp.next()
            h.TT("dve", kinv, kinv[:], kt[:], EkTM[:], ALU.mult, [kt, EkTM])
            qdec = qdecp.next()
            h.STT("dve", qdec, qdec[:], qT[:], SCALE, EqT[:], ALU.mult, ALU.mult, [qT, EqT])
            kinvT = kinvTp.next()
            h.TT("dve", kinvT, kinvT[:], kT[:], EkT[:], ALU.mult, [kT, EkT])
            for hh in range(4):
                hs = slice(hh * 128, (hh + 1) * 128)
                h.MM(psc, psc[:, hs], kinvT[:, hs], qdec[:, hs], [kinvT, qdec])
            scT = scTp.next()
            h.TT("dve", scT, scT[:], psc[:], maskT[d][:], ALU.mult, [psc, maskT[d]])
            sbf = Sb[kind, d]
            sf = Sf[kind, d]
            for hh in range(4):
                hs = slice(hh * 128, (hh + 1) * 128)
                vs = slice(hh * 256, (hh + 1) * 256)
                h.MM(po, po[:, vs], scT[:, hs], vt[:, vs], [scT, vt], start=True, stop=False)
                h.MM(po, po[:, vs], qdec[:, hs], sbf[:, vs], [qdec, sbf], start=False, stop=True)
            osb = osbp.next()
            h.CP("act", osb, osb[:], po[:], [po])
            h.ST("sp", O[ts_, :], osb, osb[:], G.st0 if d == 0 else G.st1, bO)
            for hh in range(4):
                hs = slice(hh * 128, (hh + 1) * 128)
                vs = slice(hh * 256, (hh + 1) * 256)
                h.MM(pkv, pkv[:, vs], kinv[:, hs], vt[:, vs], [kinv, vt])
            h.TT("dve", sf, sf[:], sf[:], pkv[:], ALU.add, [sf, pkv])
            for hh in range(4):
                vs = slice(hh * 256, (hh + 1) * 256)
                h.TS("dve", sf, sf[:, vs], sf[:, vs], dec[hh], None, ALU.mult, None, [sf, dec_t])
            h.CP("act", sbf, sbf[:], sf[:], [sf])

        fwd = list(range(NT))
        bwd = [1, 0] + list(range(NT - 1, 1, -1))
        cnt = 0
        import os
        kinds = [int(x) for x in os.environ.get("C1KINDS", "0,1").split(",")]
        nsteps = int(os.environ.get("C1N", NT))
        gen = side(h) if side is not None else None
        for s in range(nsteps):
            for kind in kinds:
                tile(kind, 0, fwd[s], cnt)
                cnt += 1
                tile(kind, 1, bwd[s], cnt)
                cnt += 1
            if gen is not None:
                for _ in range(SIDE_PER_STEP):
                    next(gen, None)
        if gen is not None:
            for _ in gen:
                pass
        P.end_phase(f"c1{li}")


def rev_ap(ap2d, c0, n):
    a = ap2d[:, c0:c0 + n]
    return AP(a.tensor, a.offset + (n - 1) * a.ap[-1][0], [list(a.ap[0]), [-a.ap[-1][0], n]])


def c2_body(k, li, h):
    I, S, B, G, P = k.I, k.S, k.B, k.G, k.P
    convw = h.sb("l_convw", [128, 8, 5])
    lruw = h.sb("l_w", [128, 32, 256], BF16)
    lruv = h.sb("l_v", [128, 48])
    c8 = h.sb("l_c8", [128, 32])
    tmpv = h.sb("l_tmpv", [128, 16])
    h.LD("sp", convw, convw[:], I.convw[li], G.ld0)
    for q_ in range(8):
        h.LD("pool", lruw, lruw[:, q_ * 4:(q_ + 1) * 4, :], I.lruw[li, :, q_ * 4:(q_ + 1) * 4, :], G.w0)
    h.LD("sp", lruv, lruv[:], I.lruv[li], G.ld0)
    h.ACT(tmpv, tmpv[:], lruv[:, 32:48], AF.Exp, [lruv], scale=-1.0)
    h.ACT(tmpv, tmpv[:], tmpv[:], AF.Ln, [tmpv], bias=1.0)
    h.TS("dve", c8, c8[:, 0:16], tmpv[:], -8.0, None, ALU.mult, None, [tmpv])
    h.TS("dve", c8, c8[:, 16:32], tmpv[:], -16.0, None, ALU.mult, None, [tmpv])
    NPAD = TALL + 8
    xcb = [h.sb(f"l_xcb{j}", [128, TALL], BF16) for j in range(2)]
    hf = h.sb("l_hf", [128, NPAD])
    xc = h.sb("l_xc", [128, TALL])
    p7 = h.sb("l_p7", [128, TALL], BF16)
    TB = 256
    rp = Pool(h.sb, "l_r", [128, TB], F32, 2)
    ip = Pool(h.sb, "l_i", [128, TB], F32, 2)
    a2p = Pool(h.sb, "l_a2", [128, TB], F32, 2)
    ap_ = Pool(h.sb, "l_ab", [128, TB], F32, 2)
    bp_ = Pool(h.sb, "l_bb", [128, TB], F32, 2)
    pc2 = h.ps("l_pc2", [128, 2, TB])
    NB_ = TALL // TB
    CO, LO = 2, 261
    yield
    for g in range(4):
        for j in range(2):
            cc = 2 * g + j
            xp = hf
            h.MEMSET("dve", xp, xp[:, 0:2], 0.0)
            h.MEMSET("dve", xp, xp[:, 258:261], 0.0)
            h.MEMSET("dve", xp, xp[:, NPAD - 3:NPAD], 0.0)
            h.LD("sp", xp, xp[:, CO:CO + NCTX], S.P6T[cc * 128:(cc + 1) * 128, 0:NCTX], G.ld1, [B.P6T])
            h.LD("sp", xp, xp[:, LO:LO + NLAT], S.P6T[cc * 128:(cc + 1) * 128, NCTX:TALL], G.ld1, [B.P6T])
            for (o0, d0, n) in ((CO, 0, NCTX), (LO, NCTX, NLAT)):
                h.TS("dve", xc, xc[:, d0:d0 + n], xp[:, o0 - 2:o0 - 2 + n], convw[:, cc, 0:1], convw[:, cc, 4:5],
                     ALU.mult, ALU.add, [xp, convw])
                yield
                for tap in range(1, 4):
                    h.STT("dve", xc, xc[:, d0:d0 + n], xp[:, o0 - 2 + tap:o0 - 2 + tap + n], convw[:, cc, tap:tap + 1],
                          xc[:, d0:d0 + n], ALU.mult, ALU.add, [xp, convw, xc])
                    yield
            h.CP("act", xcb[j], xcb[j][:], xc[:], [xc])
            yield
        for j in range(2):
            cc = 2 * g + j
            hb = xc
            h.LD("sp", p7, p7[:], S.P7T[cc * 128:(cc + 1) * 128, :], G.ld2, [B.P7T])
            for d in range(2):
                order = list(range(NB_)) if d == 0 else [0] + list(range(NB_ - 1, 0, -1))
                for bi_ in order:
                    t0 = bi_ * TB
                    tn = TB
                    for gate in range(2):
                        for ic in range(2):
                            widx = ((d * 2 + gate) * 4 + g) * 2 + ic
                            h.MM(pc2, pc2[:, gate, :], lruw[:, widx, j * 128:(j + 1) * 128], xcb[ic][:, t0:t0 + tn], [lruw, xcb[ic]],
                                 start=(ic == 0), stop=(ic == 1))
                    r = rp.next()
                    ii = ip.next()
                    a2 = a2p.next()
                    ab = ap_.next()
                    bb = bp_.next()
                    vcol = d * 8 + cc
                    h.ACT(r, r[:], pc2[:, 0, :], AF.Sigmoid, [pc2, lruv], bias=lruv[:, vcol:vcol + 1])
                    h.ACT(ii, ii[:], pc2[:, 1, :], AF.Sigmoid, [pc2, lruv], bias=lruv[:, 16 + vcol:16 + vcol + 1])
                    h.ACT(ab, ab[:], r[:], AF.Exp, [r, c8], scale=c8[:, vcol:vcol + 1])
                    h.ACT(a2, a2[:], r[:], AF.Exp, [r, c8], scale=c8[:, 16 + vcol:16 + vcol + 1])
                    h.ACT(a2, a2[:], a2[:], AF.Sqrt, [a2], scale=-1.0, bias=1.0)
                    h.TT("dve", ii, ii[:], ii[:], xcb[j][:, t0:t0 + tn], ALU.mult, [ii, xcb[j]])
                    h.TT("dve", bb, bb[:], ii[:], a2[:], ALU.mult, [ii, a2])
                    if d == 0:
                        init = 0.0 if bi_ == 0 else hf[:, t0 - 1:t0]
                        P.op("dve", lambda e, ab=ab, bb=bb, init=init, t0=t0, tn=tn: e.tensor_tensor_scan(
                            out=hf[:, t0:t0 + tn], data0=ab[:], data1=bb[:], initial=init, op0=ALU.mult, op1=ALU.add),
                            reads=[ab, bb, hf], writes=[hf])
                    else:
                        if bi_ == 0:
                            init = 0.0
                        elif bi_ == NB_ - 1:
                            init = hb[:, 0:1]
                        else:
                            init = hb[:, t0 + tn:t0 + tn + 1]
                        P.op("dve", lambda e, ab=ab, bb=bb, init=init, t0=t0, tn=tn, hb=hb: e.tensor_tensor_scan(
                            out=rev_ap(hb[:], t0, tn), data0=rev_ap(ab[:], 0, tn), data1=rev_ap(bb[:], 0, tn), initial=init,
                            op0=ALU.mult, op1=ALU.add), reads=[ab, bb, hb], writes=[hb])
                    yield
            hfv = hf[:, 0:TALL]
            h.TT("dve", hf, hfv, hfv, hb[:], ALU.add, [hf, hb])
            yield
            gl = xc
            h.TT("dve", gl, gl[:], p7[:], p7[:], ALU.mult, [p7])
            h.TS("dve", gl, gl[:], gl[:], 0.044715, 1.0, ALU.mult, ALU.add, [gl])
            yield
            h.TT("dve", gl, gl[:], gl[:], p7[:], ALU.mult, [gl, p7])
            h.ACT(gl, gl[:], gl[:], AF.Sigmoid, [gl], scale=1.5957691216057308)
            yield
            h.TT("dve", gl, gl[:], gl[:], p7[:], ALU.mult, [gl, p7])
            h.TT("dve", p7, p7[:], gl[:], hfv, ALU.mult, [gl, hf])
            h.ST("sp", S.LRUT[cc * 128:(cc + 1) * 128, :], p7, p7[:], G.st0, B.LRUT)
            yield


def phase_d1(k, li, last):
    I, S, B, G, P = k.I, k.S, k.B, k.G, k.P
    t_first = 2 if last else 0
    with ExitStack() as st:
        h = mk_helpers(k, st)
        identf = h.sb("d_identf", [128, 128])
        identb = h.sb("d_identb", [128, 128], BF16)
        h.LD("sp", identf, identf[:], I.ident, G.ld0)
        h.CP("dve", identb, identb[:], identf[:], [identf])
        gn = h.sb("d_gn", [128, D])
        rn = h.sb("d_rn", [128, D])
        h.LD("sp", gn, gn[:], bcast_row(I.gla_norm[li]), G.ld0)
        h.LD("sp", rn, rn[:], bcast_row(I.ret_norm[li]), G.ld0)
        oa = Pool(h.sb, "d_oa", [128, D], F32, 3)
        obp = Pool(h.sb, "d_ob", [128, D], F32, 3)
        gp = Pool(h.sb, "d_g", [128, D], BF16, 3)
        sqp = Pool(h.sb, "d_sq", [128, D], F32, 2)
        stat = Pool(h.sb, "d_stat", [128, 16], F32, 3)
        nb = Pool(h.sb, "d_nb", [128, D], BF16, 2)
        oT = Pool(h.sb, "d_oT", [128, 8, 128], BF16, 3)
        ptr = Pool(h.ps, "d_ptr", [128, 8, 128], BF16, 3)

        def headnorm(o, gate_src, bsrc, normw, center, i, dst, bdst):
            stt = stat.next()
            sq = sqp.next()
            ov = o[:].rearrange("p (h e) -> p h e", h=4)
            if center:
                P.op("dve", lambda e: e.tensor_reduce(out=stt[:, 0:4], in_=ov, axis=AX.X, op=ALU.add), reads=[o], writes=[stt])
                h.TS("dve", stt, stt[:, 0:4], stt[:, 0:4], -1.0 / 256, None, ALU.mult, None, [stt])
                for hh in range(4):
                    h.TS("dve", o, o[:, hh * 256:(hh + 1) * 256], o[:, hh * 256:(hh + 1) * 256], stt[:, hh:hh + 1], None, ALU.add, None, [o, stt])
            h.TT("dve", sq, sq[:], o[:], o[:], ALU.mult, [o])
            P.op("dve", lambda e: e.tensor_reduce(out=stt[:, 4:8], in_=sq[:].rearrange("p (h e) -> p h e", h=4), axis=AX.X, op=ALU.add),
                 reads=[sq], writes=[stt])
            h.TS("dve", stt, stt[:, 8:12], stt[:, 4:8], 1.0 / 256, EPS, ALU.mult, ALU.add, [stt])
            h.ACT(stt, stt[:, 8:12], stt[:, 8:12], AF.Sqrt, [stt])
            h.RECIP(stt, stt[:, 12:16], stt[:, 8:12], [stt])
            g = gp.next()
            h.LD("sp", g, g[:], gate_src[i * 128:(i + 1) * 128, :], G.ld2, [bsrc])
            h.ACT(sq, sq[:], g[:], AF.Sigmoid, [g])
            h.TT("dve", sq, sq[:], sq[:], g[:], ALU.mult, [sq, g])
            h.TT("dve", sq, sq[:], sq[:], normw[:], ALU.mult, [sq, normw])
            n_ = nb.next()
            for hh in range(4):
                vs = slice(hh * 256, (hh + 1) * 256)
                h.STT("dve", n_, n_[:, vs], o[:, vs], stt[:, 12 + hh:13 + hh], sq[:, vs], ALU.mult, ALU.mult, [o, stt, sq])
            p = ptr.next()
            for kc in range(8):
                h.TR(p, p[:, kc, :], n_[:, kc * 128:(kc + 1) * 128], identb[:], [n_, identb])
            t_ = oT.next()
            h.CP("act", t_, t_[:], p[:], [p])
            h.ST("sp", dst.rearrange("(kc p) t -> p kc t", p=128)[:, :, i * 128:(i + 1) * 128], t_, t_[:], G.st1, bdst)

        for i in range(t_first, NT):
            o1 = oa.next()
            o2 = obp.next()
            h.LD("sp", o1, o1[:], S.OG[0][i * 128:(i + 1) * 128, :], G.ld1, [B.OGf])
            h.LD("sp", o2, o2[:], S.OG[1][i * 128:(i + 1) * 128, :], G.ld1, [B.OGb])
            h.TT("dve", o1, o1[:], o1[:], o2[:], ALU.add, [o1, o2])
            headnorm(o1, S.P3, B.P3, gn, False, i, S.GLAT, B.GLAT)
            o1 = oa.next()
            o2 = obp.next()
            h.LD("sp", o1, o1[:], S.OR[0][i * 128:(i + 1) * 128, :], G.ld3, [B.ORf])
            h.LD("sp", o2, o2[:], S.OR[1][i * 128:(i + 1) * 128, :], G.ld3, [B.ORb])
            h.TT("dve", o1, o1[:], o1[:], o2[:], ALU.add, [o1, o2])
            headnorm(o1, S.P11, B.P11, rn, True, i, S.RETT, B.RETT)
        P.end_phase(f"d1{li}")


def phase_d2(k, li, x_src, last):
    I, S, B, G, P = k.I, k.S, k.B, k.G, k.P
    t_first = 2 if last else 0
    with ExitStack() as st:
        h = mk_helpers(k, st)
        identf = h.sb("e_identf", [128, 128])
        identb = h.sb("e_identb", [128, 128], BF16)
        h.LD("sp", identf, identf[:], I.ident, G.ld0)
        h.CP("dve", identb, identb[:], identf[:], [identf])
        wbr = [h.sb(f"e_wbr{j}", [128, 8, D], BF16) for j in range(3)]
        wout = h.sb("e_wout", [128, 8, D], BF16)
        for j in range(3):
            h.LD("pool", wbr[j], wbr[j][:], I.w_branch[li, j].rearrange("(kc p) n -> p kc n", p=128), G.w0)
        h.LD("pool", wout, wout[:], I.w_out[li].rearrange("(kc p) n -> p kc n", p=128), G.w0)
        wr = h.sb("e_wr", [128, 8, 32])
        h.LD("sp", wr, wr[:], I.w_router[li].rearrange("(kc p) n -> p kc n", p=128), G.ld0)
        wrh = h.sb("e_wrh", [128, 8, 32], BF16)
        wrl = h.sb("e_wrl", [128, 8, 32], BF16)
        h.CP("dve", wrh, wrh[:], wr[:], [wr])
        h.TT("dve", wr, wr[:], wr[:], wrh[:], ALU.subtract, [wr, wrh])
        h.CP("dve", wrl, wrl[:], wr[:], [wr])
        brb = h.sb("e_brb", [128, 32])
        h.LD("sp", brb, brb[:], bcast_row(I.b_router[li]), G.ld0)
        bdf = h.sb("e_bdf", [32, D])
        bdh = h.sb("e_bdh", [32, D], BF16)
        bdl = h.sb("e_bdl", [32, D], BF16)
        h.LD("sp", bdf, bdf[:], I.b_down[li], G.ld0)
        h.CP("dve", bdh, bdh[:], bdf[:], [bdf])
        h.TT("dve", bdf, bdf[:], bdf[:], bdh[:], ALU.subtract, [bdf, bdh])
        h.CP("dve", bdl, bdl[:], bdf[:], [bdf])
        whl = Pool(h.sb, "e_whl", [128, 64], BF16, 2)
        wT = Pool(h.sb, "e_wT", [32, 2, 128], BF16, 2)
        a0p = Pool(h.sb, "e_a0", [128, D], F32, 2)
        G2, S2 = load_mod_tiles(h, k, li, I.norm2, 3, 4, "e")
        gate1 = []
        for v in range(2):
            g = h.sb(f"e_gate{v}", [128, D])
            h.LD("sp", g, g[:], bcast_row(S.modv[v, 2 * D:3 * D]), G.ld0, [B.modv])
            gate1.append(g)
        srcT = [Pool(h.sb, f"e_src{j}_", [128, 8, 512], BF16, 1) for j in range(3)]
        p12p = Pool(h.sb, "e_p12", [128, 3, 512], BF16, 2)
        sg = Pool(h.sb, "e_sg", [128, 512], F32, 3)
        mrg = Pool(h.sb, "e_mrg", [128, 512], F32, 2)
        mT = h.sb("e_mT", [128, 8, 512], BF16)
        stat = Pool(h.sb, "e_stat", [128, 16], F32, 3)
        xp = Pool(h.sb, "e_x", [128, D], F32, 2)
        tmp = h.sb("e_tmp", [128, D])
        xn2 = h.sb("e_xn2", [128, D])
        xh = h.sb("e_xh", [128, D], BF16)
        xl = h.sb("e_xl", [128, D], BF16)
        xn2Tb = Pool(h.sb, "e_xn2Tb", [128, 8, 128], BF16, 2)
        xlTp = Pool(h.sb, "e_xlT", [128, 8, 128], BF16, 2)
        lg = Pool(h.sb, "e_lg", [128, 32], F32, 2)
        m8 = Pool(h.sb, "e_m8", [128, 8], F32, 2)
        wro = Pool(h.sb, "e_wro", [128, 64], F32, 2)
        ptr = Pool(h.ps, "e_ptr", [128, 8, 128], BF16, 2)
        plg = h.ps("e_plg", [128, 512])
        pbr = Pool(h.ps, "e_pbr", [128, 512], F32, 3)
        pout = h.ps("e_pout", [128, D])
        srcD = [(S.GLAT, B.GLAT), (S.LRUT, B.LRUT), (S.RETT, B.RETT)]
        groups = []
        t = t_first
        while t < NT:
            n = min(4, NT - t)
            groups.append((t, n))
            t += n
        import os
        d2stop = float(os.environ.get("D2STOP", 99))
        if d2stop <= 1:
            P.end_phase(f"d2{li}")
            return
        for (g0, gn_) in groups:
            ntok = gn_ * 128
            c0 = g0 * 128
            srcs = []
            for j in range(3):
                t_ = srcT[j].next()
                h.LD("sp", t_, t_[:, :, 0:ntok], srcD[j][0].rearrange("(kc p) t -> p kc t", p=128)[:, :, c0:c0 + ntok], G.ld3, [srcD[j][1]])
                srcs.append(t_)
            for dc in range(8):
                m = mrg.next()
                p12 = p12p.next()
                h.LD("sp", p12, p12[:, :, 0:ntok], S.P12T.rearrange("(j r) t -> r j t", j=3)[dc * 128:(dc + 1) * 128, :, c0:c0 + ntok], G.ld2, [B.P12T])
                for j in range(3):
                    pb = pbr.next()
                    for kc in range(8):
                        h.MM(pb, pb[:, 0:ntok], wbr[j][:, kc, dc * 128:(dc + 1) * 128], srcs[j][:, kc, 0:ntok], [wbr[j], srcs[j]],
                             start=(kc == 0), stop=(kc == 7))
                    s_ = sg.next()
                    h.ACT(s_, s_[:, 0:ntok], p12[:, j, 0:ntok], AF.Sigmoid, [p12])
                    if j == 0:
                        h.TT("dve", m, m[:, 0:ntok], pb[:, 0:ntok], s_[:, 0:ntok], ALU.mult, [pb, s_])
                    else:
                        h.TT("dve", s_, s_[:, 0:ntok], pb[:, 0:ntok], s_[:, 0:ntok], ALU.mult, [pb, s_])
                        if j == 1:
                            h.TT("dve", m, m[:, 0:ntok], m[:, 0:ntok], s_[:, 0:ntok], ALU.add, [m, s_])
                        else:
                            h.TT("dve", mT, mT[:, dc, 0:ntok], m[:, 0:ntok], s_[:, 0:ntok], ALU.add, [m, s_])
            if d2stop <= 2:
                P.end_phase(f"d2{li}")
                return
            for tl in range(gn_):
                i = g0 + tl
                v = 1 if i < 2 else 0
                for half in range(2):
                    for kc in range(8):
                        h.MM(pout, pout[:, half * 512:(half + 1) * 512], mT[:, kc, tl * 128:(tl + 1) * 128],
                             wout[:, kc, half * 512:(half + 1) * 512], [mT, wout], start=(kc == 0), stop=(kc == 7))
                xt = xp.next()
                h.LD("sp", xt, xt[:], x_src[i * 128:(i + 1) * 128, :], G.ld1, [B.X] if li > 0 else [])
                h.TT("dve", tmp, tmp[:], pout[:], gate1[v][:], ALU.mult, [pout, gate1[v]])
                h.TT("dve", xt, xt[:], xt[:], tmp[:], ALU.add, [xt, tmp])
                h.ST("sp", S.X[i * 128:(i + 1) * 128, :], xt, xt[:], G.st0, B.X)
                if d2stop <= 3:
                    P.end_phase(f"d2{li}")
                    return
                rms_mod_tile(h, k, xt, G2[v], S2[v], xn2, xn2[:], tmp, stat.next())
                if d2stop <= 3.2:
                    P.end_phase(f"d2{li}")
                    return
                xb = xn2Tb.next()
                xlT = xlTp.next()
                h.CP("act", xh, xh[:], xn2[:], [xn2])
                h.TT("dve", xl, xl[:], xn2[:], xh[:], ALU.subtract, [xn2, xh])
                for src_, dst_ in ((xh, xb), (xl, xlT)):
                    p = ptr.next()
                    for kc in range(8):
                        h.TR(p, p[:, kc, :], src_[:, kc * 128:(kc + 1) * 128], identb[:], [src_, identb])
                    h.CP("act" if src_ is xh else "dve", dst_, dst_[:], p[:], [p])
                if d2stop <= 3.5:
                    P.end_phase(f"d2{li}")
                    return
                h.ST("sp", S.XN2T.rearrange("(kc p) t -> p kc t", p=128)[:, :, i * 128:(i + 1) * 128], xb, xb[:], G.st1, B.XN2T)
                if d2stop <= 4:
                    P.end_phase(f"d2{li}")
                    return
                nmm = 0
                for (a_, w_t) in ((xb, wrh), (xlT, wrh), (xb, wrl)):
                    for kc in range(8):
                        h.MM(plg, plg[:, 0:32], a_[:, kc, :], w_t[:, kc, :], [a_, w_t], start=(nmm == 0), stop=(nmm == 23))
                        nmm += 1
                l_ = lg.next()
                h.TT("dve", l_, l_[:], plg[:, 0:32], brb[:], ALU.add, [plg, brb])
                m_ = m8.next()
                P.op("dve", lambda e, m_=m_, l_=l_: e.max(out=m_[:], in_=l_[:]), reads=[l_], writes=[m_])
                w_ = wro.next()
                stt = stat.next()
                h.TS("dve", w_, w_[:, 32:64], l_[:], m_[:, 3:4], None, ALU.is_ge, None, [l_, m_])
                h.TS("dve", stt, stt[:, 0:1], m_[:, 0:1], -1.0, None, ALU.mult, None, [m_])
                h.ACT(l_, l_[:], l_[:], AF.Exp, [l_, stt], bias=stt[:, 0:1])
                h.TT("dve", l_, l_[:], l_[:], w_[:, 32:64], ALU.mult, [l_, w_])
                P.op("dve", lambda e, stt=stt, l_=l_: e.tensor_reduce(out=stt[:, 1:2], in_=l_[:], axis=AX.X, op=ALU.add), reads=[l_], writes=[stt])
                h.RECIP(stt, stt[:, 2:3], stt[:, 1:2], [stt])
                h.TS("dve", w_, w_[:, 0:32], l_[:], stt[:, 2:3], None, ALU.mult, None, [l_, stt])
                h.ST("sp", S.WR[i * 128:(i + 1) * 128, :], w_, w_[:], G.st2, B.WR)
                wh_ = whl.next()
                h.CP("dve", wh_, wh_[:, 0:32], w_[:, 0:32], [w_])
                h.TT("dve", wh_, wh_[:, 32:64], w_[:, 0:32], wh_[:, 0:32], ALU.subtract, [w_, wh_])
                pw = ptr.next()
                h.TR(pw, pw[0:32, 0, :], wh_[:, 0:32], identb[:], [wh_, identb])
                h.TR(pw, pw[0:32, 1, :], wh_[:, 32:64], identb[:], [wh_, identb])
                wT_ = wT.next()
                h.CP("act", wT_, wT_[:], pw[0:32, 0:2, :], [pw])
                for half in range(2):
                    hs_ = slice(half * 512, (half + 1) * 512)
                    h.MM(pout, pout[:, hs_], wT_[:, 0, :], bdh[:, hs_], [wT_, bdh], start=True, stop=False)
                    h.MM(pout, pout[:, hs_], wT_[:, 1, :], bdh[:, hs_], [wT_, bdh], start=False, stop=False)
                    h.MM(pout, pout[:, hs_], wT_[:, 0, :], bdl[:, hs_], [wT_, bdl], start=False, stop=True)
                a0 = a0p.next()
                h.CP("act", a0, a0[:], pout[:], [pout])
                h.ST("sp", S.ACC0[i * 128:(i + 1) * 128, :], a0, a0[:], G.st2, B.ACC0)
        P.end_phase(f"d2{li}")


def phase_f(k, li, last):
    I, S, B, G, P = k.I, k.S, k.B, k.G, k.P
    out = k.out
    t_first = 2 if last else 0
    tiles = list(range(t_first, NT))
    if last:
        groups = [tiles[j:j + 8] for j in range(0, 32, 8)]
    else:
        groups = [tiles[0:9], tiles[9:18], tiles[18:26], tiles[26:34]]
    with ExitStack() as st:
        h = mk_helpers(k, st)
        bgu = h.sb("f_bgu", [128, 32 * 16])
        h.LD("sp", bgu, bgu[:], I.bgu[li], G.ld0)
        gate2 = []
        for v in range(2):
            g = h.sb(f"f_gate{v}", [128, D])
            h.LD("sp", g, g[:], bcast_row(S.modv[v, 5 * D:6 * D]), G.ld0, [B.modv])
            gate2.append(g)
        fnb = None
        if last:
            fnb = h.sb("f_fnb", [128, D])
            h.LD("sp", fnb, fnb[:], bcast_row(I.final_norm), G.ld0)
        wgu = Pool(h.sb, "f_wgu", [128, 8, 1024], BF16, 3)
        wdn = Pool(h.sb, "f_wdn", [128, 4, D], BF16, 3)
        xT = h.sb("f_xT", [128, 8, 9 * 128], BF16)
        acc = h.sb("f_acc", [128, 9, D])
        wr = h.sb("f_wr", [128, 9, 64])
        gcp = Pool(h.sb, "f_gc", [128, 512], F32, 2)
        ucp = Pool(h.sb, "f_uc", [128, 512], F32, 2)
        sip = Pool(h.sb, "f_si", [128, 512], F32, 2)
        actT = Pool(h.sb, "f_actT", [128, 4, 512], BF16, 2)
        xo = Pool(h.sb, "f_xo", [128, D], F32, 2)
        tmp = h.sb("f_tmp", [128, D])
        stat = Pool(h.sb, "f_stat", [128, 4], F32, 2)
        pg = Pool(h.ps, "f_pg", [128, 512], F32, 2)
        pu = Pool(h.ps, "f_pu", [128, 512], F32, 2)
        pd = Pool(h.ps, "f_pd", [128, D], F32, 2)
        wgi = [0]
        for grp in groups:
            ng = len(grp)
            c0 = grp[0] * 128
            ntok = ng * 128
            h.LD("sp", xT, xT[:, :, 0:ntok], S.XN2T.rearrange("(kc p) t -> p kc t", p=128)[:, :, c0:c0 + ntok], G.ld1, [B.XN2T])
            h.LD("sp", wr, wr[:, 0:ng, :], S.WR[c0:c0 + ntok, :].rearrange("(n p) e -> p n e", p=128), G.ld1, [B.WR])
            h.LD("sp", acc, acc[:, 0:ng, :], S.ACC0[c0:c0 + ntok, :].rearrange("(n p) d -> p n d", p=128), G.ld1, [B.ACC0])
            nsub = (ng + 3) // 4
            subs = []
            s0_ = 0
            for q_ in range(nsub):
                sn_ = ng // nsub + (1 if q_ < ng % nsub else 0)
                subs.append((s0_, sn_))
                s0_ += sn_
            def emit_gu(e, hf_, wg_, s0, sn):
                tn = sn * 128
                tc0 = s0 * 128
                aT = actT.next()
                for fl in range(4):
                    fc = hf_ * 4 + fl
                    pg_ = pg.next()
                    pu_ = pu.next()
                    for kc in range(8):
                        h.MM(pg_, pg_[:, 0:tn], wg_[:, kc, fl * 128:(fl + 1) * 128], xT[:, kc, tc0:tc0 + tn], [wg_, xT],
                             start=(kc == 0), stop=(kc == 7))
                    for kc in range(8):
                        h.MM(pu_, pu_[:, 0:tn], wg_[:, kc, 512 + fl * 128:512 + (fl + 1) * 128], xT[:, kc, tc0:tc0 + tn], [wg_, xT],
                             start=(kc == 0), stop=(kc == 7))
                    gc = gcp.next()
                    uc = ucp.next()
                    si = sip.next()
                    bcol = e * 16 + fc
                    h.TS("dve", gc, gc[:, 0:tn], pg_[:, 0:tn], bgu[:, bcol:bcol + 1], 7.0, ALU.add, ALU.min, [pg_, bgu])
                    h.ACT(uc, uc[:, 0:tn], pu_[:, 0:tn], AF.Identity, [pu_, bgu], bias=bgu[:, bcol + 8:bcol + 9])
                    h.ACT(si, si[:, 0:tn], gc[:, 0:tn], AF.Sigmoid, [gc], scale=1.702)
                    h.TS("dve", uc, uc[:, 0:tn], uc[:, 0:tn], 7.0, -7.0, ALU.min, ALU.max, [uc])
                    h.TT("dve", gc, gc[:, 0:tn], gc[:, 0:tn], si[:, 0:tn], ALU.mult, [gc, si])
                    h.STT("dve", aT, aT[:, fl, 0:tn], uc[:, 0:tn], 1.0, gc[:, 0:tn], ALU.add, ALU.mult, [uc, gc])
                return aT

            def emit_down(e, wd_, aT, s0, sn):
                for tl in range(sn):
                    ti = s0 + tl
                    pd_ = pd.next()
                    for half in range(2):
                        for fl in range(4):
                            h.MM(pd_, pd_[:, half * 512:(half + 1) * 512], aT[:, fl, tl * 128:(tl + 1) * 128],
                                 wd_[:, fl, half * 512:(half + 1) * 512], [aT, wd_], start=(fl == 0), stop=(fl == 3))
                    h.STT("dve", acc, acc[:, ti, :], pd_[:], wr[:, ti, e:e + 1], acc[:, ti, :], ALU.mult, ALU.add, [pd_, wr, acc])

            pending = None
            for e in range(32):
                for hf_ in range(2):
                    wg_ = wgu.next()
                    wd_ = wdn.next()
                    src = I.w_gu[li, e].rearrange("(kc p) n -> p kc n", p=128)
                    h.LD("pool", wg_, wg_[:, :, 0:512], src[:, :, hf_ * 512:(hf_ + 1) * 512], None)
                    h.LD("pool", wg_, wg_[:, :, 512:1024], src[:, :, 1024 + hf_ * 512:1024 + (hf_ + 1) * 512], None)
                    h.LD("pool", wd_, wd_[:], I.w_down[li, e, hf_ * 512:(hf_ + 1) * 512, :].rearrange("(fc p) n -> p fc n", p=128), None)
                    for (s0, sn) in subs:
                        aT = emit_gu(e, hf_, wg_, s0, sn)
                        if pending is not None:
                            emit_down(*pending)
                        pending = (e, wd_, aT, s0, sn)
            emit_down(*pending)
            for tl in range(ng):
                i = grp[tl]
                v = 1 if i < 2 else 0
                xt = xo.next()
                h.LD("sp", xt, xt[:], S.X[i * 128:(i + 1) * 128, :], G.ld2, [B.X])
                h.TT("dve", tmp, tmp[:], acc[:, tl, :], gate2[v][:], ALU.mult, [acc, gate2[v]])
                h.TT("dve", xt, xt[:], xt[:], tmp[:], ALU.add, [xt, tmp])
                if not last:
                    h.ST("sp", S.X[i * 128:(i + 1) * 128, :], xt, xt[:], G.st0, B.X)
                else:
                    stt = stat.next()
                    h.ACT(tmp, tmp[:], xt[:], AF.Square, [xt], accum=stt[:, 0:1], extra_w=[stt])
                    h.TS("dve", stt, stt[:, 1:2], stt[:, 0:1], 1.0 / D, EPS, ALU.mult, ALU.add, [stt])
                    h.ACT(stt, stt[:, 2:3], stt[:, 1:2], AF.Sqrt, [stt])
                    h.RECIP(stt, stt[:, 3:4], stt[:, 2:3], [stt])
                    h.STT("dve", xt, xt[:], xt[:], stt[:, 3:4], fnb[:], ALU.mult, ALU.mult, [xt, stt, fnb])
                    h.ST("sp", out[(i - 2) * 128:(i - 1) * 128, :], xt, xt[:], G.out, B.out)
        P.end_phase(f"f{li}")


def host_constants():
    f32 = np.float32
    c = {}
    c["ident"] = np.eye(128, dtype=f32)
    s = np.arange(128)[:, None]
    cc = np.arange(128)[None, :]
    c["tri"] = np.stack([(s <= cc), (s >= cc)]).astype(f32)
    c["maskT"] = np.stack([np.tile((s <= cc), (1, 4)), np.tile((s > cc), (1, 4))]).astype(f32)
    rows = NLAT // 64
    row = np.repeat(np.arange(rows), 64).astype(np.float64)
    col = np.tile(np.arange(64), rows).astype(np.float64)
    nf = 32
    inv = (10000.0 ** (-np.arange(nf, dtype=np.float32) / np.float32(nf))).astype(np.float32)
    ang = np.concatenate([row[:, None].astype(f32) * inv, col[:, None].astype(f32) * inv], axis=-1).astype(f32)
    cos = np.concatenate([np.ones((NCTX, 64), f32), np.cos(ang).astype(f32)], 0)
    sin = np.concatenate([np.zeros((NCTX, 64), f32), np.sin(ang).astype(f32)], 0)
    cos128 = np.concatenate([cos, cos], 1)
    sin128 = np.concatenate([-sin, sin], 1)
    c["cosT"] = np.ascontiguousarray(cos128.T)
    c["sinT"] = np.ascontiguousarray(sin128.T)
    c["cos2"] = np.ascontiguousarray(np.tile(cos128, (1, 4)))
    c["sin2"] = np.ascontiguousarray(np.tile(sin128, (1, 4)))
    gamma = 1.0 - 2.0 ** (-5.0 - np.arange(4, dtype=np.float64))
    lg = np.log(gamma)
    pos = np.arange(128, dtype=np.float64)
    rtab = np.zeros((2, 3, 128, 512), f32)
    rdec = np.zeros((2, 128, 4), f32)
    for d in range(2):
        steps = (pos + 1) if d == 0 else (128 - pos)
        for hh in range(4):
            G_ = steps * lg[hh]
            rtab[d, 0, :, hh * 128:(hh + 1) * 128] = np.exp(G_)[None, :]
            rtab[d, 1, :, hh * 128:(hh + 1) * 128] = np.exp(-G_)[None, :]
            rtab[d, 2, :, hh * 128:(hh + 1) * 128] = np.exp(-G_)[:, None]
            rdec[d, :, hh] = np.exp(128 * lg[hh])
    c["rtab"] = rtab
    c["rdec"] = rdec
    return c


def host_weights(inp):
    f32 = np.float32
    w = {}
    w_in = inp["w_in"]
    rq = w_in[:, :, OFF["rq"]:OFF["rq"] + 512].reshape(2, D, 4, 2, 64)[:, :, :, ::-1, :].reshape(2, D, 512)
    rk = w_in[:, :, OFF["rk"]:OFF["rk"] + 512].reshape(2, D, 4, 2, 64)[:, :, :, ::-1, :].reshape(2, D, 512)
    w["w_in_p"] = np.ascontiguousarray(np.concatenate([w_in, rq, rk], axis=2))
    for n in ["w_ada", "b_ada", "norm1", "norm2", "final_norm", "gla_wa2", "gla_ba", "gla_norm", "ret_norm", "w_branch",
              "w_out", "w_router", "b_router", "w_down", "b_down"]:
        w[n] = np.ascontiguousarray(inp[n])
    cw = np.concatenate([inp["lru_conv_w"], inp["lru_conv_b"][:, None, :]], axis=1)
    w["convw"] = np.ascontiguousarray(cw.reshape(2, 5, 8, 128).transpose(0, 3, 2, 1))
    lw = np.stack([inp["lru_wa"], inp["lru_wi"]], axis=2)
    lw = lw.reshape(2, 2, 2, 4, 2, 128, 256).transpose(0, 5, 1, 2, 3, 4, 6)
    w["lruw"] = np.ascontiguousarray(lw.reshape(2, 128, 32, 256))
    lv = np.stack([inp["lru_ba"], inp["lru_bi"], inp["lru_lam"]], axis=1)
    lv = lv.reshape(2, 3, 2, 8, 128).transpose(0, 4, 1, 2, 3)
    w["lruv"] = np.ascontiguousarray(lv.reshape(2, 128, 48))
    wg = inp["w_gu"].reshape(2, 32, D, 1024, 2).transpose(0, 1, 2, 4, 3)
    w["w_gu_d"] = np.ascontiguousarray(wg.reshape(2, 32, D, 2048))
    bg = inp["b_gu"].reshape(2, 32, 8, 128, 2).transpose(0, 3, 1, 4, 2)
    w["bgu"] = np.ascontiguousarray(bg.reshape(2, 128, 32 * 16))
    return w


_CACHE = {}


def kernel(**inputs):
    inp = {k_: np.asarray(v) for k_, v in inputs.items()}
    if "prog" not in _CACHE:
        _CACHE["prog"] = build_program()
    nc, k = _CACHE["prog"]
    consts = host_constants()
    wts = host_weights(inp)
    in_maps = []
    for b in range(8):
        m = dict(consts)
        m.update(wts)
        m["xall"] = np.ascontiguousarray(np.concatenate([inp["ctx"][b], inp["x"][b]], axis=0))
        cv = np.concatenate([inp["c"][b].reshape(8, 128).T, inp["c_ctx"].reshape(8, 128).T], axis=1)
        m["cvec"] = np.ascontiguousarray(cv.astype(np.float32))
        in_maps.append(m)
    res = run_bass_kernel_spmd(nc, in_maps, core_ids=list(range(8)))
    return np.stack([np.asarray(r["out"]) for r in res.results], axis=0).astype(np.float32)
```

```python
import numpy as np
import ml_dtypes
from contextlib import ExitStack
import concourse.bass as bass
import concourse.mybir as mybir
from concourse.ap import AP
from concourse.bass_utils import run_bass_kernel_spmd

F32 = mybir.dt.float32
BF16 = mybir.dt.bfloat16
AF = mybir.ActivationFunctionType
ALU = mybir.AluOpType
AX = mybir.AxisListType

SEM_EPOCH = 30000
SIDE_PER_STEP = 12
D = 1024
NCTX = 256
NLAT = 4096
TALL = NCTX + NLAT
NT = TALL // 128
EPS = 1e-6
NCOLP = 11296 + 1024
OFF = dict(q=0, k=512, v=1024, p3=2048, lrf=3072, lrb=3088, p6=3104, p7=4128, rq=5152, rk=5664, rv=6176,
           p11=7200, p12=8224, rqs=11296, rks=11808)


class Buf:
    __slots__ = ("name", "last_w", "readers")

    def __init__(self, name):
        self.name = name
        self.last_w = None
        self.readers = []


class DmaGroup:
    def __init__(self, sem, name):
        self.sem = sem
        self.count = 0
        self.name = name


class Op:
    __slots__ = ("eng", "fn", "waits", "signal", "idx", "semval", "dma_group")

    def __init__(self, eng, fn, idx):
        self.eng = eng
        self.fn = fn
        self.idx = idx
        self.waits = []
        self.signal = False
        self.semval = None
        self.dma_group = None


class TT_:
    def __init__(self, h, name):
        self.h = h
        self.b = Buf(name)
        self.dsem = None

    def __getitem__(self, k):
        return self.h[k]


class Prog:
    ENGS = ("pe", "act", "dve", "pool", "sp")

    def __init__(self, nc, gstack):
        self.nc = nc
        self.gstack = gstack
        self.base = {e: 0 for e in self.ENGS}
        self.ops = {e: [] for e in self.ENGS}
        self.waited_ops = {e: {x: -1 for x in self.ENGS} for e in self.ENGS}
        self.waited_dma = {e: {} for e in self.ENGS}
        self.groups = []
        self.nsem = 0
        self.cur_sem = {e: None for e in self.ENGS}
        self.cur_cnt = {e: 0 for e in self.ENGS}
        self.bufs = []
        self.ninst = 0
        self.free_dsems = {}
        self.used_dsems = []
        self.gen = 0
        self.eng_sems = {}

    def tile_sem(self, t, queue="sp"):
        if t.dsem is None or getattr(t, "dsem_gen", -1) != self.gen:
            t.dsem_gen = self.gen
            fl = self.free_dsems.setdefault(queue, [])
            if fl:
                t.dsem = fl.pop()
            else:
                t.dsem = DmaGroup(self.new_sem(f"d{self.nsem}"), f"d{self.nsem}")
                t.dsem.queue = queue
            self.used_dsems.append(t.dsem)
        assert t.dsem.queue == queue, "tile DMA'd from two queue types"
        return t.dsem

    def new_sem(self, name):
        self.nsem += 1
        return self.gstack.enter_context(self.nc.semaphore(name))

    def group(self, name):
        g = DmaGroup(self.new_sem("g_" + name), name)
        self.groups.append(g)
        return g

    def buf(self, name):
        b = Buf(name)
        self.bufs.append(b)
        return b

    def _add_dep(self, op, tok):
        if tok is None:
            return
        E = op.eng
        if tok[0] == "op":
            x = tok[1]
            if x.eng == E and E == "pe":
                return
            if self.waited_ops[E][x.eng] >= x.idx:
                return
            self.waited_ops[E][x.eng] = x.idx
            x.signal = True
            op.waits.append(tok)
        else:
            _, g, cnt, gen = tok
            if gen != self.gen:
                return
            if self.waited_dma[E].get(g, 0) >= cnt:
                return
            self.waited_dma[E][g] = cnt
            op.waits.append(tok)

    def _record(self, eng, fn, reads, writes, dma_group=None):
        op = Op(eng, fn, self.base[eng] + len(self.ops[eng]))
        self.ops[eng].append(op)
        for b in reads:
            self._add_dep(op, b.last_w)
        for b in writes:
            self._add_dep(op, b.last_w)
            for r in b.readers:
                self._add_dep(op, r)
        if dma_group is not None:
            dma_group.count += 1
            op.dma_group = dma_group
            tok = ("dma", dma_group, dma_group.count, self.gen)
        else:
            tok = ("op", op)
        for b in writes:
            b.last_w = tok
            b.readers = []
        for b in reads:
            if b not in writes:
                b.readers.append(tok)
        return op

    def op(self, eng, fn, reads=(), writes=()):
        rd = [r.b if isinstance(r, TT_) else r for r in reads if not getattr(r, "is_psum", False)]
        wr = [w.b if isinstance(w, TT_) else w for w in writes]
        wr += [r.b for r in reads if getattr(r, "is_psum", False) and r.b not in wr]
        return self._record(eng, fn, rd, wr)

    def dma(self, queue, out, in_, tile, reads=(), writes=(), **kw):
        def fn(e):
            return e.dma_start(out=out, in_=in_, **kw)
        return self._record(queue, fn, [r.b if isinstance(r, TT_) else r for r in reads],
                            [w.b if isinstance(w, TT_) else w for w in writes], dma_group=self.tile_sem(tile, queue))

    def _simulate(self, name):
        if not hasattr(self, "simvals"):
            self.simvals = {}
        vals = self.simvals
        pc = {e: 0 for e in self.ENGS}
        progress = True
        while progress:
            progress = False
            for e in self.ENGS:
                ops = self.ops[e]
                while pc[e] < len(ops):
                    op = ops[pc[e]]
                    ok = True
                    for w in op.waits:
                        if w[0] == "op":
                            s_, v = w[1].semval
                            if vals.get(id(s_), 0) < v:
                                ok = False
                        else:
                            if vals.get(id(w[1]), 0) < 16 * w[2]:
                                ok = False
                    if not ok:
                        break
                    if op.dma_group is not None:
                        vals[id(op.dma_group)] = vals.get(id(op.dma_group), 0) + 16
                    elif op.signal:
                        vals[id(op.semval[0])] = vals.get(id(op.semval[0]), 0) + 1
                        assert vals[id(op.semval[0])] == op.semval[1], (name, e, pc[e])
                    pc[e] += 1
                    progress = True
        for e in self.ENGS:
            if pc[e] < len(self.ops[e]):
                op = self.ops[e][pc[e]]
                desc = []
                for w in op.waits:
                    if w[0] == "op":
                        desc.append(("op", w[1].eng, w[1].idx, w[1].semval[1], vals.get(id(w[1].semval[0]), 0)))
                    else:
                        desc.append(("dma", w[1].name, 16 * w[2], vals.get(id(w[1]), 0)))
                raise RuntimeError(f"DEADLOCK in phase {name}: engine {e} stuck at op {pc[e]}/{len(self.ops[e])} waits={desc}")

    def end_phase(self, name):
        nc = self.nc
        fin = Op("sp", lambda e: e.nop(), self.base["sp"] + len(self.ops["sp"]))
        for g in self.used_dsems:
            if g.count > self.waited_dma["sp"].get(g, 0):
                fin.waits.append(("dma", g, g.count, self.gen))
                self.waited_dma["sp"][g] = g.count
        self.ops["sp"].append(fin)
        self.phase_eng_sems = []
        for e in self.ENGS:
            epoch = 0
            cnt = 0
            sem = None
            for op in self.ops[e]:
                if op.signal and op.dma_group is None:
                    if sem is None or cnt >= SEM_EPOCH:
                        lst = self.eng_sems.setdefault(e, [])
                        if epoch >= len(lst):
                            lst.append(self.new_sem(f"e_{e}_{epoch}"))
                        sem = lst[epoch]
                        epoch += 1
                        cnt = 0
                        self.phase_eng_sems.append(sem)
                    cnt += 1
                    op.semval = (sem, cnt)
        self._simulate(name)
        with nc.Block() as block:
            def run(e, handle):
                for op in self.ops[e]:
                    for w in op.waits:
                        if w[0] == "op":
                            s, v = w[1].semval
                            handle.wait_ge(s, v)
                        else:
                            handle.wait_ge(w[1].sem, 16 * w[2])
                    ins = op.fn(handle)
                    self.ninst += 1
                    if op.dma_group is not None:
                        ins.then_inc(op.dma_group.sem, 16)
                    elif op.signal:
                        ins.then_inc(op.semval[0], 1)

            @block.tensor
            def _(h):
                run("pe", h)

            @block.scalar
            def _(h):
                run("act", h)

            @block.vector
            def _(h):
                run("dve", h)

            @block.gpsimd
            def _(h):
                run("pool", h)

            @block.sync
            def _(h):
                run("sp", h)
        used_eng_sems = list(self.phase_eng_sems)
        dsems = list(self.used_dsems)
        with nc.Block() as block2:
            @block2.sync
            def _(h):
                for g in dsems:
                    if g.queue != "pool":
                        h.sem_clear(g.sem)
                for s_ in used_eng_sems:
                    h.sem_clear(s_)
        if hasattr(self, "simvals"):
            self.simvals = {}
        for e in self.ENGS:
            self.base[e] += len(self.ops[e])
            self.ops[e] = []
            self.cur_cnt[e] = 0
        for e in self.ENGS:
            for x in self.ENGS:
                self.waited_ops[e][x] = self.base[x] - 1
            self.waited_dma[e] = {}
        for b in self.bufs:
            b.last_w = None
            b.readers = []
        for g in dsems:
            assert 16 * g.count < 60000, (g.name, g.count)
            if g.queue != "pool":
                g.count = 0
                self.free_dsems.setdefault(g.queue, []).append(g)
        self.used_dsems = []
        self.gen += 1


class Pool:
    def __init__(self, alloc, name, shape, dt, n):
        self.items = [TT_(alloc(f"{name}{i}", shape, dt), f"{name}{i}") for i in range(n)]
        self.i = 0

    def next(self):
        t = self.items[self.i % len(self.items)]
        self.i += 1
        return t


class K:
    pass


def build_program(debug_outs=(), stop_after=None, n_layers=2, n_exp=32):
    nc = bass.Bass("TRN2", target_bir_lowering=False)
    k = K()
    k.nc = nc

    def din(name, shape, dt=F32):
        return nc.dram_tensor(name, list(shape), dt, kind="ExternalInput").ap()

    def dscr(name, shape, dt=F32):
        kind = "ExternalOutput" if name in debug_outs else "Internal"
        return nc.dram_tensor(name, list(shape), dt, kind=kind).ap()

    I = K()
    I.xall = din("xall", [TALL, D])
    I.cvec = din("cvec", [128, 16])
    I.w_ada = din("w_ada", [2, D, 6 * D])
    I.b_ada = din("b_ada", [2, 6 * D])
    I.norm1 = din("norm1", [2, D])
    I.norm2 = din("norm2", [2, D])
    I.final_norm = din("final_norm", [D])
    I.w_in = din("w_in_p", [2, D, NCOLP])
    I.gla_wa2 = din("gla_wa2", [2, 2, 16, 512])
    I.gla_ba = din("gla_ba", [2, 2, 512])
    I.gla_norm = din("gla_norm", [2, D])
    I.ret_norm = din("ret_norm", [2, D])
    I.convw = din("convw", [2, 128, 8, 5])
    I.lruw = din("lruw", [2, 128, 32, 256])
    I.lruv = din("lruv", [2, 128, 48])
    I.w_branch = din("w_branch", [2, 3, D, D])
    I.w_out = din("w_out", [2, D, D])
    I.w_router = din("w_router", [2, D, 32])
    I.b_router = din("b_router", [2, 32])
    I.w_gu = din("w_gu_d", [2, n_exp, D, 2048])
    I.bgu = din("bgu", [2, 128, 32 * 16])
    I.w_down = din("w_down", [2, n_exp, D, D])
    I.b_down = din("b_down", [2, 32, D])
    I.ident = din("ident", [128, 128])
    I.tri = din("tri", [2, 128, 128])
    I.maskT = din("maskT", [2, 128, 512])
    I.cosT = din("cosT", [128, TALL])
    I.sinT = din("sinT", [128, TALL])
    I.cos2 = din("cos2", [TALL, 512])
    I.sin2 = din("sin2", [TALL, 512])
    I.rtab = din("rtab", [2, 3, 128, 512])
    I.rdec = din("rdec", [2, 128, 4])
    out = nc.dram_tensor("out", [NLAT, D], F32, kind="ExternalOutput").ap()

    S = K()
    S.modv = dscr("modv", [2, 6 * D])
    S.X = dscr("X", [TALL, D])
    S.QT = dscr("QT", [512, TALL], BF16)
    S.KT = dscr("KT", [512, TALL], BF16)
    S.LRT = dscr("LRT", [32, TALL], BF16)
    S.P6T = dscr("P6T", [D, TALL], F32)
    S.P7T = dscr("P7T", [D, TALL], BF16)
    S.RQT = dscr("RQT", [512, TALL], BF16)
    S.RKT = dscr("RKT", [512, TALL], BF16)
    S.P12T = dscr("P12T", [3 * D, TALL], BF16)
    S.Kt = dscr("Kt", [TALL, 512], BF16)
    S.Vt = dscr("Vt", [TALL, D], BF16)
    S.P3 = dscr("P3", [TALL, D], BF16)
    S.RKt = dscr("RKt", [TALL, 512], BF16)
    S.RVt = dscr("RVt", [TALL, D], BF16)
    S.P11 = dscr("P11", [TALL, D], BF16)
    S.OG = [dscr("OGf", [TALL, D]), dscr("OGb", [TALL, D])]
    S.OR = [dscr("ORf", [TALL, D]), dscr("ORb", [TALL, D])]
    S.LRUT = dscr("LRUT", [D, TALL], BF16)
    S.XN2T = dscr("XN2T", [D, TALL], BF16)
    S.ACC0 = dscr("ACC0", [TALL, D])
    S.GLAT = dscr("GLAT", [D, TALL], BF16)
    S.RETT = dscr("RETT", [D, TALL], BF16)
    S.WR = dscr("WR", [TALL, 64])
    S.XNT = dscr("XNT", [D, TALL], BF16) if "XNT" in debug_outs else None
    S.XNT2 = dscr("XNT2", [D, TALL], BF16) if "XNT2" in debug_outs else None
    k.debug_outs = debug_outs

    with ExitStack() as gst:
        P = Prog(nc, gst)
        k.P = P
        B = K()
        for n in ["modv", "X", "QT", "KT", "LRT", "P6T", "P7T", "RQT", "RKT", "P12T", "Kt", "Vt", "P3", "RKt", "RVt",
                  "P11", "OGf", "OGb", "ORf", "ORb", "LRUT", "XN2T", "WR", "out", "GLAT", "RETT", "ACC0"]:
            setattr(B, n, P.buf(n))
        G = K()
        for n in ["ld0", "ld1", "ld2", "ld3", "w0", "w1", "w2", "st0", "st1", "st2", "st3", "out"]:
            setattr(G, n, None)
        k.I, k.S, k.B, k.G, k.out = I, S, B, G, out

        k.stop_after = stop_after
        for li in range(n_layers):
            last = li == 1
            x_src = I.xall if li == 0 else S.X
            seq = [("ada", lambda: phase_ada(k, li)), ("ab", lambda: phase_ab(k, li, x_src)), ("c1", lambda: phase_c1(k, li, side=lambda h_: c2_body(k, li, h_))), ("d1", lambda: phase_d1(k, li, last)), ("d2", lambda: phase_d2(k, li, x_src, last)),
                   ("f", lambda: phase_f(k, li, last))]
            done = False
            for name, fn in seq:
                fn()
                if stop_after == (name, li) or (name == "ab" and stop_after == ("a", li)):
                    done = True
                    break
            if done:
                break
    k.ninst = P.ninst
    return nc, k


def mk_helpers(k, st):
    nc, P = k.nc, k.P

    def uniq(name):
        k.uid = getattr(k, "uid", 0) + 1
        return f"{name}_u{k.uid}"

    def sb(name, shape, dt=F32):
        return TT_(st.enter_context(nc.sbuf_tensor(uniq(name), list(shape), dt)), name)

    def ps(name, shape, dt=F32):
        t = TT_(st.enter_context(nc.psum_tensor(uniq(name), list(shape), dt)), name)
        t.is_psum = True
        return t

    def sb_raw(name, shape, dt=F32):
        return st.enter_context(nc.sbuf_tensor(name, list(shape), dt))

    def MM(out, out_ap, lhsT, rhs, rd, start=True, stop=True):
        P.op("pe", lambda e: e.matmul(out_ap, lhsT=lhsT, rhs=rhs, start=start, stop=stop), reads=rd, writes=[out])

    def TR(out, out_ap, in_ap, ident_ap, rd):
        P.op("pe", lambda e: e.transpose(out=out_ap, in_=in_ap, identity=ident_ap), reads=rd, writes=[out])

    def ACT(out, out_ap, in_ap, func, rd, bias=None, scale=None, accum=None, extra_w=()):
        kw = {}
        if bias is not None:
            kw["bias"] = bias
        if scale is not None:
            kw["scale"] = scale
        if accum is not None:
            kw["accum_out"] = accum
        P.op("act", lambda e: e.activation(out=out_ap, in_=in_ap, func=func, **kw), reads=rd, writes=[out] + list(extra_w))

    def TT(eng, out, out_ap, in0, in1, op, rd):
        P.op(eng, lambda e: e.tensor_tensor(out=out_ap, in0=in0, in1=in1, op=op), reads=rd, writes=[out])

    def TS(eng, out, out_ap, in0, s1, s2, op0, op1, rd, accum=None, extra_w=()):
        if op1 is None:
            P.op(eng, lambda e: e.tensor_scalar(out=out_ap, in0=in0, scalar1=s1, scalar2=None, op0=op0), reads=rd, writes=[out])
        elif accum is not None:
            P.op(eng, lambda e: e.tensor_scalar(out=out_ap, in0=in0, scalar1=s1, scalar2=s2, op0=op0, op1=op1, accum_out=accum),
                 reads=rd, writes=[out] + list(extra_w))
        else:
            P.op(eng, lambda e: e.tensor_scalar(out=out_ap, in0=in0, scalar1=s1, scalar2=s2, op0=op0, op1=op1), reads=rd, writes=[out])

    def STT(eng, out, out_ap, in0, scalar, in1, op0, op1, rd):
        P.op(eng, lambda e: e.scalar_tensor_tensor(out=out_ap, in0=in0, scalar=scalar, in1=in1, op0=op0, op1=op1),
             reads=rd, writes=[out])

    def CP(eng, out, out_ap, in_ap, rd):
        if eng == "act":
            P.op("act", lambda e: e.copy(out=out_ap, in_=in_ap), reads=rd, writes=[out])
        else:
            P.op(eng, lambda e: e.tensor_copy(out=out_ap, in_=in_ap), reads=rd, writes=[out])

    def RECIP(out, out_ap, in_ap, rd):
        P.op("dve", lambda e: e.reciprocal(out=out_ap, in_=in_ap), reads=rd, writes=[out])

    def MEMSET(eng, out, out_ap, val):
        P.op(eng, lambda e: e.memset(out_ap, val), reads=[], writes=[out])

    def LD(queue, dst, dst_ap, src_ap, group, src_bufs=(), **kw):
        P.dma(queue, dst_ap, src_ap, dst, reads=list(src_bufs), writes=[dst], **kw)

    def ST(queue, dst_ap, src, src_ap, group, dst_buf, **kw):
        P.dma(queue, dst_ap, src_ap, src, reads=[src], writes=[dst_buf], **kw)

    h = K()
    for n, f in list(locals().items()):
        if callable(f) and n not in ("h",):
            setattr(h, n, f)
    return h


def bcast_row(ap_row, n=128):
    return ap_row.partition_broadcast(n)


def phase_ada(k, li):
    I, S, B, G, P = k.I, k.S, k.B, k.G, k.P
    with ExitStack() as st:
        h = mk_helpers(k, st)
        cv = h.sb("ada_cv", [128, 16])
        sc = h.sb("ada_sc", [128, 16])
        sg = h.sb("ada_sg", [128, 16])
        brow = h.sb("ada_brow", [1, 6 * D])
        rows = [h.sb(f"ada_row{v}", [1, 6 * D]) for v in range(2)]
        wpool = Pool(h.sb, "ada_w", [128, 8, 512], F32, 2)
        pm = [Pool(h.ps, f"ada_pm{v}_", [1, 512], F32, 2) for v in range(2)]
        h.LD("sp", cv, cv[:], I.cvec, G.ld0)
        h.LD("sp", brow, brow[:], I.b_ada[li:li + 1, :], G.ld0)
        h.ACT(sg, sg[:], cv[:], AF.Sigmoid, [cv])
        h.TT("dve", sc, sc[:], cv[:], sg[:], ALU.mult, [cv, sg])
        wv = I.w_ada[li].rearrange("(kc p) n -> p kc n", p=128)
        for j in range(12):
            w = wpool.next()
            h.LD("sp", w, w[:], wv[:, :, j * 512:(j + 1) * 512], G.w0)
            for v in range(2):
                p = pm[v].next()
                for kc in range(8):
                    h.MM(p, p[:], sc[:, v * 8 + kc:v * 8 + kc + 1], w[:, kc, :], [sc, w], start=(kc == 0), stop=(kc == 7))
                h.TT("dve", rows[v], rows[v][:, j * 512:(j + 1) * 512], p[:], brow[:, j * 512:(j + 1) * 512], ALU.add, [p, brow])
        for v in range(2):
            h.ST("sp", S.modv[v:v + 1, :], rows[v], rows[v][:], G.st0, B.modv)
        P.end_phase(f"ada{li}")


def rms_mod_tile(h, k, xt, G_, S_, out_t, out_ap, tmp, stat):
    h.ACT(tmp, tmp[:], xt[:], AF.Square, [xt], accum=stat[:, 0:1], extra_w=[stat])
    h.TS("dve", stat, stat[:, 1:2], stat[:, 0:1], 1.0 / D, EPS, ALU.mult, ALU.add, [stat])
    h.ACT(stat, stat[:, 2:3], stat[:, 1:2], AF.Sqrt, [stat])
    h.RECIP(stat, stat[:, 3:4], stat[:, 2:3], [stat])
    h.STT("dve", tmp, tmp[:], xt[:], stat[:, 3:4], G_[:], ALU.mult, ALU.mult, [xt, stat, G_])
    h.TT("dve", out_t, out_ap, tmp[:], S_[:], ALU.add, [tmp, S_])


def load_mod_tiles(h, k, li, normw, idx_shift, idx_scale, names):
    I, S, B, G = k.I, k.S, k.B, k.G
    nb = h.sb(names + "_nb", [128, D])
    h.LD("sp", nb, nb[:], bcast_row(normw[li]), G.ld0)
    Gs, Ss = [], []
    for v in range(2):
        g = h.sb(f"{names}_G{v}", [128, D])
        s = h.sb(f"{names}_S{v}", [128, D])
        h.LD("sp", g, g[:], bcast_row(S.modv[v, idx_scale * D:(idx_scale + 1) * D]), G.ld0, [B.modv])
        h.LD("sp", s, s[:], bcast_row(S.modv[v, idx_shift * D:(idx_shift + 1) * D]), G.ld0, [B.modv])
        h.STT("dve", g, g[:], g[:], 1.0, nb[:], ALU.add, ALU.mult, [g, nb])
        Gs.append(g)
        Ss.append(s)
    return Gs, Ss


def phase_ab(k, li, x_src):
    I, S, B, G, P = k.I, k.S, k.B, k.G, k.P
    with ExitStack() as st:
        h = mk_helpers(k, st)
        xnT = h.sb("xnT", [128, 8, TALL], BF16)
        identf = h.sb("identf", [128, 128])
        identb = h.sb("identb", [128, 128], BF16)
        h.LD("sp", identf, identf[:], I.ident, G.ld0)
        h.CP("dve", identb, identb[:], identf[:], [identf])
        with ExitStack() as st2:
            h2 = mk_helpers(k, st2)
            Gs, Ss = load_mod_tiles(h2, k, li, I.norm1, 0, 1, "a")
            xpool = Pool(h2.sb, "a_x", [128, D], F32, 2)
            tmp = h2.sb("a_tmp", [128, D])
            xnp = Pool(h2.sb, "a_xn", [128, D], BF16, 2)
            stat = Pool(h2.sb, "a_stat", [128, 4], F32, 2)
            ptr = Pool(h2.ps, "a_ptr", [128, 8, 128], BF16, 2)
            for i in range(NT):
                v = 1 if i < 2 else 0
                xt = xpool.next()
                h2.LD("sp", xt, xt[:], x_src[i * 128:(i + 1) * 128, :], G.ld1, [B.X] if li > 0 else [])
                xn = xnp.next()
                rms_mod_tile(h2, k, xt, Gs[v], Ss[v], xn, xn[:], tmp, stat.next())
                p = ptr.next()
                for kc in range(8):
                    h2.TR(p, p[:, kc, :], xn[:, kc * 128:(kc + 1) * 128], identb[:], [xn, identb])
                h2.CP("act" if i % 2 == 0 else "dve", xnT, xnT[:, :, i * 128:(i + 1) * 128], p[:], [p])
            if S.XNT is not None:
                h2.ST("sp", S.XNT.rearrange("(kc p) t -> p kc t", p=128), xnT, xnT[:], G.st3, P.buf("XNT"))
            P.end_phase(f"a{li}")
        if k.stop_after == ("a", li):
            return
        with ExitStack() as st2:
            h2 = mk_helpers(k, st2)
            cosT = h2.sb("b_cosT", [128, TALL])
            sinT = h2.sb("b_sinT", [128, TALL])
            h2.LD("sp", cosT, cosT[:], I.cosT, G.ld0)
            h2.LD("sp", sinT, sinT[:], I.sinT, G.ld0)
            wpool = Pool(h2.sb, "b_w", [128, 8, 512], BF16, 3)
            stage_bf = Pool(h2.sb, "b_stb", [128, TALL], BF16, 2)
            stage_f = Pool(h2.sb, "b_stf", [128, TALL], F32, 1)
            stage_tm = Pool(h2.sb, "b_sttm", [128, 512], BF16, 3)
            t1p = Pool(h2.sb, "b_t1", [128, 512], F32, 2)
            t2p = Pool(h2.sb, "b_t2", [128, 512], F32, 2)
            c2p = Pool(h2.sb, "b_c2", [128, 512], F32, 2)
            s2p = Pool(h2.sb, "b_s2", [128, 512], F32, 2)
            pp = Pool(h2.ps, "b_p", [128, 512], F32, 6)
            wv = I.w_in[li].rearrange("(kc p) n -> p kc n", p=128)
            wgi = [0]

            def load_w(c0, ncol):
                w = wpool.next()
                g = [G.w0, G.w1, G.w2][wgi[0] % 3]
                wgi[0] += 1
                h2.LD("pool", w, w[:, :, 0:ncol], wv[:, :, c0:c0 + ncol], g)
                return w

            tbs = [(t0, min(512, TALL - t0)) for t0 in range(0, TALL, 512)]
            evi = [0]

            def ev_eng():
                evi[0] += 1
                return "act" if evi[0] % 2 == 0 else "dve"

            def fm_job(c0, ncol, dst, dst_buf, f32=False, swap_c0=None):
                for s0 in range(0, ncol, 512):
                    nc_ = min(512, ncol - s0)
                    w = load_w(c0 + s0, nc_)
                    ws = load_w(swap_c0 + s0, nc_) if swap_c0 is not None else None
                    for sub in range(0, nc_, 128):
                        m = min(128, nc_ - sub)
                        stg = (stage_f if f32 else stage_bf).next()
                        for (t0, tn) in tbs:
                            p = pp.next()
                            for kc in range(8):
                                h2.MM(p, p[0:m, 0:tn], w[:, kc, sub:sub + m], xnT[:, kc, t0:t0 + tn], [w, xnT],
                                      start=(kc == 0), stop=(kc == 7))
                            if ws is None:
                                h2.CP(ev_eng(), stg, stg[0:m, t0:t0 + tn], p[0:m, 0:tn], [p])
                            else:
                                p2 = pp.next()
                                for kc in range(8):
                                    h2.MM(p2, p2[0:m, 0:tn], ws[:, kc, sub:sub + m], xnT[:, kc, t0:t0 + tn], [ws, xnT],
                                          start=(kc == 0), stop=(kc == 7))
                                t1 = t1p.next()
                                t2 = t2p.next()
                                h2.TT("dve", t1, t1[:, 0:tn], p[:, 0:tn], cosT[:, t0:t0 + tn], ALU.mult, [p, cosT])
                                h2.TT("dve", t2, t2[:, 0:tn], p2[:, 0:tn], sinT[:, t0:t0 + tn], ALU.mult, [p2, sinT])
                                h2.TT("dve", stg, stg[:, t0:t0 + tn], t1[:, 0:tn], t2[:, 0:tn], ALU.add, [t1, t2])
                        r0 = s0 + sub
                        h2.ST("sp", dst[r0:r0 + m, :], stg, stg[0:m, :], G.st0, dst_buf)

            def tm_job(c0, ncol, dst, dst_buf, dcol0=0, rope=False):
                for s0 in range(0, ncol, 512):
                    w = load_w(c0 + s0, 512)
                    for i in range(NT):
                        p = pp.next()
                        for kc in range(8):
                            h2.MM(p, p[:], xnT[:, kc, i * 128:(i + 1) * 128], w[:, kc, :], [w, xnT], start=(kc == 0), stop=(kc == 7))
                        stg = stage_tm.next()
                        if not rope:
                            h2.CP(ev_eng(), stg, stg[:], p[:], [p])
                        else:
                            c2 = c2p.next()
                            s2 = s2p.next()
                            h2.LD("sp", c2, c2[:], I.cos2[i * 128:(i + 1) * 128, :], G.ld2)
                            h2.LD("sp", s2, s2[:], I.sin2[i * 128:(i + 1) * 128, :], G.ld2)
                            t1 = t1p.next()
                            t2 = t2p.next()
                            h2.TT("dve", t1, t1[:], p[:], c2[:], ALU.mult, [p, c2])
                            pv = p[:].rearrange("p (h two s) -> p h two s", h=4, two=2)
                            t2v = t2[:].rearrange("p (h two s) -> p h two s", h=4, two=2)
                            s2v = s2[:].rearrange("p (h two s) -> p h two s", h=4, two=2)
                            h2.TT("dve", t2, t2v[:, :, 0, :], pv[:, :, 1, :], s2v[:, :, 0, :], ALU.mult, [p, s2])
                            h2.TT("dve", t2, t2v[:, :, 1, :], pv[:, :, 0, :], s2v[:, :, 1, :], ALU.mult, [p, s2])
                            h2.TT("dve", stg, stg[:], t1[:], t2[:], ALU.add, [t1, t2])
                        h2.ST("sp", dst[i * 128:(i + 1) * 128, dcol0 + s0:dcol0 + s0 + 512], stg, stg[:], G.st1, dst_buf)

            import os
            sel = os.environ.get("BJOBS")
            sel = sel.split(",") if sel else None
            jobs = [
                ("QT", lambda: fm_job(OFF["q"], 512, S.QT, B.QT)),
                ("KT", lambda: fm_job(OFF["k"], 512, S.KT, B.KT)),
                ("LRT", lambda: fm_job(OFF["lrf"], 32, S.LRT, B.LRT)),
                ("Kt", lambda: tm_job(OFF["k"], 512, S.Kt, B.Kt)),
                ("Vt", lambda: tm_job(OFF["v"], 1024, S.Vt, B.Vt)),
                ("RQT", lambda: fm_job(OFF["rq"], 512, S.RQT, B.RQT, swap_c0=OFF["rqs"])),
                ("RKT", lambda: fm_job(OFF["rk"], 512, S.RKT, B.RKT, swap_c0=OFF["rks"])),
                ("RKt", lambda: tm_job(OFF["rk"], 512, S.RKt, B.RKt, rope=True)),
                ("RVt", lambda: tm_job(OFF["rv"], 1024, S.RVt, B.RVt)),
                ("P6T", lambda: fm_job(OFF["p6"], 1024, S.P6T, B.P6T, f32=True)),
                ("P7T", lambda: fm_job(OFF["p7"], 1024, S.P7T, B.P7T)),
                ("P3", lambda: tm_job(OFF["p3"], 1024, S.P3, B.P3)),
                ("P11", lambda: tm_job(OFF["p11"], 1024, S.P11, B.P11)),
                ("P12T", lambda: fm_job(OFF["p12"], 3072, S.P12T, B.P12T)),
            ]
            for jn, jf in jobs:
                if sel is None or jn in sel:
                    jf()
            if S.XNT2 is not None:
                h2.ST("sp", S.XNT2.rearrange("(kc p) t -> p kc t", p=128), xnT, xnT[:], G.st3, P.buf("XNT2"))
            P.end_phase(f"b{li}")


def phase_c1(k, li, side=None):
    I, S, B, G, P = k.I, k.S, k.B, k.G, k.P
    SCALE = 128.0 ** -0.5
    with ExitStack() as st:
        h = mk_helpers(k, st)
        tri = [h.sb(f"c_tri{d}", [128, 128]) for d in range(2)]
        maskT = [h.sb(f"c_mask{d}", [128, 512]) for d in range(2)]
        rtab = [[h.sb(f"c_rtab{d}{j}", [128, 512]) for j in range(3)] for d in range(2)]
        rdec = [h.sb(f"c_rdec{d}", [128, 4]) for d in range(2)]
        for d in range(2):
            h.LD("sp", tri[d], tri[d][:], I.tri[d], G.ld0)
            h.LD("sp", maskT[d], maskT[d][:], I.maskT[d], G.ld0)
            h.LD("sp", rdec[d], rdec[d][:], I.rdec[d], G.ld0)
            for j in range(3):
                h.LD("sp", rtab[d][j], rtab[d][j][:], I.rtab[d, j], G.ld0)
        wa2 = h.sb("c_wa2", [16, 2, 512], BF16)
        ba = h.sb("c_ba", [1, 2, 512], BF16)
        ones = h.sb("c_ones", [1, 128], BF16)
        wa2f = h.sb("c_wa2f", [16, 2, 512])
        baf = h.sb("c_baf", [1, 2, 512])
        for d_ in range(2):
            h.LD("sp", wa2f, wa2f[:, d_, :], I.gla_wa2[li, d_], G.w0)
            h.LD("sp", baf, baf[:, d_, :], I.gla_ba[li, d_:d_ + 1, :], G.w0)
        h.CP("dve", wa2, wa2[:], wa2f[:], [wa2f])
        h.CP("dve", ba, ba[:], baf[:], [baf])
        h.MEMSET("dve", ones, ones[:], 1.0)
        Sf = {}
        Sb = {}
        for kind in range(2):
            for d in range(2):
                Sf[kind, d] = h.sb(f"c_S{kind}{d}", [128, 1024])
                Sb[kind, d] = h.sb(f"c_Sb{kind}{d}", [128, 1024], BF16)
                h.MEMSET("dve", Sf[kind, d], Sf[kind, d][:], 0.0)
                h.MEMSET("dve", Sb[kind, d], Sb[kind, d][:], 0.0)
        NB = 3
        qTp = Pool(h.sb, "c_qT", [128, 512], BF16, NB)
        kTp = Pool(h.sb, "c_kT", [128, 512], BF16, NB)
        ktp = Pool(h.sb, "c_kt", [128, 512], BF16, NB)
        vtp = Pool(h.sb, "c_vt", [128, 1024], BF16, NB)
        lrp = Pool(h.sb, "c_lr", [16, 128], BF16, NB)
        e1p = Pool(h.sb, "c_e1", [128, 512], F32, 1)
        spp = Pool(h.sb, "c_sp", [128, 512], F32, 1)
        ektmp = Pool(h.sb, "c_ektm", [128, 512], F32, 1)
        eqtp = Pool(h.sb, "c_eqt", [128, 512], F32, 1)
        ektp = Pool(h.sb, "c_ekt", [128, 512], F32, 1)
        kinvp = Pool(h.sb, "c_kinv", [128, 512], BF16, 2)
        qdecp = Pool(h.sb, "c_qdec", [128, 512], BF16, 2)
        kinvTp = Pool(h.sb, "c_kinvT", [128, 512], BF16, 2)
        scTp = Pool(h.sb, "c_scT", [128, 512], BF16, 2)
        osbp = Pool(h.sb, "c_osb", [128, 1024], F32, 2)
        px = h.ps("c_px", [128, 512])
        pG = px
        pGT = h.ps("c_pGT", [128, 512])
        psc = h.ps("c_psc", [128, 512])
        po = h.ps("c_po", [128, 1024])
        pkv = h.ps("c_pkv", [128, 1024])

        def tile(kind, d, i, cnt):
            QT, KT, Kt, Vt = (S.QT, S.KT, S.Kt, S.Vt) if kind == 0 else (S.RQT, S.RKT, S.RKt, S.RVt)
            bQT, bKT, bKt, bVt = (B.QT, B.KT, B.Kt, B.Vt) if kind == 0 else (B.RQT, B.RKT, B.RKt, B.RVt)
            O = (S.OG if kind == 0 else S.OR)[d]
            bO = getattr(B, ("OG" if kind == 0 else "OR") + ("f" if d == 0 else "b"))
            ts_ = slice(i * 128, (i + 1) * 128)
            qT = qTp.next()
            kT = kTp.next()
            kt = ktp.next()
            vt = vtp.next()
            gl = [G.ld1, G.ld2, G.ld3][cnt % 3]
            h.LD("sp", qT, qT[:].rearrange("p (h c) -> p h c", h=4), QT.rearrange("(h p) t -> p h t", p=128)[:, :, ts_], gl, [bQT])
            h.LD("sp", kT, kT[:].rearrange("p (h c) -> p h c", h=4), KT.rearrange("(h p) t -> p h t", p=128)[:, :, ts_], gl, [bKT])
            h.LD("sp", kt, kt[:], Kt[ts_, :], gl, [bKt])
            h.LD("sp", vt, vt[:], Vt[ts_, :], gl, [bVt])
            yield
            if kind == 0:
                lr = lrp.next()
                h.LD("sp", lr, lr[:], S.LRT[d * 16:(d + 1) * 16, ts_], gl, [B.LRT])
                h.MM(px, px[:], lr[:], wa2[:, d, :], [lr, wa2], start=True, stop=False)
                h.MM(px, px[:], ones[:], ba[:, d, :], [ones, ba], start=False, stop=True)
                yield
                e1 = e1p.next()
                h.ACT(e1, e1[:], px[:], AF.Exp, [px], scale=-1.0)
                sp = spp.next()
                h.ACT(sp, sp[:], e1[:], AF.Ln, [e1], bias=1.0)
                yield
                h.MM(pG, pG[:], tri[d][:], sp[:], [tri[d], sp])
                for hh in range(4):
                    h.MM(pGT, pGT[:, hh * 128:(hh + 1) * 128], sp[:, hh * 128:(hh + 1) * 128], tri[d][:], [tri[d], sp])
                EkTM = ektmp.next()
                h.ACT(EkTM, EkTM[:], pG[:], AF.Exp, [pG], scale=1.0 / 16)
                EqT = eqtp.next()
                h.ACT(EqT, EqT[:], pGT[:], AF.Exp, [pGT], scale=-1.0 / 16)
                EkT = ektp.next()
                h.ACT(EkT, EkT[:], pGT[:], AF.Exp, [pGT], scale=1.0 / 16)
                yield
                lastc = 127 if d == 0 else 0
                dec = [EqT[:, hh * 128 + lastc:hh * 128 + lastc + 1] for hh in range(4)]
                dec_t = EqT
            else:
                EqT, EkT, EkTM = rtab[d]
                dec = [rdec[d][:, hh:hh + 1] for hh in range(4)]
                dec_t = rdec[d]
            kinv = kinvp.next()
            h.TT("dve", kinv, kinv[:], kt[:], EkTM[:], ALU.mult, [kt, EkTM])
            qdec = qdecp.next()
            h.STT("dve", qdec, qdec[:], qT[:], SCALE, EqT[:], ALU.mult, ALU.mult, [qT, EqT])
            kinvT = kinvTp.next()
            h.TT("dve", kinvT, kinvT[:], kT[:], EkT[:], ALU.mult, [kT, EkT])
            yield
            for hh in range(4):
                hs = slice(hh * 128, (hh + 1) * 128)
                h.MM(psc, psc[:, hs], kinvT[:, hs], qdec[:, hs], [kinvT, qdec])
            scT = scTp.next()
            h.TT("dve", scT, scT[:], psc[:], maskT[d][:], ALU.mult, [psc, maskT[d]])
            yield
            sbf = Sb[kind, d]
            sf = Sf[kind, d]
            for hh in range(4):
                hs = slice(hh * 128, (hh + 1) * 128)
                vs = slice(hh * 256, (hh + 1) * 256)
                h.MM(po, po[:, vs], scT[:, hs], vt[:, vs], [scT, vt], start=True, stop=False)
                h.MM(po, po[:, vs], qdec[:, hs], sbf[:, vs], [qdec, sbf], start=False, stop=True)
            osb = osbp.next()
            h.CP("act", osb, osb[:], po[:], [po])
            h.ST("sp", O[ts_, :], osb, osb[:], G.st0 if d == 0 else G.st1, bO)
            yield
            for hh in range(4):
                hs = slice(hh * 128, (hh + 1) * 128)
                vs = slice(hh * 256, (hh + 1) * 256)
                h.MM(pkv, pkv[:, vs], kinv[:, hs], vt[:, vs], [kinv, vt])
            h.TT("dve", sf, sf[:], sf[:], pkv[:], ALU.add, [sf, pkv])
            yield
            for hh in range(4):
                vs = slice(hh * 256, (hh + 1) * 256)
                h.TS("dve", sf, sf[:, vs], sf[:, vs], dec[hh], None, ALU.mult, None, [sf, dec_t])
            h.CP("act", sbf, sbf[:], sf[:], [sf])

        fwd = list(range(NT))
        bwd = [1, 0] + list(range(NT - 1, 1, -1))
        cnt = 0
        import os
        kinds = [int(x) for x in os.environ.get("C1KINDS", "0,1").split(",")]
        nsteps = int(os.environ.get("C1N", NT))
        gen = side(h) if side is not None else None
        def run_pair(gens):
            act_ = list(gens)
            while act_:
                nxt_ = []
                for g_ in act_:
                    try:
                        next(g_)
                        nxt_.append(g_)
                    except StopIteration:
                        pass
                act_ = nxt_

        for s in range(nsteps):
            run_pair([tile(kind, 0, fwd[s], cnt + kind) for kind in kinds])
            cnt += 2
            run_pair([tile(kind, 1, bwd[s], cnt + kind) for kind in kinds])
            cnt += 2
            if gen is not None:
                for _ in range(SIDE_PER_STEP):
                    next(gen, None)
        if gen is not None:
            for _ in gen:
                pass
        P.end_phase(f"c1{li}")


def rev_ap(ap2d, c0, n):
    a = ap2d[:, c0:c0 + n]
    return AP(a.tensor, a.offset + (n - 1) * a.ap[-1][0], [list(a.ap[0]), [-a.ap[-1][0], n]])


def c2_body(k, li, h):
    I, S, B, G, P = k.I, k.S, k.B, k.G, k.P
    convw = h.sb("l_convw", [128, 8, 5])
    lruw = h.sb("l_w", [128, 32, 256], BF16)
    lruv = h.sb("l_v", [128, 48])
    c8 = h.sb("l_c8", [128, 32])
    tmpv = h.sb("l_tmpv", [128, 16])
    h.LD("sp", convw, convw[:], I.convw[li], G.ld0)
    for q_ in range(8):
        h.LD("pool", lruw, lruw[:, q_ * 4:(q_ + 1) * 4, :], I.lruw[li, :, q_ * 4:(q_ + 1) * 4, :], G.w0)
    h.LD("sp", lruv, lruv[:], I.lruv[li], G.ld0)
    h.ACT(tmpv, tmpv[:], lruv[:, 32:48], AF.Exp, [lruv], scale=-1.0)
    h.ACT(tmpv, tmpv[:], tmpv[:], AF.Ln, [tmpv], bias=1.0)
    h.TS("dve", c8, c8[:, 0:16], tmpv[:], -8.0, None, ALU.mult, None, [tmpv])
    h.TS("dve", c8, c8[:, 16:32], tmpv[:], -16.0, None, ALU.mult, None, [tmpv])
    NPAD = TALL + 8
    xcb = [h.sb(f"l_xcb{j}", [128, TALL], BF16) for j in range(2)]
    hf = h.sb("l_hf", [128, NPAD])
    xc = h.sb("l_xc", [128, TALL])
    p7 = h.sb("l_p7", [128, TALL], BF16)
    TB = 256
    rp = Pool(h.sb, "l_r", [128, TB], F32, 2)
    ip = Pool(h.sb, "l_i", [128, TB], F32, 2)
    a2p = Pool(h.sb, "l_a2", [128, TB], F32, 2)
    ap_ = Pool(h.sb, "l_ab", [128, TB], F32, 2)
    bp_ = Pool(h.sb, "l_bb", [128, TB], F32, 2)
    pc2 = h.ps("l_pc2", [128, 2, TB])
    NB_ = TALL // TB
    CO, LO = 2, 261
    yield
    for g in range(4):
        for j in range(2):
            cc = 2 * g + j
            xp = hf
            h.MEMSET("dve", xp, xp[:, 0:2], 0.0)
            h.MEMSET("dve", xp, xp[:, 258:261], 0.0)
            h.MEMSET("dve", xp, xp[:, NPAD - 3:NPAD], 0.0)
            h.LD("sp", xp, xp[:, CO:CO + NCTX], S.P6T[cc * 128:(cc + 1) * 128, 0:NCTX], G.ld1, [B.P6T])
            h.LD("sp", xp, xp[:, LO:LO + NLAT], S.P6T[cc * 128:(cc + 1) * 128, NCTX:TALL], G.ld1, [B.P6T])
            for (o0, d0, n) in ((CO, 0, NCTX), (LO, NCTX, NLAT)):
                h.TS("dve", xc, xc[:, d0:d0 + n], xp[:, o0 - 2:o0 - 2 + n], convw[:, cc, 0:1], convw[:, cc, 4:5],
                     ALU.mult, ALU.add, [xp, convw])
                yield
                for tap in range(1, 4):
                    h.STT("dve", xc, xc[:, d0:d0 + n], xp[:, o0 - 2 + tap:o0 - 2 + tap + n], convw[:, cc, tap:tap + 1],
                          xc[:, d0:d0 + n], ALU.mult, ALU.add, [xp, convw, xc])
                    yield
            h.CP("act", xcb[j], xcb[j][:], xc[:], [xc])
            yield
        for j in range(2):
            cc = 2 * g + j
            hb = xc
            h.LD("sp", p7, p7[:], S.P7T[cc * 128:(cc + 1) * 128, :], G.ld2, [B.P7T])
            for d in range(2):
                order = list(range(NB_)) if d == 0 else [0] + list(range(NB_ - 1, 0, -1))
                for bi_ in order:
                    t0 = bi_ * TB
                    tn = TB
                    for gate in range(2):
                        for ic in range(2):
                            widx = ((d * 2 + gate) * 4 + g) * 2 + ic
                            h.MM(pc2, pc2[:, gate, :], lruw[:, widx, j * 128:(j + 1) * 128], xcb[ic][:, t0:t0 + tn], [lruw, xcb[ic]],
                                 start=(ic == 0), stop=(ic == 1))
                    r = rp.next()
                    ii = ip.next()
                    a2 = a2p.next()
                    ab = ap_.next()
                    bb = bp_.next()
                    vcol = d * 8 + cc
                    h.ACT(r, r[:], pc2[:, 0, :], AF.Sigmoid, [pc2, lruv], bias=lruv[:, vcol:vcol + 1])
                    h.ACT(ii, ii[:], pc2[:, 1, :], AF.Sigmoid, [pc2, lruv], bias=lruv[:, 16 + vcol:16 + vcol + 1])
                    h.ACT(ab, ab[:], r[:], AF.Exp, [r, c8], scale=c8[:, vcol:vcol + 1])
                    h.ACT(a2, a2[:], r[:], AF.Exp, [r, c8], scale=c8[:, 16 + vcol:16 + vcol + 1])
                    h.ACT(a2, a2[:], a2[:], AF.Sqrt, [a2], scale=-1.0, bias=1.0)
                    h.TT("dve", ii, ii[:], ii[:], xcb[j][:, t0:t0 + tn], ALU.mult, [ii, xcb[j]])
                    h.TT("dve", bb, bb[:], ii[:], a2[:], ALU.mult, [ii, a2])
                    if d == 0:
                        init = 0.0 if bi_ == 0 else hf[:, t0 - 1:t0]
                        P.op("dve", lambda e, ab=ab, bb=bb, init=init, t0=t0, tn=tn: e.tensor_tensor_scan(
                            out=hf[:, t0:t0 + tn], data0=ab[:], data1=bb[:], initial=init, op0=ALU.mult, op1=ALU.add),
                            reads=[ab, bb, hf], writes=[hf])
                    else:
                        if bi_ == 0:
                            init = 0.0
                        elif bi_ == NB_ - 1:
                            init = hb[:, 0:1]
                        else:
                            init = hb[:, t0 + tn:t0 + tn + 1]
                        P.op("dve", lambda e, ab=ab, bb=bb, init=init, t0=t0, tn=tn, hb=hb: e.tensor_tensor_scan(
                            out=rev_ap(hb[:], t0, tn), data0=rev_ap(ab[:], 0, tn), data1=rev_ap(bb[:], 0, tn), initial=init,
                            op0=ALU.mult, op1=ALU.add), reads=[ab, bb, hb], writes=[hb])
                    yield
            hfv = hf[:, 0:TALL]
            h.TT("dve", hf, hfv, hfv, hb[:], ALU.add, [hf, hb])
            yield
            gl = xc
            h.TT("dve", gl, gl[:], p7[:], p7[:], ALU.mult, [p7])
            h.TS("dve", gl, gl[:], gl[:], 0.044715, 1.0, ALU.mult, ALU.add, [gl])
            yield
            h.TT("dve", gl, gl[:], gl[:], p7[:], ALU.mult, [gl, p7])
            h.ACT(gl, gl[:], gl[:], AF.Sigmoid, [gl], scale=1.5957691216057308)
            yield
            h.TT("dve", gl, gl[:], gl[:], p7[:], ALU.mult, [gl, p7])
            h.TT("dve", p7, p7[:], gl[:], hfv, ALU.mult, [gl, hf])
            h.ST("sp", S.LRUT[cc * 128:(cc + 1) * 128, :], p7, p7[:], G.st0, B.LRUT)
            yield


def phase_d1(k, li, last):
    I, S, B, G, P = k.I, k.S, k.B, k.G, k.P
    t_first = 2 if last else 0
    with ExitStack() as st:
        h = mk_helpers(k, st)
        identf = h.sb("d_identf", [128, 128])
        identb = h.sb("d_identb", [128, 128], BF16)
        h.LD("sp", identf, identf[:], I.ident, G.ld0)
        h.CP("dve", identb, identb[:], identf[:], [identf])
        gn = h.sb("d_gn", [128, D])
        rn = h.sb("d_rn", [128, D])
        h.LD("sp", gn, gn[:], bcast_row(I.gla_norm[li]), G.ld0)
        h.LD("sp", rn, rn[:], bcast_row(I.ret_norm[li]), G.ld0)
        oa = Pool(h.sb, "d_oa", [128, D], F32, 3)
        obp = Pool(h.sb, "d_ob", [128, D], F32, 3)
        gp = Pool(h.sb, "d_g", [128, D], BF16, 3)
        sqp = Pool(h.sb, "d_sq", [128, D], F32, 3)
        stat = Pool(h.sb, "d_stat", [128, 16], F32, 3)
        nb = Pool(h.sb, "d_nb", [128, D], BF16, 3)
        oT = Pool(h.sb, "d_oT", [128, 8, 128], BF16, 3)
        ptr = Pool(h.ps, "d_ptr", [128, 8, 128], BF16, 3)

        def item(i, kind):
            OS, bOf, bOb = (S.OG, B.OGf, B.OGb) if kind == 0 else (S.OR, B.ORf, B.ORb)
            gate_src, bsrc, normw, center = (S.P3, B.P3, gn, False) if kind == 0 else (S.P11, B.P11, rn, True)
            dst, bdst = (S.GLAT, B.GLAT) if kind == 0 else (S.RETT, B.RETT)
            o = oa.next()
            o2 = obp.next()
            g = gp.next()
            h.LD("sp", o, o[:], OS[0][i * 128:(i + 1) * 128, :], G.ld1, [bOf])
            h.LD("sp", o2, o2[:], OS[1][i * 128:(i + 1) * 128, :], G.ld1, [bOb])
            h.LD("sp", g, g[:], gate_src[i * 128:(i + 1) * 128, :], G.ld2, [bsrc])
            yield
            h.TT("dve", o, o[:], o[:], o2[:], ALU.add, [o, o2])
            stt = stat.next()
            sq = sqp.next()
            ov = o[:].rearrange("p (h e) -> p h e", h=4)
            h.ACT(sq, sq[:], g[:], AF.Sigmoid, [g])
            if center:
                P.op("dve", lambda e: e.tensor_reduce(out=stt[:, 0:4], in_=ov, axis=AX.X, op=ALU.add), reads=[o], writes=[stt])
                h.TS("dve", stt, stt[:, 0:4], stt[:, 0:4], -1.0 / 256, None, ALU.mult, None, [stt])
                for hh in range(4):
                    h.TS("dve", o, o[:, hh * 256:(hh + 1) * 256], o[:, hh * 256:(hh + 1) * 256], stt[:, hh:hh + 1], None, ALU.add, None, [o, stt])
            yield
            h.TT("dve", sq, sq[:], sq[:], g[:], ALU.mult, [sq, g])
            h.TT("dve", sq, sq[:], sq[:], normw[:], ALU.mult, [sq, normw])
            n_ = nb.next()
            tmpq = tmpqp.next()
            P.op("dve", lambda e: e.tensor_tensor(out=tmpq[:], in0=o[:], in1=o[:], op=ALU.mult), reads=[o], writes=[tmpq])
            P.op("dve", lambda e: e.tensor_reduce(out=stt[:, 4:8], in_=tmpq[:].rearrange("p (h e) -> p h e", h=4), axis=AX.X, op=ALU.add),
                 reads=[tmpq], writes=[stt])
            h.TS("dve", stt, stt[:, 8:12], stt[:, 4:8], 1.0 / 256, EPS, ALU.mult, ALU.add, [stt])
            h.ACT(stt, stt[:, 8:12], stt[:, 8:12], AF.Sqrt, [stt])
            yield
            h.RECIP(stt, stt[:, 12:16], stt[:, 8:12], [stt])
            for hh in range(4):
                vs = slice(hh * 256, (hh + 1) * 256)
                h.STT("dve", n_, n_[:, vs], o[:, vs], stt[:, 12 + hh:13 + hh], sq[:, vs], ALU.mult, ALU.mult, [o, stt, sq])
            p = ptr.next()
            for kc in range(8):
                h.TR(p, p[:, kc, :], n_[:, kc * 128:(kc + 1) * 128], identb[:], [n_, identb])
            yield
            t_ = oT.next()
            h.CP("act", t_, t_[:], p[:], [p])
            h.ST("sp", dst.rearrange("(kc p) t -> p kc t", p=128)[:, :, i * 128:(i + 1) * 128], t_, t_[:], G.st1, bdst)
            yield

        tmpqp = Pool(h.sb, "d_tmpq", [128, D], F32, 3)
        jobs = [(i, kind) for i in range(t_first, NT) for kind in range(2)]
        active = []
        ji = 0
        NFLY = 3
        while ji < len(jobs) or active:
            if ji < len(jobs) and len(active) < NFLY:
                active.append(item(*jobs[ji]))
                ji += 1
            nxt = []
            for g_ in active:
                try:
                    next(g_)
                    nxt.append(g_)
                except StopIteration:
                    pass
            active = nxt
        P.end_phase(f"d1{li}")


def phase_d2(k, li, x_src, last):
    I, S, B, G, P = k.I, k.S, k.B, k.G, k.P
    t_first = 2 if last else 0
    with ExitStack() as st:
        h = mk_helpers(k, st)
        identf = h.sb("e_identf", [128, 128])
        identb = h.sb("e_identb", [128, 128], BF16)
        h.LD("sp", identf, identf[:], I.ident, G.ld0)
        h.CP("dve", identb, identb[:], identf[:], [identf])
        wbr = [h.sb(f"e_wbr{j}", [128, 8, D], BF16) for j in range(3)]
        wout = h.sb("e_wout", [128, 8, D], BF16)
        for j in range(3):
            h.LD("pool", wbr[j], wbr[j][:], I.w_branch[li, j].rearrange("(kc p) n -> p kc n", p=128), G.w0)
        h.LD("pool", wout, wout[:], I.w_out[li].rearrange("(kc p) n -> p kc n", p=128), G.w0)
        wr = h.sb("e_wr", [128, 8, 32])
        h.LD("sp", wr, wr[:], I.w_router[li].rearrange("(kc p) n -> p kc n", p=128), G.ld0)
        wrh = h.sb("e_wrh", [128, 8, 32], BF16)
        wrl = h.sb("e_wrl", [128, 8, 32], BF16)
        h.CP("dve", wrh, wrh[:], wr[:], [wr])
        h.TT("dve", wr, wr[:], wr[:], wrh[:], ALU.subtract, [wr, wrh])
        h.CP("dve", wrl, wrl[:], wr[:], [wr])
        brb = h.sb("e_brb", [128, 32])
        h.LD("sp", brb, brb[:], bcast_row(I.b_router[li]), G.ld0)
        bdf = h.sb("e_bdf", [32, D])
        bdh = h.sb("e_bdh", [32, D], BF16)
        bdl = h.sb("e_bdl", [32, D], BF16)
        h.LD("sp", bdf, bdf[:], I.b_down[li], G.ld0)
        h.CP("dve", bdh, bdh[:], bdf[:], [bdf])
        h.TT("dve", bdf, bdf[:], bdf[:], bdh[:], ALU.subtract, [bdf, bdh])
        h.CP("dve", bdl, bdl[:], bdf[:], [bdf])
        whl = Pool(h.sb, "e_whl", [128, 64], BF16, 2)
        wT = Pool(h.sb, "e_wT", [32, 2, 128], BF16, 2)
        a0p = Pool(h.sb, "e_a0", [128, D], F32, 2)
        G2, S2 = load_mod_tiles(h, k, li, I.norm2, 3, 4, "e")
        gate1 = []
        for v in range(2):
            g = h.sb(f"e_gate{v}", [128, D])
            h.LD("sp", g, g[:], bcast_row(S.modv[v, 2 * D:3 * D]), G.ld0, [B.modv])
            gate1.append(g)
        srcT = [Pool(h.sb, f"e_src{j}_", [128, 8, 512], BF16, 1) for j in range(3)]
        p12p = Pool(h.sb, "e_p12", [128, 3, 512], BF16, 2)
        sg = Pool(h.sb, "e_sg", [128, 512], F32, 3)
        mrg = Pool(h.sb, "e_mrg", [128, 512], F32, 2)
        mT = h.sb("e_mT", [128, 8, 512], BF16)
        stat = Pool(h.sb, "e_stat", [128, 16], F32, 3)
        xp = Pool(h.sb, "e_x", [128, D], F32, 2)
        tmp = h.sb("e_tmp", [128, D])
        xn2 = h.sb("e_xn2", [128, D])
        xh = h.sb("e_xh", [128, D], BF16)
        xl = h.sb("e_xl", [128, D], BF16)
        xn2Tb = Pool(h.sb, "e_xn2Tb", [128, 8, 128], BF16, 2)
        xlTp = Pool(h.sb, "e_xlT", [128, 8, 128], BF16, 2)
        lg = Pool(h.sb, "e_lg", [128, 32], F32, 2)
        m8 = Pool(h.sb, "e_m8", [128, 8], F32, 2)
        wro = Pool(h.sb, "e_wro", [128, 64], F32, 2)
        ptr = Pool(h.ps, "e_ptr", [128, 8, 128], BF16, 2)
        plg = h.ps("e_plg", [128, 512])
        pbr = Pool(h.ps, "e_pbr", [128, 512], F32, 3)
        pout = h.ps("e_pout", [128, D])
        srcD = [(S.GLAT, B.GLAT), (S.LRUT, B.LRUT), (S.RETT, B.RETT)]
        groups = []
        t = t_first
        while t < NT:
            n = min(4, NT - t)
            groups.append((t, n))
            t += n
        import os
        d2stop = float(os.environ.get("D2STOP", 99))
        if d2stop <= 1:
            P.end_phase(f"d2{li}")
            return
        for (g0, gn_) in groups:
            ntok = gn_ * 128
            c0 = g0 * 128
            srcs = []
            for j in range(3):
                t_ = srcT[j].next()
                h.LD("sp", t_, t_[:, :, 0:ntok], srcD[j][0].rearrange("(kc p) t -> p kc t", p=128)[:, :, c0:c0 + ntok], G.ld3, [srcD[j][1]])
                srcs.append(t_)
            for dc in range(8):
                m = mrg.next()
                p12 = p12p.next()
                h.LD("sp", p12, p12[:, :, 0:ntok], S.P12T.rearrange("(j r) t -> r j t", j=3)[dc * 128:(dc + 1) * 128, :, c0:c0 + ntok], G.ld2, [B.P12T])
                for j in range(3):
                    pb = pbr.next()
                    for kc in range(8):
                        h.MM(pb, pb[:, 0:ntok], wbr[j][:, kc, dc * 128:(dc + 1) * 128], srcs[j][:, kc, 0:ntok], [wbr[j], srcs[j]],
                             start=(kc == 0), stop=(kc == 7))
                    s_ = sg.next()
                    h.ACT(s_, s_[:, 0:ntok], p12[:, j, 0:ntok], AF.Sigmoid, [p12])
                    if j == 0:
                        h.TT("dve", m, m[:, 0:ntok], pb[:, 0:ntok], s_[:, 0:ntok], ALU.mult, [pb, s_])
                    else:
                        h.TT("dve", s_, s_[:, 0:ntok], pb[:, 0:ntok], s_[:, 0:ntok], ALU.mult, [pb, s_])
                        if j == 1:
                            h.TT("dve", m, m[:, 0:ntok], m[:, 0:ntok], s_[:, 0:ntok], ALU.add, [m, s_])
                        else:
                            h.TT("dve", mT, mT[:, dc, 0:ntok], m[:, 0:ntok], s_[:, 0:ntok], ALU.add, [m, s_])
            if d2stop <= 2:
                P.end_phase(f"d2{li}")
                return
            for tl in range(gn_):
                i = g0 + tl
                v = 1 if i < 2 else 0
                for half in range(2):
                    for kc in range(8):
                        h.MM(pout, pout[:, half * 512:(half + 1) * 512], mT[:, kc, tl * 128:(tl + 1) * 128],
                             wout[:, kc, half * 512:(half + 1) * 512], [mT, wout], start=(kc == 0), stop=(kc == 7))
                xt = xp.next()
                h.LD("sp", xt, xt[:], x_src[i * 128:(i + 1) * 128, :], G.ld1, [B.X] if li > 0 else [])
                h.TT("dve", tmp, tmp[:], pout[:], gate1[v][:], ALU.mult, [pout, gate1[v]])
                h.TT("dve", xt, xt[:], xt[:], tmp[:], ALU.add, [xt, tmp])
                h.ST("sp", S.X[i * 128:(i + 1) * 128, :], xt, xt[:], G.st0, B.X)
                if d2stop <= 3:
                    P.end_phase(f"d2{li}")
                    return
                rms_mod_tile(h, k, xt, G2[v], S2[v], xn2, xn2[:], tmp, stat.next())
                if d2stop <= 3.2:
                    P.end_phase(f"d2{li}")
                    return
                xb = xn2Tb.next()
                xlT = xlTp.next()
                h.CP("act", xh, xh[:], xn2[:], [xn2])
                h.TT("dve", xl, xl[:], xn2[:], xh[:], ALU.subtract, [xn2, xh])
                for src_, dst_ in ((xh, xb), (xl, xlT)):
                    p = ptr.next()
                    for kc in range(8):
                        h.TR(p, p[:, kc, :], src_[:, kc * 128:(kc + 1) * 128], identb[:], [src_, identb])
                    h.CP("act" if src_ is xh else "dve", dst_, dst_[:], p[:], [p])
                if d2stop <= 3.5:
                    P.end_phase(f"d2{li}")
                    return
                h.ST("sp", S.XN2T.rearrange("(kc p) t -> p kc t", p=128)[:, :, i * 128:(i + 1) * 128], xb, xb[:], G.st1, B.XN2T)
                if d2stop <= 4:
                    P.end_phase(f"d2{li}")
                    return
                nmm = 0
                for (a_, w_t) in ((xb, wrh), (xlT, wrh), (xb, wrl)):
                    for kc in range(8):
                        h.MM(plg, plg[:, 0:32], a_[:, kc, :], w_t[:, kc, :], [a_, w_t], start=(nmm == 0), stop=(nmm == 23))
                        nmm += 1
                l_ = lg.next()
                h.TT("dve", l_, l_[:], plg[:, 0:32], brb[:], ALU.add, [plg, brb])
                m_ = m8.next()
                P.op("dve", lambda e, m_=m_, l_=l_: e.max(out=m_[:], in_=l_[:]), reads=[l_], writes=[m_])
                w_ = wro.next()
                stt = stat.next()
                h.TS("dve", w_, w_[:, 32:64], l_[:], m_[:, 3:4], None, ALU.is_ge, None, [l_, m_])
                h.TS("dve", stt, stt[:, 0:1], m_[:, 0:1], -1.0, None, ALU.mult, None, [m_])
                h.ACT(l_, l_[:], l_[:], AF.Exp, [l_, stt], bias=stt[:, 0:1])
                h.TT("dve", l_, l_[:], l_[:], w_[:, 32:64], ALU.mult, [l_, w_])
                P.op("dve", lambda e, stt=stt, l_=l_: e.tensor_reduce(out=stt[:, 1:2], in_=l_[:], axis=AX.X, op=ALU.add), reads=[l_], writes=[stt])
                h.RECIP(stt, stt[:, 2:3], stt[:, 1:2], [stt])
                h.TS("dve", w_, w_[:, 0:32], l_[:], stt[:, 2:3], None, ALU.mult, None, [l_, stt])
                h.ST("sp", S.WR[i * 128:(i + 1) * 128, :], w_, w_[:], G.st2, B.WR)
                wh_ = whl.next()
                h.CP("dve", wh_, wh_[:, 0:32], w_[:, 0:32], [w_])
                h.TT("dve", wh_, wh_[:, 32:64], w_[:, 0:32], wh_[:, 0:32], ALU.subtract, [w_, wh_])
                pw = ptr.next()
                h.TR(pw, pw[0:32, 0, :], wh_[:, 0:32], identb[:], [wh_, identb])
                h.TR(pw, pw[0:32, 1, :], wh_[:, 32:64], identb[:], [wh_, identb])
                wT_ = wT.next()
                h.CP("act", wT_, wT_[:], pw[0:32, 0:2, :], [pw])
                for half in range(2):
                    hs_ = slice(half * 512, (half + 1) * 512)
                    h.MM(pout, pout[:, hs_], wT_[:, 0, :], bdh[:, hs_], [wT_, bdh], start=True, stop=False)
                    h.MM(pout, pout[:, hs_], wT_[:, 1, :], bdh[:, hs_], [wT_, bdh], start=False, stop=False)
                    h.MM(pout, pout[:, hs_], wT_[:, 0, :], bdl[:, hs_], [wT_, bdl], start=False, stop=True)
                a0 = a0p.next()
                h.CP("act", a0, a0[:], pout[:], [pout])
                h.ST("sp", S.ACC0[i * 128:(i + 1) * 128, :], a0, a0[:], G.st2, B.ACC0)
        P.end_phase(f"d2{li}")


def phase_f(k, li, last):
    I, S, B, G, P = k.I, k.S, k.B, k.G, k.P
    out = k.out
    t_first = 2 if last else 0
    tiles = list(range(t_first, NT))
    if last:
        groups = [tiles[j:j + 8] for j in range(0, 32, 8)]
    else:
        groups = [tiles[0:9], tiles[9:18], tiles[18:26], tiles[26:34]]
    with ExitStack() as st:
        h = mk_helpers(k, st)
        bgu = h.sb("f_bgu", [128, 32 * 16])
        h.LD("sp", bgu, bgu[:], I.bgu[li], G.ld0)
        gate2 = []
        for v in range(2):
            g = h.sb(f"f_gate{v}", [128, D])
            h.LD("sp", g, g[:], bcast_row(S.modv[v, 5 * D:6 * D]), G.ld0, [B.modv])
            gate2.append(g)
        fnb = None
        if last:
            fnb = h.sb("f_fnb", [128, D])
            h.LD("sp", fnb, fnb[:], bcast_row(I.final_norm), G.ld0)
        wgu = Pool(h.sb, "f_wgu", [128, 8, 1024], BF16, 3)
        wdn = Pool(h.sb, "f_wdn", [128, 4, D], BF16, 3)
        xT = h.sb("f_xT", [128, 8, 9 * 128], BF16)
        acc = h.sb("f_acc", [128, 9, D])
        wr = h.sb("f_wr", [128, 9, 64])
        gcp = Pool(h.sb, "f_gc", [128, 512], F32, 2)
        ucp = Pool(h.sb, "f_uc", [128, 512], F32, 2)
        sip = Pool(h.sb, "f_si", [128, 512], F32, 2)
        actT = Pool(h.sb, "f_actT", [128, 4, 512], BF16, 2)
        xo = Pool(h.sb, "f_xo", [128, D], F32, 2)
        tmp = h.sb("f_tmp", [128, D])
        stat = Pool(h.sb, "f_stat", [128, 4], F32, 2)
        pg = Pool(h.ps, "f_pg", [128, 512], F32, 2)
        pu = Pool(h.ps, "f_pu", [128, 512], F32, 2)
        pd = Pool(h.ps, "f_pd", [128, D], F32, 2)
        wgi = [0]
        for grp in groups:
            ng = len(grp)
            c0 = grp[0] * 128
            ntok = ng * 128
            h.LD("sp", xT, xT[:, :, 0:ntok], S.XN2T.rearrange("(kc p) t -> p kc t", p=128)[:, :, c0:c0 + ntok], G.ld1, [B.XN2T])
            h.LD("sp", wr, wr[:, 0:ng, :], S.WR[c0:c0 + ntok, :].rearrange("(n p) e -> p n e", p=128), G.ld1, [B.WR])
            h.LD("sp", acc, acc[:, 0:ng, :], S.ACC0[c0:c0 + ntok, :].rearrange("(n p) d -> p n d", p=128), G.ld1, [B.ACC0])
            nsub = (ng + 3) // 4
            subs = []
            s0_ = 0
            for q_ in range(nsub):
                sn_ = ng // nsub + (1 if q_ < ng % nsub else 0)
                subs.append((s0_, sn_))
                s0_ += sn_
            def emit_gu(e, hf_, wg_, s0, sn):
                tn = sn * 128
                tc0 = s0 * 128
                aT = actT.next()
                for fl in range(4):
                    fc = hf_ * 4 + fl
                    pg_ = pg.next()
                    pu_ = pu.next()
                    for kc in range(8):
                        h.MM(pg_, pg_[:, 0:tn], wg_[:, kc, fl * 128:(fl + 1) * 128], xT[:, kc, tc0:tc0 + tn], [wg_, xT],
                             start=(kc == 0), stop=(kc == 7))
                    for kc in range(8):
                        h.MM(pu_, pu_[:, 0:tn], wg_[:, kc, 512 + fl * 128:512 + (fl + 1) * 128], xT[:, kc, tc0:tc0 + tn], [wg_, xT],
                             start=(kc == 0), stop=(kc == 7))
                    gc = gcp.next()
                    uc = ucp.next()
                    si = sip.next()
                    bcol = e * 16 + fc
                    h.TS("dve", gc, gc[:, 0:tn], pg_[:, 0:tn], bgu[:, bcol:bcol + 1], 7.0, ALU.add, ALU.min, [pg_, bgu])
                    h.ACT(uc, uc[:, 0:tn], pu_[:, 0:tn], AF.Identity, [pu_, bgu], bias=bgu[:, bcol + 8:bcol + 9])
                    h.ACT(si, si[:, 0:tn], gc[:, 0:tn], AF.Sigmoid, [gc], scale=1.702)
                    h.TS("dve", uc, uc[:, 0:tn], uc[:, 0:tn], 7.0, -7.0, ALU.min, ALU.max, [uc])
                    h.TT("dve", gc, gc[:, 0:tn], gc[:, 0:tn], si[:, 0:tn], ALU.mult, [gc, si])
                    h.STT("dve", aT, aT[:, fl, 0:tn], uc[:, 0:tn], 1.0, gc[:, 0:tn], ALU.add, ALU.mult, [uc, gc])
                return aT

            def emit_down(e, wd_, aT, s0, sn):
                for tl in range(sn):
                    ti = s0 + tl
                    pd_ = pd.next()
                    for half in range(2):
                        for fl in range(4):
                            h.MM(pd_, pd_[:, half * 512:(half + 1) * 512], aT[:, fl, tl * 128:(tl + 1) * 128],
                                 wd_[:, fl, half * 512:(half + 1) * 512], [aT, wd_], start=(fl == 0), stop=(fl == 3))
                    h.STT("dve", acc, acc[:, ti, :], pd_[:], wr[:, ti, e:e + 1], acc[:, ti, :], ALU.mult, ALU.add, [pd_, wr, acc])

            pending = None
            for e in range(32):
                for hf_ in range(2):
                    wg_ = wgu.next()
                    wd_ = wdn.next()
                    src = I.w_gu[li, e].rearrange("(kc p) n -> p kc n", p=128)
                    h.LD("pool", wg_, wg_[:, :, 0:512], src[:, :, hf_ * 512:(hf_ + 1) * 512], None)
                    h.LD("pool", wg_, wg_[:, :, 512:1024], src[:, :, 1024 + hf_ * 512:1024 + (hf_ + 1) * 512], None)
                    h.LD("pool", wd_, wd_[:], I.w_down[li, e, hf_ * 512:(hf_ + 1) * 512, :].rearrange("(fc p) n -> p fc n", p=128), None)
                    for (s0, sn) in subs:
                        aT = emit_gu(e, hf_, wg_, s0, sn)
                        if pending is not None:
                            emit_down(*pending)
                        pending = (e, wd_, aT, s0, sn)
            emit_down(*pending)
            for tl in range(ng):
                i = grp[tl]
                v = 1 if i < 2 else 0
                xt = xo.next()
                h.LD("sp", xt, xt[:], S.X[i * 128:(i + 1) * 128, :], G.ld2, [B.X])
                h.TT("dve", tmp, tmp[:], acc[:, tl, :], gate2[v][:], ALU.mult, [acc, gate2[v]])
                h.TT("dve", xt, xt[:], xt[:], tmp[:], ALU.add, [xt, tmp])
                if not last:
                    h.ST("sp", S.X[i * 128:(i + 1) * 128, :], xt, xt[:], G.st0, B.X)
                else:
                    stt = stat.next()
                    h.ACT(tmp, tmp[:], xt[:], AF.Square, [xt], accum=stt[:, 0:1], extra_w=[stt])
                    h.TS("dve", stt, stt[:, 1:2], stt[:, 0:1], 1.0 / D, EPS, ALU.mult, ALU.add, [stt])
                    h.ACT(stt, stt[:, 2:3], stt[:, 1:2], AF.Sqrt, [stt])
                    h.RECIP(stt, stt[:, 3:4], stt[:, 2:3], [stt])
                    h.STT("dve", xt, xt[:], xt[:], stt[:, 3:4], fnb[:], ALU.mult, ALU.mult, [xt, stt, fnb])
                    h.ST("sp", out[(i - 2) * 128:(i - 1) * 128, :], xt, xt[:], G.out, B.out)
        P.end_phase(f"f{li}")


def host_constants():
    f32 = np.float32
    c = {}
    c["ident"] = np.eye(128, dtype=f32)
    s = np.arange(128)[:, None]
    cc = np.arange(128)[None, :]
    c["tri"] = np.stack([(s <= cc), (s >= cc)]).astype(f32)
    c["maskT"] = np.stack([np.tile((s <= cc), (1, 4)), np.tile((s > cc), (1, 4))]).astype(f32)
    rows = NLAT // 64
    row = np.repeat(np.arange(rows), 64).astype(np.float64)
    col = np.tile(np.arange(64), rows).astype(np.float64)
    nf = 32
    inv = (10000.0 ** (-np.arange(nf, dtype=np.float32) / np.float32(nf))).astype(np.float32)
    ang = np.concatenate([row[:, None].astype(f32) * inv, col[:, None].astype(f32) * inv], axis=-1).astype(f32)
    cos = np.concatenate([np.ones((NCTX, 64), f32), np.cos(ang).astype(f32)], 0)
    sin = np.concatenate([np.zeros((NCTX, 64), f32), np.sin(ang).astype(f32)], 0)
    cos128 = np.concatenate([cos, cos], 1)
    sin128 = np.concatenate([-sin, sin], 1)
    c["cosT"] = np.ascontiguousarray(cos128.T)
    c["sinT"] = np.ascontiguousarray(sin128.T)
    c["cos2"] = np.ascontiguousarray(np.tile(cos128, (1, 4)))
    c["sin2"] = np.ascontiguousarray(np.tile(sin128, (1, 4)))
    gamma = 1.0 - 2.0 ** (-5.0 - np.arange(4, dtype=np.float64))
    lg = np.log(gamma)
    pos = np.arange(128, dtype=np.float64)
    rtab = np.zeros((2, 3, 128, 512), f32)
    rdec = np.zeros((2, 128, 4), f32)
    for d in range(2):
        steps = (pos + 1) if d == 0 else (128 - pos)
        for hh in range(4):
            G_ = steps * lg[hh]
            rtab[d, 0, :, hh * 128:(hh + 1) * 128] = np.exp(G_)[None, :]
            rtab[d, 1, :, hh * 128:(hh + 1) * 128] = np.exp(-G_)[None, :]
            rtab[d, 2, :, hh * 128:(hh + 1) * 128] = np.exp(-G_)[:, None]
            rdec[d, :, hh] = np.exp(128 * lg[hh])
    c["rtab"] = rtab
    c["rdec"] = rdec
    return c


def host_weights(inp):
    f32 = np.float32
    w = {}
    w_in = inp["w_in"]
    rq = w_in[:, :, OFF["rq"]:OFF["rq"] + 512].reshape(2, D, 4, 2, 64)[:, :, :, ::-1, :].reshape(2, D, 512)
    rk = w_in[:, :, OFF["rk"]:OFF["rk"] + 512].reshape(2, D, 4, 2, 64)[:, :, :, ::-1, :].reshape(2, D, 512)
    w["w_in_p"] = np.ascontiguousarray(np.concatenate([w_in, rq, rk], axis=2))
    for n in ["w_ada", "b_ada", "norm1", "norm2", "final_norm", "gla_wa2", "gla_ba", "gla_norm", "ret_norm", "w_branch",
              "w_out", "w_router", "b_router", "w_down", "b_down"]:
        w[n] = np.ascontiguousarray(inp[n])
    cw = np.concatenate([inp["lru_conv_w"], inp["lru_conv_b"][:, None, :]], axis=1)
    w["convw"] = np.ascontiguousarray(cw.reshape(2, 5, 8, 128).transpose(0, 3, 2, 1))
    lw = np.stack([inp["lru_wa"], inp["lru_wi"]], axis=2)
    lw = lw.reshape(2, 2, 2, 4, 2, 128, 256).transpose(0, 5, 1, 2, 3, 4, 6)
    w["lruw"] = np.ascontiguousarray(lw.reshape(2, 128, 32, 256))
    lv = np.stack([inp["lru_ba"], inp["lru_bi"], inp["lru_lam"]], axis=1)
    lv = lv.reshape(2, 3, 2, 8, 128).transpose(0, 4, 1, 2, 3)
    w["lruv"] = np.ascontiguousarray(lv.reshape(2, 128, 48))
    wg = inp["w_gu"].reshape(2, 32, D, 1024, 2).transpose(0, 1, 2, 4, 3)
    w["w_gu_d"] = np.ascontiguousarray(wg.reshape(2, 32, D, 2048))
    bg = inp["b_gu"].reshape(2, 32, 8, 128, 2).transpose(0, 3, 1, 4, 2)
    w["bgu"] = np.ascontiguousarray(bg.reshape(2, 128, 32 * 16))
    return w


_CACHE = {}


def kernel(**inputs):
    inp = {k_: np.asarray(v) for k_, v in inputs.items()}
    if "prog" not in _CACHE:
        _CACHE["prog"] = build_program()
    nc, k = _CACHE["prog"]
    consts = host_constants()
    wts = host_weights(inp)
    in_maps = []
    for b in range(8):
        m = dict(consts)
        m.update(wts)
        m["xall"] = np.ascontiguousarray(np.concatenate([inp["ctx"][b], inp["x"][b]], axis=0))
        cv = np.concatenate([inp["c"][b].reshape(8, 128).T, inp["c_ctx"].reshape(8, 128).T], axis=1)
        m["cvec"] = np.ascontiguousarray(cv.astype(np.float32))
        in_maps.append(m)
    res = run_bass_kernel_spmd(nc, in_maps, core_ids=list(range(8)))
    return np.stack([np.asarray(r["out"]) for r in res.results], axis=0).astype(np.float32)
```
